# Optimizing a Trainium2 kernel written in Bass

```python
import jax
import jax.numpy as jnp
from jax import lax
import numpy as np

D_MODEL = 1024
BATCH = 8
SEQ = 4096
DEPTH = 2

CTX_LEN = 256
GRID_W = 64

BRANCH_WIDTH = 512
N_BRANCH = 3

POOL_WINDOWS = (2, 4, 8, 16)
POOL_WIDTH = BRANCH_WIDTH
POOL_GROUP = POOL_WIDTH // len(POOL_WINDOWS)

N_Q_HEADS = 8
N_KV_HEADS = 2
HEAD_DIM = 64
GQA_GROUP = N_Q_HEADS // N_KV_HEADS
Q_WIDTH = N_Q_HEADS * HEAD_DIM
KV_WIDTH = N_KV_HEADS * HEAD_DIM
Q_BLOCK = 128
ROPE_THETA = 10000.0
ATTN_SCALE = HEAD_DIM ** -0.5

LRU_WIDTH = BRANCH_WIDTH
LRU_BLOCKS = 8
LRU_BLOCK_DIM = LRU_WIDTH // LRU_BLOCKS
CONV_WIDTH = 4
RG_C = 8.0

OFF_Q = POOL_WIDTH
OFF_K = OFF_Q + Q_WIDTH
OFF_V = OFF_K + KV_WIDTH
OFF_LX = OFF_V + KV_WIDTH
OFF_LG = OFF_LX + LRU_WIDTH
OFF_GATE = OFF_LG + LRU_WIDTH
W_IN = OFF_GATE + N_BRANCH * D_MODEL
SPLITS = (OFF_Q, OFF_K, OFF_V, OFF_LX, OFF_LG, OFF_GATE)

D_FF = 2816
N_EXPERTS = 8
TOP_K = 2
D_EXPERT = 3584
EXPERT_BLOCK = 256
N_DENSE = (DEPTH + 1) // 2
N_MOE = DEPTH // 2

ALPHA = (2 * DEPTH) ** 0.25
BETA = (8 * DEPTH) ** -0.25
LN_EPS = 1e-5
RMS_EPS = 1e-6
F32 = jnp.float32

kernel_name = 'hybrid_pool_gqa_rglru_moe_dit'


def layer_norm(x, g, b):
    xf = x.astype(F32)
    mu = jnp.mean(xf, axis=-1, keepdims=True)
    var = jnp.mean(jnp.square(xf - mu), axis=-1, keepdims=True)
    return ((xf - mu) * lax.rsqrt(var + LN_EPS) * g.astype(F32) + b.astype(F32)).astype(x.dtype)


def rms_norm(x, g):
    xf = x.astype(F32)
    return (xf * lax.rsqrt(jnp.mean(xf * xf, axis=-1, keepdims=True) + RMS_EPS) * g.astype(F32)).astype(x.dtype)


def axial_rope(x, row_pos, col_pos):
    half = HEAD_DIM // 2
    nf = half // 2
    inv = ROPE_THETA ** (-jnp.arange(nf, dtype=F32) / nf)
    xf = x.astype(F32)

    def rotate(xa, pos):
        ang = pos[:, None] * inv
        cos = jnp.cos(ang)[None, :, None, :]
        sin = jnp.sin(ang)[None, :, None, :]
        x1, x2 = xa[..., :nf], xa[..., nf:]
        return jnp.concatenate([x1 * cos - x2 * sin, x2 * cos + x1 * sin], axis=-1)

    out = jnp.concatenate([rotate(xf[..., :half], row_pos), rotate(xf[..., half:], col_pos)], axis=-1)
    return out.astype(x.dtype)


def attend(q, k, v):
    B, Lq = q.shape[:2]
    qg = q.reshape(B, Lq, N_KV_HEADS, GQA_GROUP, HEAD_DIM)
    s = jnp.einsum('bqhgd,bkhd->bhgqk', qg, k, preferred_element_type=F32) * ATTN_SCALE
    p = jax.nn.softmax(s, axis=-1).astype(v.dtype)
    o = jnp.einsum('bhgqk,bkhd->bqhgd', p, v)
    return o.reshape(B, Lq, Q_WIDTH)


def blocked_attention(q, k, v):
    B, S = q.shape[:2]
    nb = S // Q_BLOCK
    qb = jnp.moveaxis(q.reshape(B, nb, Q_BLOCK, N_Q_HEADS, HEAD_DIM), 1, 0)
    ob = lax.map(lambda qq: attend(qq, k, v), qb)
    return jnp.moveaxis(ob, 0, 1).reshape(B, S, Q_WIDTH)


def multiscale_pool(x, pool_w, pool_scale):
    B, L, _ = x.shape
    xf = x.astype(F32)
    cs = jnp.concatenate([jnp.zeros((B, 1, POOL_WIDTH), F32), jnp.cumsum(xf, axis=1)], axis=1)
    t = jnp.arange(L)
    outs = []
    for g, w in enumerate(POOL_WINDOWS):
        lo = w // 2
        start = jnp.clip(t - lo, 0, L)
        end = jnp.clip(t - lo + w, 0, L)
        sl = slice(g * POOL_GROUP, (g + 1) * POOL_GROUP)
        csg = cs[..., sl]
        mean = (jnp.take(csg, end, axis=1) - jnp.take(csg, start, axis=1)) / (end - start).astype(F32)[None, :, None]
        outs.append(jnp.einsum('blc,cd->bld', (mean - xf[..., sl]).astype(x.dtype), pool_w[g]))
    return jnp.concatenate(outs, axis=-1) * pool_scale


def centred_dwconv(x, w, b):
    L = x.shape[1]
    lo = CONV_WIDTH // 2
    hi = CONV_WIDTH - 1 - lo
    xp = jnp.pad(x, ((0, 0), (lo, hi), (0, 0)))
    return sum(xp[:, k:k + L] * w[k] for k in range(CONV_WIDTH)) + b


def _lin_combine(left, right):
    a1, b1 = left
    a2, b2 = right
    return a1 * a2, a2 * b1 + b2


def rglru(x, wa, ba, wx, bx, lam, h0, reverse):
    B, L, _ = x.shape
    xb = x.reshape(B, L, LRU_BLOCKS, LRU_BLOCK_DIM)
    r = jax.nn.sigmoid(jnp.einsum('blhi,hij->blhj', xb, wa).reshape(B, L, LRU_WIDTH).astype(F32) + ba.astype(F32))
    i = jax.nn.sigmoid(jnp.einsum('blhi,hij->blhj', xb, wx).reshape(B, L, LRU_WIDTH).astype(F32) + bx.astype(F32))
    log_a = -RG_C * r * jax.nn.softplus(-lam.astype(F32))
    a = jnp.exp(log_a)
    u = jnp.sqrt(-jnp.expm1(2.0 * log_a)) * i * x.astype(F32)
    if h0 is not None:
        edge = -1 if reverse else 0
        u = u.at[:, edge].add(a[:, edge] * h0)
    _, h = lax.associative_scan(_lin_combine, (a, u), reverse=reverse, axis=1)
    return h


def merge_branches(pool_y, attn_y, lru_y, gates, w_branch, w_out):
    ys = jnp.stack([pool_y, attn_y, lru_y], axis=2)
    branch = jnp.einsum('blnc,ncd->blnd', ys, w_branch)
    return jnp.sum(gates.astype(branch.dtype) * branch, axis=2) @ w_out


def token_mixer(h_ctx, h_lat, row_pos, col_pos, w_in, b_merge, pool_w, pool_scale, q_norm, k_norm,
                conv_w, conv_b, lru_wa, lru_ba, lru_wx, lru_bx, lru_lambda, w_branch, w_out, need_ctx):
    dtype = h_lat.dtype

    def project(h):
        B, L, _ = h.shape
        z = h @ w_in
        p_in, q, k, v, lx, lg, gz = jnp.split(z, SPLITS, axis=-1)
        q = rms_norm(q.reshape(B, L, N_Q_HEADS, HEAD_DIM), q_norm)
        k = rms_norm(k.reshape(B, L, N_KV_HEADS, HEAD_DIM), k_norm)
        v = v.reshape(B, L, N_KV_HEADS, HEAD_DIM)
        lx = centred_dwconv(lx, conv_w, conv_b)
        gates = jax.nn.sigmoid(gz.reshape(B, L, N_BRANCH, D_MODEL).astype(F32) + b_merge.astype(F32))
        return p_in, q, k, v, lx, lg, gates

    p_c, q_c, k_c, v_c, lx_c, lg_c, g_c = project(h_ctx)
    p_l, q_l, k_l, v_l, lx_l, lg_l, g_l = project(h_lat)

    q_l = axial_rope(q_l, row_pos, col_pos)
    k_l = axial_rope(k_l, row_pos, col_pos)
    k_all = jnp.concatenate([k_c, k_l], axis=1)
    v_all = jnp.concatenate([v_c, v_l], axis=1)
    attn_l = blocked_attention(q_l, k_all, v_all)

    h_cf = rglru(lx_c, lru_wa[0], lru_ba[0], lru_wx[0], lru_bx[0], lru_lambda[0], None, False)
    h_cb = rglru(lx_c, lru_wa[1], lru_ba[1], lru_wx[1], lru_bx[1], lru_lambda[1], None, True)
    h_lf = rglru(lx_l, lru_wa[0], lru_ba[0], lru_wx[0], lru_bx[0], lru_lambda[0], h_cf[:, -1], False)
    h_lb = rglru(lx_l, lru_wa[1], lru_ba[1], lru_wx[1], lru_bx[1], lru_lambda[1], h_cb[:, 0], True)
    lru_l = ((h_lf + h_lb) * jax.nn.gelu(lg_l.astype(F32))).astype(dtype)

    y_lat = merge_branches(multiscale_pool(p_l, pool_w, pool_scale), attn_l, lru_l, g_l, w_branch, w_out)
    if need_ctx:
        attn_c = attend(q_c, k_c, v_c)
        lru_c = ((h_cf + h_cb) * jax.nn.gelu(lg_c.astype(F32))).astype(dtype)
        y_ctx = merge_branches(multiscale_pool(p_c, pool_w, pool_scale), attn_c, lru_c, g_c, w_branch, w_out)
    else:
        y_ctx = None
    return y_ctx, y_lat


def swiglu(x, w_gate, w_up, w_down):
    return (jax.nn.silu(x @ w_gate) * (x @ w_up)) @ w_down


def moe_swiglu(t, router, router_b, w_gate, w_up, w_down):
    N, D = t.shape
    logits = (t @ router + router_b).astype(F32)
    top_v, top_i = lax.top_k(logits, TOP_K)
    probs = jax.nn.softmax(top_v, axis=-1)
    M = N * TOP_K
    flat_e = top_i.reshape(M)
    order = jnp.argsort(flat_e)
    sorted_e = flat_e[order]
    sorted_tok = order // TOP_K
    sorted_p = probs.reshape(M)[order]
    counts = jnp.bincount(flat_e, length=N_EXPERTS)
    padded = (counts + EXPERT_BLOCK - 1) // EXPERT_BLOCK * EXPERT_BLOCK
    start = jnp.cumsum(counts) - counts
    ends_p = jnp.cumsum(padded)
    start_p = ends_p - padded
    dest = start_p[sorted_e] + jnp.arange(M) - start[sorted_e]
    n_blocks = -(-(M + N_EXPERTS * (EXPERT_BLOCK - 1)) // EXPERT_BLOCK)
    P = n_blocks * EXPERT_BLOCK
    buf_tok = jnp.full((P,), N, jnp.int32).at[dest].set(sorted_tok.astype(jnp.int32))
    buf_p = jnp.zeros((P,), F32).at[dest].set(sorted_p)
    block_e = jnp.minimum(jnp.searchsorted(ends_p, jnp.arange(n_blocks) * EXPERT_BLOCK, side='right'), N_EXPERTS - 1)
    t_pad = jnp.concatenate([t, jnp.zeros((1, D), t.dtype)], axis=0)
    xb = t_pad[buf_tok].reshape(n_blocks, EXPERT_BLOCK, D)

    def expert_block(args):
        xe, e = args
        return swiglu(xe, w_gate[e], w_up[e], w_down[e])

    yb = lax.map(expert_block, (xb, block_e))
    y = yb.reshape(P, D) * buf_p[:, None].astype(t.dtype)
    return jnp.zeros((N + 1, D), t.dtype).at[buf_tok].add(y)[:N]


def setup_inputs(seed: int = 0) -> dict:
    key = jax.random.key(seed)
    ks = jax.random.split(key, 40)

    def nrm(i, shape, scale):
        return jax.random.normal(ks[i], shape, F32) * scale

    u = jax.random.uniform(ks[30], (DEPTH, 2, LRU_WIDTH), F32, 0.9, 0.999)
    a0 = u ** (1.0 / RG_C)
    lru_lambda = jnp.log(a0) - jnp.log1p(-a0)
    return {
        'x': nrm(0, (BATCH, SEQ, D_MODEL), 1.0),
        'c': nrm(1, (BATCH, D_MODEL), 1.0),
        'ctx': nrm(2, (BATCH, CTX_LEN, D_MODEL), 1.0),
        'c_ctx': nrm(3, (D_MODEL,), 1.0),
        'w_mod': nrm(4, (DEPTH, D_MODEL, 6 * D_MODEL), 0.5 * D_MODEL ** -0.5),
        'b_mod': nrm(5, (DEPTH, 6 * D_MODEL), 0.02),
        'w_in': nrm(6, (DEPTH, D_MODEL, W_IN), D_MODEL ** -0.5),
        'b_merge': nrm(7, (DEPTH, N_BRANCH, D_MODEL), 0.1),
        'pool_w': nrm(8, (DEPTH, len(POOL_WINDOWS), POOL_GROUP, POOL_GROUP), POOL_GROUP ** -0.5),
        'pool_scale': 1.0 + nrm(9, (DEPTH, POOL_WIDTH), 0.1),
        'q_norm': 1.0 + nrm(10, (DEPTH, HEAD_DIM), 0.1),
        'k_norm': 1.0 + nrm(11, (DEPTH, HEAD_DIM), 0.1),
        'conv_w': nrm(12, (DEPTH, CONV_WIDTH, LRU_WIDTH), CONV_WIDTH ** -0.5),
        'conv_b': nrm(13, (DEPTH, LRU_WIDTH), 0.02),
        'lru_wa': nrm(14, (DEPTH, 2, LRU_BLOCKS, LRU_BLOCK_DIM, LRU_BLOCK_DIM), LRU_BLOCK_DIM ** -0.5),
        'lru_ba': nrm(15, (DEPTH, 2, LRU_WIDTH), 0.1),
        'lru_wx': nrm(16, (DEPTH, 2, LRU_BLOCKS, LRU_BLOCK_DIM, LRU_BLOCK_DIM), LRU_BLOCK_DIM ** -0.5),
        'lru_bx': nrm(17, (DEPTH, 2, LRU_WIDTH), 0.1),
        'lru_lambda': lru_lambda,
        'w_branch': nrm(18, (DEPTH, N_BRANCH, BRANCH_WIDTH, D_MODEL), BRANCH_WIDTH ** -0.5),
        'w_out': nrm(19, (DEPTH, D_MODEL, D_MODEL), BETA * D_MODEL ** -0.5),
        'ln1_g': 1.0 + nrm(20, (DEPTH, D_MODEL), 0.1),
        'ln1_b': nrm(21, (DEPTH, D_MODEL), 0.02),
        'ffn_w_gate': nrm(22, (N_DENSE, D_MODEL, D_FF), D_MODEL ** -0.5),
        'ffn_w_up': nrm(23, (N_DENSE, D_MODEL, D_FF), D_MODEL ** -0.5),
        'ffn_w_down': nrm(24, (N_DENSE, D_FF, D_MODEL), BETA * D_FF ** -0.5),
        'moe_router': nrm(25, (N_MOE, D_MODEL, N_EXPERTS), D_MODEL ** -0.5),
        'moe_router_b': nrm(26, (N_MOE, N_EXPERTS), 0.01),
        'moe_w_gate': nrm(27, (N_MOE, N_EXPERTS, D_MODEL, D_EXPERT), D_MODEL ** -0.5),
        'moe_w_up': nrm(28, (N_MOE, N_EXPERTS, D_MODEL, D_EXPERT), D_MODEL ** -0.5),
        'moe_w_down': nrm(29, (N_MOE, N_EXPERTS, D_EXPERT, D_MODEL), BETA * D_EXPERT ** -0.5),
        'ln2_g': 1.0 + nrm(31, (DEPTH, D_MODEL), 0.1),
        'ln2_b': nrm(32, (DEPTH, D_MODEL), 0.02),
    }


def reference(x, c, ctx, c_ctx, w_mod, b_mod, w_in, b_merge, pool_w, pool_scale, q_norm, k_norm,
              conv_w, conv_b, lru_wa, lru_ba, lru_wx, lru_bx, lru_lambda, w_branch, w_out, ln1_g, ln1_b,
              ffn_w_gate, ffn_w_up, ffn_w_down, moe_router, moe_router_b, moe_w_gate, moe_w_up, moe_w_down,
              ln2_g, ln2_b):
    B, S, D = x.shape
    rows = S // GRID_W
    row_pos = jnp.repeat(jnp.arange(rows, dtype=F32), GRID_W)
    col_pos = jnp.tile(jnp.arange(GRID_W, dtype=F32), rows)
    x_lat, x_ctx = x, ctx
    for l in range(DEPTH):
        last = l == DEPTH - 1
        m_lat = jnp.split((jax.nn.silu(c) @ w_mod[l] + b_mod[l])[:, None, :], 6, axis=-1)
        m_ctx = jnp.split(jax.nn.silu(c_ctx) @ w_mod[l] + b_mod[l], 6, axis=-1)

        h_lat = x_lat * (1 + m_lat[1]) + m_lat[0]
        h_ctx = x_ctx * (1 + m_ctx[1]) + m_ctx[0]
        y_ctx, y_lat = token_mixer(h_ctx, h_lat, row_pos, col_pos, w_in[l], b_merge[l], pool_w[l], pool_scale[l],
                                   q_norm[l], k_norm[l], conv_w[l], conv_b[l], lru_wa[l], lru_ba[l], lru_wx[l],
                                   lru_bx[l], lru_lambda[l], w_branch[l], w_out[l], not last)
        x_lat = layer_norm(ALPHA * x_lat + m_lat[2] * y_lat, ln1_g[l], ln1_b[l])
        if not last:
            x_ctx = layer_norm(ALPHA * x_ctx + m_ctx[2] * y_ctx, ln1_g[l], ln1_b[l])

        h2_lat = x_lat * (1 + m_lat[4]) + m_lat[3]
        if last:
            tokens = h2_lat.reshape(B * S, D)
        else:
            h2_ctx = x_ctx * (1 + m_ctx[4]) + m_ctx[3]
            tokens = jnp.concatenate([h2_ctx.reshape(-1, D), h2_lat.reshape(B * S, D)], axis=0)
        if l % 2 == 0:
            j = l // 2
            f = swiglu(tokens, ffn_w_gate[j], ffn_w_up[j], ffn_w_down[j])
        else:
            j = l // 2
            f = moe_swiglu(tokens, moe_router[j], moe_router_b[j], moe_w_gate[j], moe_w_up[j], moe_w_down[j])
        n_ctx_tok = tokens.shape[0] - B * S
        x_lat = layer_norm(ALPHA * x_lat + m_lat[5] * f[n_ctx_tok:].reshape(B, S, D), ln2_g[l], ln2_b[l])
        if not last:
            f_ctx = f[:n_ctx_tok].reshape(x_ctx.shape)
            x_ctx = layer_norm(ALPHA * x_ctx + m_ctx[5] * f_ctx, ln2_g[l], ln2_b[l])
    return x_lat
```

```python
import contextlib
import os
BLK_STAGE = int(os.environ.get('BLK_STAGE', '3'))
NBLK_RUN = int(os.environ.get('NBLK_RUN', '24'))
import numpy as np
import concourse.bass as bass
import concourse.mybir as mybir
from concourse.bass_utils import run_bass_kernel_spmd
from concourse.bass import IndirectOffsetOnAxis

F32 = mybir.dt.float32
BF16 = mybir.dt.bfloat16
I32 = mybir.dt.int32
U32 = mybir.dt.uint32
AF = mybir.ActivationFunctionType
ALU = mybir.AluOpType
AX = mybir.AxisListType

D = 1024
SEQ = 4096
CTX = 256
NT = SEQ + CTX
DEPTH = 2
W_IN = 5376
D_FF = 2816
D_EXP = 3584
N_EXP = 8
MB = 512
NBLK = 24
NSLOT = MB * NBLK
SPARSE_MOE = True
ALPHA = (2 * DEPTH) ** 0.25
LN_EPS = 1e-5
RMS_EPS = 1e-6
PADW = NT + 32
TILES = [(0, 256)] + [(256 + 512 * i, 512) for i in range(8)]
POOL_WINDOWS = (2, 4, 8, 16)

V_BMOD, V_BMERGE, V_PSCALE, V_CONVW, V_CONVB = 0, 48, 72, 76, 92
V_BA, V_BX, V_LAM, V_QN, V_KN = 96, 104, 112, 120, 121
V_LN1G, V_LN1B, V_LN2G, V_LN2B, V_RB = 122, 130, 138, 146, 154
NV = 162


def pad_pos(s):
    return s + 8 if s < CTX else s + 24


class Buf:
    __slots__ = ("name", "w", "r")

    def __init__(self, name=""):
        self.name = name
        self.w = None
        self.r = []


class Op:
    __slots__ = ("eng", "fn", "deps", "needs_inc", "count", "dma", "sem", "waits")

    def __init__(self, eng, fn, dma):
        self.eng = eng
        self.fn = fn
        self.deps = set()
        self.needs_inc = False
        self.count = 0
        self.dma = dma
        self.sem = None
        self.waits = []


ENGS = ["pe", "act", "dve", "pool", "sp"]


class Sched:
    def __init__(self, nc, es, n_dma_sems=48):
        self.nc = nc
        self.n_dma_sems = n_dma_sems
        self.esem = {e: es.enter_context(nc.semaphore(f"se_{e}")) for e in ENGS}
        self.dsem = [es.enter_context(nc.semaphore(f"sd_{i}")) for i in range(n_dma_sems)]
        self.ecount = {e: 0 for e in ENGS}
        self.dma_cnt = [0] * n_dma_sems
        self.dma_rr = 0
        self.total_ops = 0
        self._reset_phase()

    def _reset_phase(self):
        self.ops = {e: [] for e in ENGS}
        self.all = []
        self.dma_last = [None] * self.n_dma_sems
        self.touched = set()

    def add(self, eng, fn, reads=(), writes=(), dma=False):
        op = Op(eng, fn, dma)
        for b in reads:
            if b.w is not None:
                op.deps.add(b.w)
        for b in writes:
            if b.w is not None:
                op.deps.add(b.w)
            for r in b.r:
                op.deps.add(r)
        for b in reads:
            b.r.append(op)
            self.touched.add(b)
        for b in writes:
            b.w = op
            b.r = []
            self.touched.add(b)
        if dma:
            s = self.dma_rr
            self.dma_rr = (self.dma_rr + 1) % self.n_dma_sems
            prev = self.dma_last[s]
            if prev is not None:
                op.deps.add(prev)
            self.dma_last[s] = op
            self.dma_cnt[s] += 1
            op.sem = s
            op.count = self.dma_cnt[s] * 16
            op.needs_inc = True
        op.deps.discard(op)
        self.ops[eng].append(op)
        self.all.append(op)
        return op

    def flush(self):
        nc = self.nc
        for op in self.all:
            nd = set()
            for d in op.deps:
                if (not d.dma) and (not op.dma) and d.eng == "pe" and op.eng == "pe":
                    continue
                nd.add(d)
            op.deps = nd
            for d in nd:
                d.needs_inc = True
        for e in ENGS:
            for op in reversed(self.ops[e]):
                if not op.dma:
                    op.needs_inc = True
                    break
        for e in ENGS:
            c = self.ecount[e]
            for op in self.ops[e]:
                if op.dma:
                    continue
                if op.needs_inc:
                    c += 1
                    op.count = c
            self.ecount[e] = c
        seen = {e: {} for e in ENGS}
        for e in ENGS:
            sn = seen[e]
            for op in self.ops[e]:
                need = {}
                for d in op.deps:
                    key = ("d", d.sem) if d.dma else ("e", d.eng)
                    if d.count > need.get(key, 0):
                        need[key] = d.count
                for key, v in need.items():
                    if sn.get(key, 0) >= v:
                        continue
                    sn[key] = v
                    op.waits.append((key, v))
        bar = []
        for e in ENGS:
            if self.ecount[e] > 0:
                bar.append((("e", e), self.ecount[e]))
        for s in range(self.n_dma_sems):
            if self.dma_cnt[s] > 0:
                bar.append((("d", s), self.dma_cnt[s] * 16))
        handles = {"pe": "tensor", "act": "scalar", "dve": "vector", "pool": "gpsimd", "sp": "sync"}
        with nc.Block() as block:
            def run(ename, eng):
                sn = seen[ename]
                for op in self.ops[ename]:
                    for key, v in op.waits:
                        sem = self.dsem[key[1]] if key[0] == "d" else self.esem[key[1]]
                        eng.wait_ge(sem, v)
                    ins = op.fn(eng)
                    if op.dma:
                        ins.then_inc(self.dsem[op.sem], 16)
                    elif op.needs_inc:
                        ins.then_inc(self.esem[ename], 1)
                for key, v in bar:
                    if key == ("e", ename):
                        continue
                    if sn.get(key, 0) >= v:
                        continue
                    sem = self.dsem[key[1]] if key[0] == "d" else self.esem[key[1]]
                    eng.wait_ge(sem, v)

            for ename in ENGS:
                getattr(block, handles[ename])(lambda eng, ename=ename: run(ename, eng))
        self.total_ops += len(self.all)
        for b in self.touched:
            b.w = None
            b.r = []
        self._reset_phase()


def rev(ap2d):
    (ps, pn), (fs, fn) = ap2d.ap
    return bass.AP(ap2d.tensor, ap2d.offset + fs * (fn - 1), [[ps, pn], [-fs, fn]])


def bcast_last(ap2d, n):
    (ps, pn), (fs, fn) = ap2d.ap
    return bass.AP(ap2d.tensor, ap2d.offset, [[ps, pn], [fs, fn], [0, n]])


def pbcast(ap_row, nparts=128):
    dims = list(ap_row.ap)
    return bass.AP(ap_row.tensor, ap_row.offset, [[0, nparts]] + [list(d) for d in dims[1:]])


class Ker:
    def __init__(self, dbg=False, layers=(0, 1), stop_after=None):
        self.dbg = dbg
        self.layers = layers
        self.stop_after = stop_after
        self.nc = bass.Bass("TRN2", target_bir_lowering=False)
        self.rot = {}

    def dma(self, q, out, in_, reads=(), writes=()):
        if q == "st":
            q = "sp"
            for b in reads:
                if b.w is not None and (not b.w.dma) and b.w.eng in ("act", "dve", "pool"):
                    q = {"act": "act", "pool": "pool", "dve": getattr(self, "st_dve_q", "pool")}[b.w.eng]
                    break
        return self.S.add(q, lambda e: e.dma_start(out=out, in_=in_), reads, writes, dma=True)

    def mm(self, out, lhsT, rhs, start, stop, reads, writes):
        return self.S.add("pe", lambda e: e.matmul(out, lhsT=lhsT, rhs=rhs, start=start, stop=stop), reads, writes)

    def tr(self, out, in_, ident, reads, writes):
        return self.S.add("pe", lambda e: e.transpose(out, in_, ident), reads, writes)

    def act(self, out, in_, func, reads, writes, scale=1.0, bias=None):
        if bias is None:
            return self.S.add("act", lambda e: e.activation(out=out, in_=in_, func=func, scale=scale), reads, writes)
        return self.S.add("act", lambda e: e.activation(out=out, in_=in_, func=func, scale=scale, bias=bias), reads, writes)

    def tt(self, eng, out, in0, in1, op, reads, writes):
        return self.S.add(eng, lambda e: e.tensor_tensor(out=out, in0=in0, in1=in1, op=op), reads, writes)

    def ts(self, eng, out, in0, s1, op0, reads, writes, s2=None, op1=None):
        if op1 is None:
            return self.S.add(eng, lambda e: e.tensor_scalar(out, in0, s1, None, op0), reads, writes)
        return self.S.add(eng, lambda e: e.tensor_scalar(out=out, in0=in0, scalar1=s1, scalar2=s2, op0=op0, op1=op1), reads, writes)

    def stt(self, out, in0, scalar, in1, op0, op1, reads, writes):
        return self.S.add("dve", lambda e: e.scalar_tensor_tensor(out=out, in0=in0, scalar=scalar, in1=in1, op0=op0, op1=op1), reads, writes)

    def recip(self, out, in_, reads, writes):
        return self.S.add("dve", lambda e: e.reciprocal(out=out, in_=in_), reads, writes)

    def copy(self, eng, out, in_, reads, writes):
        if eng == "act":
            return self.act(out, in_, AF.Copy, reads, writes)
        return self.S.add(eng, lambda e: e.tensor_copy(out=out, in_=in_), reads, writes)

    def memset(self, eng, out, val, writes):
        return self.S.add(eng, lambda e: e.memset(out, val), (), writes)

    def scan(self, out, d0, d1, initial, reads, writes):
        return self.S.add("dve", lambda e: e.tensor_tensor_scan(out=out, data0=d0, data1=d1, initial=initial,
                                                                  op0=ALU.mult, op1=ALU.add), reads, writes)

    def rmax(self, out, in_, reads, writes):
        return self.S.add("dve", lambda e: e.tensor_reduce(out=out, in_=in_, op=ALU.max, axis=AX.X), reads, writes)

    def subflush(self, name):
        with self.nc.named_scope(name):
            self.S.flush()

    def un(self, name):
        self.uid = getattr(self, "uid", 0) + 1
        return f"{name}_{self.uid}"

    def sb(self, es, name, shape, dt):
        t = es.enter_context(self.nc.sbuf_tensor(self.un(name), shape, dt))
        return t, Buf(name)

    def sbn(self, es, name, shape, dt, n):
        ts_, bs = [], []
        for i in range(n):
            t, b = self.sb(es, f"{name}{i}", shape, dt)
            ts_.append(t)
            bs.append(b)
        return ts_, bs

    def psn(self, es, name, n, shape=(128, 512), dt=F32):
        ts_, bs = [], []
        for i in range(n):
            ts_.append(es.enter_context(self.nc.psum_tensor(self.un(f"{name}{i}"), list(shape), dt)))
            bs.append(Buf(f"{name}{i}"))
        return ts_, bs

    def nxt(self, key, n):
        v = self.rot.get(key, 0)
        self.rot[key] = v + 1
        return v % n

    def dram(self, name, shape, dt, out=False):
        kind = "ExternalOutput" if (out or self.dbg) else "Internal"
        return self.nc.dram_tensor(name, list(shape), dt, kind=kind).ap()

    def build(self):
        nc = self.nc
        I = lambda name, shape: nc.dram_tensor(name, list(shape), F32, kind="ExternalInput").ap()
        self.i_xT = I("xT", (D, SEQ))
        self.i_ctxT = I("ctxT", (D, CTX))
        self.i_cvec = I("cvec", (128, 8, 2))
        self.i_vecs = I("vecs", (DEPTH, 128, NV))
        self.i_wmod = I("w_mod", (DEPTH, D, 6 * D))
        self.i_win = I("w_in", (DEPTH, D, W_IN))
        self.i_poolw = I("pool_w", (DEPTH, 4, 128, 128))
        self.i_lrubd = I("lru_bd", (DEPTH, 4, 4, 128, 128))
        self.i_wbr = I("w_branch", (DEPTH, 1536, D))
        self.i_wout = I("w_out", (DEPTH, D, D))
        self.i_fg = I("ffn_w_gate", (D, D_FF))
        self.i_fu = I("ffn_w_up", (D, D_FF))
        self.i_fd = I("ffn_w_down", (D_FF, D))
        self.i_router = I("moe_router", (D, N_EXP))
        self.i_mg = I("moe_g2", (N_EXP * 128 * 16, 1792))
        self.i_mu = I("moe_u2", (N_EXP * 128 * 16, 1792))
        self.i_md = I("moe_d2", (N_EXP * 128 * 16, 1792))
        self.i_consts = I("consts", (6, 128, 128))
        self.i_cos = I("cosT", (128, SEQ))
        self.i_sin = I("sinT", (128, SEQ))
        self.i_invcnt = I("invcnt", (4, PADW))
        self.i_sel8 = I("sel8", (8, 8, 128))
        self.i_mc = I("mconst", (8, 64))
        self.i_mc2 = I("mconst2", (128, 32))
        self.d_xs1 = self.dram("xs1", (128, 8, NT), F32)
        self.d_h = self.dram("hT", (128, 8, NT), BF16)
        self.d_q = self.dram("qT", (4, 128, NT), BF16)
        self.d_y = self.dram("yT", (128, 12, NT), BF16)
        self.d_x1 = self.dram("x1T", (128, 8, NT), F32)
        self.d_h2 = self.dram("h2T", (128, 8, NT), BF16)
        self.d_out = self.dram("outT", (128, 8, SEQ), F32, out=True)
        self.d_gate = self.dram("gateT", (8, SEQ), F32)
        self.d_xs = self.dram("Xs", (NSLOT, D), BF16)
        self.d_ys = self.dram("Ys", (NSLOT, D), F32)
        if self.dbg:
            self.d_mod = self.dram("dbg_mod", (DEPTH, 128, 96), F32)
            self.d_k = self.dram("dbg_k", (2, 128, NT), BF16)
            self.d_v = self.dram("dbg_v", (128, 34 * 2 * 66), BF16)

        with contextlib.ExitStack() as es:
            self.S = Sched(nc, es)
            self.cst, self.b_cst = self.sb(es, "cst", [128, 6, 128], F32)
            self.vec, self.b_vec = self.sb(es, "vec", [128, NV], F32)
            self.mod, self.b_mod = self.sb(es, "mod", [128, 48, 2], F32)
            self.modp, self.b_modp = self.sb(es, "modp", [128, 16, 2], F32)
            self.lsc, self.b_lsc = self.sb(es, "lsc", [128, 2, 8], F32)
            self.nbias, self.b_nbias = self.sb(es, "nbias", [128, 1], F32)
            self.nbx, self.b_nbx = self.sb(es, "nbx", [128, 16], F32)
            self.sel, self.b_sel = self.sb(es, "sel", [8, 8, 128], F32)
            self.dma("sp", self.cst[:], self.i_consts.rearrange("c p n -> p c n"), (), [self.b_cst])
            self.dma("sp", self.sel[:], self.i_sel8, (), [self.b_sel])
            self.S.flush()
            for l in self.layers:
                last = l == DEPTH - 1
                if last and SPARSE_MOE:
                    tail = [("merge", self.phase_merge), ("route", self.phase_route), ("blk", self.phase_blocks),
                            ("ffn", self.phase_comb)]
                else:
                    tail = [("merge", self.phase_merge), ("ffn", self.phase_ffn)]
                groups = [[("mod", self.phase_mod)], [("h", self.phase_h)],
                          [("mix", self.phase_mix), ("attn", self.phase_attn)], tail]
                for gi, grp in enumerate(groups):
                    with contextlib.ExitStack() as ges:
                        if gi == 2:
                            self.kd, self.b_kd = self.sbn(ges, "kd", [128, NT], BF16, 4)
                            self.va, self.b_va = self.sb(ges, "va", [128, 34, 2, 66], BF16)
                        if gi == 3 and last:
                            self.P12, self.b_P12 = self.sb(ges, "P12", [128, 2, 32], F32)
                            self.D12, self.b_D12 = self.sb(ges, "D12", [128, 2, 32], I32)
                            self.IG, self.b_IG = self.sb(ges, "IG", [128, NBLK, 16], I32)
                        for name, fn in grp:
                            with contextlib.ExitStack() as pes:
                                fn(pes, l, last)
                                with nc.named_scope(f"L{l}_{name}"):
                                    self.S.flush()
                            if self.stop_after == (l, name):
                                return nc
        return nc

    def ident(self):
        return self.cst[:, 0, :]

    def onesD(self):
        return self.cst[:, 1, :]

    def blk64(self):
        return self.cst[:, 2, :]

    def perm(self):
        return self.cst[:, 3, :]

    def ones(self):
        return self.cst[:, 4, :]

    def xsrc(self, l, s, n):
        if l == 0:
            if s < CTX:
                return self.i_ctxT.rearrange("(k p) n -> p k n", p=128)[:, :, s:s + n]
            return self.i_xT.rearrange("(k p) n -> p k n", p=128)[:, :, s - CTX:s - CTX + n]
        return self.d_xs1[:, :, s:s + n]

    def phase_mod(self, es, l, last):
        vec, bv = self.vec, self.b_vec
        self.dma("sp", vec[:], self.i_vecs[l], (), [bv])
        cv, bcv = self.sb(es, "cv", [128, 8, 2], F32)
        sc, bsc = self.sb(es, "sc", [128, 8, 2], F32)
        self.dma("sp", cv[:], self.i_cvec, (), [bcv])
        self.act(sc[:], cv[:], AF.Silu, [bcv], [bsc])
        wm, bwm = self.sbn(es, "wm", [128, 8, 768], F32, 2)
        psm = es.enter_context(self.nc.psum_tensor(self.un("psm"), [128, 96], F32))
        bpsm = Buf("psm")
        wsrc = self.i_wmod[l].rearrange("(k p) n -> p k n", p=128)
        for g in range(8):
            sl = g % 2
            self.dma("sp", wm[sl][:], wsrc[:, :, g * 768:(g + 1) * 768], (), [bwm[sl]])
            for jj in range(6):
                j = g * 6 + jj
                for k in range(8):
                    self.mm(psm[:, 2 * j:2 * j + 2], wm[sl][:, k, jj * 128:(jj + 1) * 128], sc[:, k, :],
                            k == 0, k == 7, [bwm[sl], bsc], [bpsm])
        psv = psm[:].rearrange("p (j w) -> p j w", w=2)
        for w in range(2):
            self.tt("dve", self.mod[:, :, w], psv[:, :, w], vec[:, V_BMOD:V_BMOD + 48], ALU.add, [bpsm, bv], [self.b_mod])
        self.ts("dve", self.modp[:, 0:8, :], self.mod[:, 8:16, :], 1.0, ALU.add, [self.b_mod], [self.b_modp])
        self.ts("dve", self.modp[:, 8:16, :], self.mod[:, 32:40, :], 1.0, ALU.add, [self.b_mod], [self.b_modp])
        if self.dbg:
            self.dma("sp", self.d_mod[l], self.mod[:].rearrange("p j w -> p (j w)"), [self.b_mod], [])
        lam = vec[:, V_LAM:V_LAM + 8]
        t = {}
        for nm in ["ab", "e", "y", "y2", "p", "r", "sp"]:
            t[nm], _ = self.sb(es, "sp_" + nm, [128, 8], F32)
        bt = Buf("sptmp")
        self.ts("dve", t["r"][:], lam, -1.0, ALU.mult, [bv], [bt])
        self.tt("dve", t["ab"][:], t["r"][:], lam, ALU.max, [bv, bt], [bt])
        self.act(t["e"][:], t["ab"][:], AF.Exp, [bt], [bt], scale=-1.0)
        self.ts("dve", t["y"][:], t["e"][:], 2.0, ALU.add, [bt], [bt])
        self.recip(t["y"][:], t["y"][:], [bt], [bt])
        self.tt("dve", t["y"][:], t["y"][:], t["e"][:], ALU.mult, [bt], [bt])
        self.tt("dve", t["y2"][:], t["y"][:], t["y"][:], ALU.mult, [bt], [bt])
        self.ts("dve", t["p"][:], t["y2"][:], 1.0 / 13, ALU.mult, [bt], [bt], s2=1.0 / 11, op1=ALU.add)
        for cf in [1.0 / 9, 1.0 / 7, 1.0 / 5, 1.0 / 3, 1.0]:
            self.tt("dve", t["p"][:], t["p"][:], t["y2"][:], ALU.mult, [bt], [bt])
            self.ts("dve", t["p"][:], t["p"][:], cf, ALU.add, [bt], [bt])
        self.tt("dve", t["p"][:], t["p"][:], t["y"][:], ALU.mult, [bt], [bt])
        self.ts("dve", t["r"][:], lam, -1.0, ALU.mult, [bv, bt], [bt], s2=0.0, op1=ALU.max)
        self.stt(t["sp"][:], t["p"][:], 2.0, t["r"][:], ALU.mult, ALU.add, [bt], [bt])
        lsv = self.lsc[:].rearrange("p a b -> p (a b)")
        self.ts("dve", self.lsc[:, 0, :], t["sp"][:], -8.0, ALU.mult, [bt], [self.b_lsc])
        self.ts("dve", self.lsc[:, 1, :], t["sp"][:], -16.0, ALU.mult, [bt], [self.b_lsc])
        self.ts("dve", self.nbx[:], vec[:, V_BA:V_BA + 16], -1.0, ALU.mult, [bv], [self.b_nbx])
        m2, _ = self.sb(es, "m2", [128, 2], F32)
        self.tt("dve", m2[:], vec[:, V_QN:V_QN + 2], vec[:, V_QN:V_QN + 2], ALU.mult, [bv], [bt])
        mx, _ = self.sb(es, "mx", [128, 2], F32)
        pst = es.enter_context(self.nc.psum_tensor(self.un("pst"), [128, 128], F32))
        bpst = Buf("pst")
        self.tr(pst[0:2, :], m2[:], self.ident(), [bt, self.b_cst], [bpst])
        r2, _ = self.sb(es, "r2", [2, 1], F32)
        self.rmax(r2[:], pst[0:2, :], [bpst], [bt])
        l2, _ = self.sb(es, "l2", [2, 1], F32)
        self.act(l2[:], r2[:], AF.Ln, [bt], [bt])
        psb = es.enter_context(self.nc.psum_tensor(self.un("psb"), [128, 2], F32))
        bpsb = Buf("psb")
        self.mm(psb[:, 0:1], self.ones()[0:2, :], l2[:], True, True, [bt, self.b_cst], [bpsb])
        self.act(mx[:, 0:1], psb[:, 0:1], AF.Exp, [bpsb], [bt], scale=0.5)
        self.ts("dve", self.nbias[:], mx[:, 0:1], -8.0, ALU.mult, [bt], [self.b_nbias])

    def phase_h(self, es, l, last):
        xt, bxt = self.sbn(es, "xt", [128, 8, 512], F32, 2)
        ht, bht = self.sbn(es, "ht", [128, 8, 512], BF16, 2)
        for ti, (s, n) in enumerate(TILES):
            w = 1 if s < CTX else 0
            sl = ti % 2
            self.dma("sp", xt[sl][:, :, 0:n], self.xsrc(l, s, n), (), [bxt[sl]])
            for k in range(8):
                if k % 2 == 0:
                    self.act(ht[sl][:, k, 0:n], xt[sl][:, k, 0:n], AF.Identity, [bxt[sl], self.b_mod, self.b_modp], [bht[sl]],
                             scale=self.modp[:, k, w:w + 1], bias=self.mod[:, k, w:w + 1])
                else:
                    self.ts("dve", ht[sl][:, k, 0:n], xt[sl][:, k, 0:n], self.modp[:, k, w:w + 1], ALU.mult,
                            [bxt[sl], self.b_mod, self.b_modp], [bht[sl]], s2=self.mod[:, k, w:w + 1], op1=ALU.add)
            self.dma("st", self.d_h[:, :, s:s + n], ht[sl][:, :, 0:n], [bht[sl]], [])

    def load_h(self, ti):
        s, n = TILES[ti]
        sl = self.nxt("hb", 3)
        self.dma("sp", self.hb[sl][:, :, 0:n], self.d_h[:, :, s:s + n], (), [self.b_hb[sl]])
        return self.hb[sl], self.b_hb[sl]

    def load_wz(self, l, cols):
        sl = self.nxt("wz", 4)
        wsrc = self.i_win[l].rearrange("(k p) n -> p k n", p=128)
        for (do, sc_, nn) in cols:
            self.dma("pool", self.wz[sl][:, :, do:do + nn], wsrc[:, :, sc_:sc_ + nn], (), [self.b_wz[sl]])
        return self.wz[sl], self.b_wz[sl]

    def zmm(self, ps, bps, wz, bwz, hb, bhb, n):
        for k in range(8):
            self.mm(ps[:, 0:n], wz[:, k, :], hb[:, k, 0:n], k == 0, k == 7, [bwz, bhb], [bps])

    def phase_mix(self, es, l, last):
        nc = self.nc
        vec, bv = self.vec, self.b_vec
        self.hb, self.b_hb = self.sbn(es, "hb", [128, 8, 512], BF16, 3)
        self.wz, self.b_wz = self.sbn(es, "wz", [128, 8, 128], BF16, 4)
        pz, bpz = self.psn(es, "pz", 3)
        pa, bpa = self.psn(es, "pa", 3)
        tA, btA = self.sbn(es, "tA", [128, 544], F32, 2)
        tB, btB = self.sbn(es, "tB", [128, 544], F32, 2)
        tC, btC = self.sbn(es, "tC", [128, 512], F32, 2)
        tD, btD = self.sbn(es, "tD", [128, 512], F32, 2)
        tE, btE = self.sbn(es, "tE", [128, 512], F32, 2)
        tF, btF = self.sbn(es, "tF", [128, 512], F32, 2)
        ob, bob = self.sbn(es, "ob", [128, 512], BF16, 3)
        db, bdb = self.sbn(es, "db", [128, 512], BF16, 2)
        ic, bic = self.sbn(es, "ic", [128, 512], F32, 2)
        pw, bpw = self.sb(es, "pw", [128, 4, 128], BF16)
        bd, bbd = self.sbn(es, "bd", [128, 4, 128], BF16, 2)
        fes = contextlib.ExitStack()
        zp, bzp = self.sb(fes, "zp", [128, PADW], F32)
        xc, bxc = self.sb(fes, "xc", [128, NT], F32)
        xcb, bxcb = self.sb(fes, "xcb", [128, NT], BF16)
        hsum, bhs = self.sb(fes, "hsum", [128, NT], F32)
        gl, bgl = self.sb(fes, "gl", [128, NT], F32)
        self.memset("pool", zp[:], 0.0, [bzp])
        self.memset("pool", self.va[:, :, :, 64:66], 1.0, [self.b_va])
        for i_ in range(4):
            self.memset("pool", self.kd[i_][:], 0.0, [self.b_kd[i_]])
        self.dma("pool", pw[:], self.i_poolw[l].rearrange("g c d -> c g d"), (), [bpw])

        for g in range(4):
            wz, bwz = self.load_wz(l, [(0, g * 128, 128)])
            for ti, (s, n) in enumerate(TILES):
                hb, bhb = self.load_h(ti)
                p = self.nxt("pz", 3)
                self.zmm(pz[p], bpz[p], wz, bwz, hb, bhb, n)
                self.copy("act", zp[:, pad_pos(s):pad_pos(s) + n], pz[p][:, 0:n], [bpz[p]], [bzp])
            m = g + 1
            for ti, (s, n) in enumerate(TILES):
                if last and s < CTX:
                    continue
                p0 = pad_pos(s)
                a = p0 - (1 << (m - 1))
                lens = [n]
                for i in range(m, 0, -1):
                    lens.append(lens[-1] + (1 << (i - 1)))
                lens = lens[::-1]
                sl = ti % 2
                src, bsrc = zp[:, a:a + lens[0]], bzp
                cur = None
                for i in range(1, m + 1):
                    dst, bdst = (tA[sl], btA[sl]) if i % 2 == 1 else (tB[sl], btB[sl])
                    sh = 1 << (i - 1)
                    if i == 1:
                        in0, in1 = zp[:, a:a + lens[1]], zp[:, a + sh:a + sh + lens[1]]
                    else:
                        in0, in1 = cur[:, 0:lens[i]], cur[:, sh:sh + lens[i]]
                    self.tt("dve" if i % 2 else "pool", dst[:, 0:lens[i]], in0, in1, ALU.add, [bsrc], [bdst])
                    cur, bsrc = dst, bdst
                self.dma("sp", ic[sl][:, 0:n], pbcast(self.i_invcnt[g:g + 1, p0:p0 + n]), (), [bic[sl]])
                self.tt("pool", tC[sl][:, 0:n], cur[:, 0:n], ic[sl][:, 0:n], ALU.mult, [bsrc, bic[sl]], [btC[sl]])
                self.tt("dve", db[sl][:, 0:n], tC[sl][:, 0:n], zp[:, p0:p0 + n], ALU.subtract, [btC[sl], bzp], [bdb[sl]])
                p = self.nxt("pa", 3)
                self.mm(pa[p][:, 0:n], pw[:, g, :], db[sl][:, 0:n], True, True, [bpw, bdb[sl]], [bpa[p]])
                o = self.nxt("ob", 3)
                self.act(ob[o][:, 0:n], pa[p][:, 0:n], AF.Copy, [bpa[p], bv], [bob[o]], scale=vec[:, V_PSCALE + g:V_PSCALE + g + 1])
                self.dma("st", self.d_y[:, g, s:s + n], ob[o][:, 0:n], [bob[o]], [])

        self.subflush(f"L{l}_mixpool")
        for c in range(4):
            wzx, bwzx = self.load_wz(l, [(0, 1280 + c * 128, 128)])
            wzg, bwzg = self.load_wz(l, [(0, 1792 + c * 128, 128)])
            bsl = c % 2
            self.dma("pool", bd[bsl][:], self.i_lrubd[l, c].rearrange("m i j -> i m j"), (), [bbd[bsl]])
            for ti, (s, n) in enumerate(TILES):
                hb, bhb = self.load_h(ti)
                p = self.nxt("pz", 3)
                self.zmm(pz[p], bpz[p], wzx, bwzx, hb, bhb, n)
                self.copy("act", zp[:, pad_pos(s):pad_pos(s) + n], pz[p][:, 0:n], [bpz[p]], [bzp])
                if not (last and s < CTX):
                    p2 = self.nxt("pz", 3)
                    self.zmm(pz[p2], bpz[p2], wzg, bwzg, hb, bhb, n)
                    sl = ti % 2
                    self.act(tC[sl][:, 0:n], pz[p2][:, 0:n], AF.Square, [bpz[p2]], [btC[sl]])
                    self.ts("pool", tC[sl][:, 0:n], tC[sl][:, 0:n], 0.044715, ALU.mult, [btC[sl]], [btC[sl]], s2=1.0, op1=ALU.add)
                    self.tt("dve", tC[sl][:, 0:n], tC[sl][:, 0:n], pz[p2][:, 0:n], ALU.mult, [btC[sl], bpz[p2]], [btC[sl]])
                    self.act(tC[sl][:, 0:n], tC[sl][:, 0:n], AF.Sigmoid, [btC[sl]], [btC[sl]], scale=1.5957691216)
                    self.tt("dve", gl[:, s:s + n], tC[sl][:, 0:n], pz[p2][:, 0:n], ALU.mult, [btC[sl], bpz[p2]], [bgl])
            cw = lambda k: vec[:, V_CONVW + c * 4 + k:V_CONVW + c * 4 + k + 1]
            for ti, (s, n) in enumerate(TILES):
                p0 = pad_pos(s)
                self.ts("dve", xc[:, s:s + n], zp[:, p0 - 2:p0 - 2 + n], cw(0), ALU.mult, [bzp, bv], [bxc],
                        s2=vec[:, V_CONVB + c:V_CONVB + c + 1], op1=ALU.add)
                for k in range(1, 4):
                    self.stt(xc[:, s:s + n], zp[:, p0 - 2 + k:p0 - 2 + k + n], cw(k), xc[:, s:s + n], ALU.mult, ALU.add,
                             [bzp, bv, bxc], [bxc])
                self.copy("pool", xcb[:, s:s + n], xc[:, s:s + n], [bxc], [bxcb])
            for dr in (1, 0):
                order = [0] + list(range(8, 0, -1)) if dr == 1 else list(range(9))
                prev = None
                for oi, ti in enumerate(order):
                    s, n = TILES[ti]
                    sl = oi % 2
                    pr = self.nxt("pa", 3)
                    self.mm(pa[pr][:, 0:n], bd[bsl][:, 2 * dr, :], xcb[:, s:s + n], True, True, [bbd[bsl], bxcb], [bpa[pr]])
                    pi = self.nxt("pa", 3)
                    self.mm(pa[pi][:, 0:n], bd[bsl][:, 2 * dr + 1, :], xcb[:, s:s + n], True, True, [bbd[bsl], bxcb], [bpa[pi]])
                    col = dr * 4 + c
                    self.act(tA[sl][:, 0:n], pa[pr][:, 0:n], AF.Sigmoid, [bpa[pr], bv], [btA[sl]],
                             bias=vec[:, V_BA + col:V_BA + col + 1])
                    self.act(tB[sl][:, 0:n], pa[pi][:, 0:n], AF.Sigmoid, [bpa[pi], bv], [btB[sl]],
                             bias=vec[:, V_BX + col:V_BX + col + 1])
                    self.act(tD[sl][:, 0:n], tA[sl][:, 0:n], AF.Exp, [btA[sl], self.b_lsc], [btD[sl]], scale=self.lsc[:, 0, col:col + 1])
                    self.act(tE[sl][:, 0:n], tA[sl][:, 0:n], AF.Exp, [btA[sl], self.b_lsc], [btE[sl]], scale=self.lsc[:, 1, col:col + 1])
                    self.act(tE[sl][:, 0:n], tE[sl][:, 0:n], AF.Sqrt, [btE[sl]], [btE[sl]], scale=-1.0, bias=self.cst[:, 5, 0:1])
                    self.tt("pool", tB[sl][:, 0:n], tB[sl][:, 0:n], tE[sl][:, 0:n], ALU.mult, [btB[sl], btE[sl]], [btB[sl]])
                    self.tt("dve", tB[sl][:, 0:n], tB[sl][:, 0:n], xc[:, s:s + n], ALU.mult, [btB[sl], bxc], [btB[sl]])
                    if dr == 1:
                        if oi == 0:
                            init, rd = 0.0, []
                        elif oi == 1:
                            init, rd = hsum[:, 0:1], [bhs]
                        else:
                            ps_, _ = TILES[order[oi - 1]]
                            init, rd = hsum[:, ps_:ps_ + 1], [bhs]
                        self.scan(rev(hsum[:, s:s + n]), rev(tD[sl][:, 0:n]), rev(tB[sl][:, 0:n]), init,
                                  [btD[sl], btB[sl]] + rd, [bhs])
                    else:
                        if oi == 0:
                            init, rd = 0.0, []
                        else:
                            pn = TILES[order[oi - 1]][1]
                            init, rd = tF[1 - sl][:, pn - 1:pn], [btF[1 - sl]]
                        self.scan(tF[sl][:, 0:n], tD[sl][:, 0:n], tB[sl][:, 0:n], init, [btD[sl], btB[sl]] + rd, [btF[sl]])
                        if not (last and s < CTX):
                            self.tt("dve", tC[sl][:, 0:n], tF[sl][:, 0:n], hsum[:, s:s + n], ALU.add, [btF[sl], bhs], [btC[sl]])
                            o = self.nxt("ob", 3)
                            self.tt("pool", ob[o][:, 0:n], tC[sl][:, 0:n], gl[:, s:s + n], ALU.mult, [btC[sl], bgl], [bob[o]])
                            self.dma("st", self.d_y[:, 8 + c, s:s + n], ob[o][:, 0:n], [bob[o]], [])

        self.subflush(f"L{l}_mixlru")
        fes.close()
        NQ = 4
        qA, bqA = self.sbn(es, "qA", [128, 512], F32, NQ)
        qB, bqB = self.sbn(es, "qB", [128, 512], F32, NQ)
        qC, bqC = self.sbn(es, "qC", [128, 512], F32, NQ)
        qD, bqD = self.sbn(es, "qD", [128, 512], F32, NQ)
        qE, bqE = self.sbn(es, "qE", [128, 512], F32, NQ)
        qF, bqF = self.sbn(es, "qF", [128, 512], F32, NQ)
        cs_, bcs = self.sbn(es, "cs4", [128, 512], F32, NQ)
        sn_, bsn = self.sbn(es, "sn4", [128, 512], F32, NQ)
        tA, btA, tB, btB, tC, btC, tD, btD, tE, btE, tF, btF = qA, bqA, qB, bqB, qC, bqC, qD, bqD, qE, bqE, qF, bqF
        qcnt = 0
        jobs = [("q", c) for c in range(4)] + [("k", kv) for kv in range(2)]
        wq, bwq = self.sbn(es, "wq", [128, 8, 128], BF16, 7)
        wsrc = self.i_win[l].rearrange("(k p) n -> p k n", p=128)
        for ji, (kind, c) in enumerate(jobs):
            if kind == "q":
                self.dma("pool", wq[ji][:], wsrc[:, :, 512 + c * 128:512 + (c + 1) * 128], (), [bwq[ji]])
            else:
                for hh in range(2):
                    self.dma("pool", wq[ji][:, :, 64 * hh:64 * hh + 64], wsrc[:, :, 1024 + c * 64:1024 + (c + 1) * 64], (), [bwq[ji]])
        self.dma("pool", wq[6][:], wsrc[:, :, 1152:1280], (), [bwq[6]])
        pend = []

        def stage2(sl, n, gcol):
            def f():
                p2 = self.nxt("pa", 3)
                self.mm(pa[p2][:, 0:n], self.blk64(), tB[sl][:, 0:n], True, True, [self.b_cst, btB[sl]], [bpa[p2]])
                self.act(tC[sl][:, 0:n], pa[p2][:, 0:n], AF.Sqrt, [bpa[p2]], [btC[sl]], bias=self.cst[:, 5, 1:2])
                self.recip(tC[sl][:, 0:n], tC[sl][:, 0:n], [btC[sl]], [btC[sl]])
                self.stt(tD[sl][:, 0:n], tA[sl][:, 0:n], vec[:, gcol:gcol + 1], tC[sl][:, 0:n], ALU.mult, ALU.mult,
                         [btA[sl], btC[sl], bv], [btD[sl]])
            return f

        def stage3(sl, n, s, kind, c, isctx, cq):
            def f():
                if kind == "q":
                    o = self.nxt("ob", 3)
                    dst, bdst = ob[o][:, 0:n], bob[o]
                if isctx:
                    if kind == "q":
                        self.copy("pool", dst, tD[sl][:, 0:n], [btD[sl]], [bdst])
                    else:
                        for hh in range(2):
                            self.copy("pool", self.kd[2 * c + hh][64 * hh:64 * hh + 64, s:s + n], tD[sl][64 * hh:64 * hh + 64, 0:n],
                                      [btD[sl]], [self.b_kd[2 * c + hh]])
                else:
                    p3 = self.nxt("pa", 3)
                    self.mm(pa[p3][:, 0:n], self.perm(), tD[sl][:, 0:n], True, True, [self.b_cst, btD[sl]], [bpa[p3]])
                    self.tt("pool", tE[sl][:, 0:n], tD[sl][:, 0:n], cs_[cq][:, 0:n], ALU.mult, [btD[sl], bcs[cq]], [btE[sl]])
                    self.tt("dve", tF[sl][:, 0:n], pa[p3][:, 0:n], sn_[cq][:, 0:n], ALU.mult, [bpa[p3], bsn[cq]], [btF[sl]])
                    if kind == "q":
                        self.tt("pool", dst, tE[sl][:, 0:n], tF[sl][:, 0:n], ALU.add, [btE[sl], btF[sl]], [bdst])
                    else:
                        for hh in range(2):
                            self.tt("dve" if hh else "pool", self.kd[2 * c + hh][64 * hh:64 * hh + 64, s:s + n], tE[sl][64 * hh:64 * hh + 64, 0:n],
                                    tF[sl][64 * hh:64 * hh + 64, 0:n], ALU.add, [btE[sl], btF[sl]], [self.b_kd[2 * c + hh]])
                if kind == "q":
                    self.dma("st", self.d_q[c, :, s:s + n], dst, [bdst], [])
            return f

        def advance():
            for ent in pend:
                ent[2] += 1
            for ent in pend:
                if ent[2] == 1:
                    ent[0]()
            while pend and pend[0][2] >= 2:
                pend.pop(0)[1]()

        tcnt = 0
        for ti, (s, n) in enumerate(TILES):
            isctx = s < CTX
            hb, bhb = self.load_h(ti)
            cq = tcnt % NQ
            tcnt += 1
            if not isctx:
                self.dma("sp", cs_[cq][:, 0:n], self.i_cos[:, s - CTX:s - CTX + n], (), [bcs[cq]])
                self.dma("sp", sn_[cq][:, 0:n], self.i_sin[:, s - CTX:s - CTX + n], (), [bsn[cq]])
            for ji, (kind, c) in enumerate(jobs):
                if kind == "q" and last and isctx:
                    continue
                gcol = V_QN if kind == "q" else V_KN
                wz, bwz = wq[ji], bwq[ji]
                sl = qcnt % NQ
                qcnt += 1
                p = self.nxt("pz", 3)
                self.zmm(pz[p], bpz[p], wz, bwz, hb, bhb, n)
                self.copy("act", tA[sl][:, 0:n], pz[p][:, 0:n], [bpz[p]], [btA[sl]])
                self.act(tB[sl][:, 0:n], pz[p][:, 0:n], AF.Square, [bpz[p]], [btB[sl]])
                pend.append([stage2(sl, n, gcol), stage3(sl, n, s, kind, c, isctx, cq), 0])
                advance()
            for sub in range(n // 128):
                tc = s // 128 + sub
                p = self.nxt("pa", 3)
                for k in range(8):
                    self.mm(pa[p][:, 0:128], hb[:, k, sub * 128:(sub + 1) * 128], wq[6][:, k, :], k == 0, k == 7, [bhb, bwq[6]], [bpa[p]])
                self.copy("act" if sub % 2 else "dve", self.va[:, tc, :, 0:64], pa[p][:, 0:128].rearrange("p (a b) -> p a b", a=2),
                          [bpa[p]], [self.b_va])
        advance()
        advance()
        assert not pend
        if self.dbg:
            for kv in range(2):
                for hh in range(2):
                    self.dma("sp", self.d_k[kv, 64 * hh:64 * hh + 64, :], self.kd[2 * kv + hh][64 * hh:64 * hh + 64, :], [self.b_kd[2 * kv + hh]], [])
            self.dma("sp", self.d_v, self.va[:].rearrange("p a b c -> p (a b c)"), [self.b_va], [])

    def phase_attn(self, es, l, last):
        qt, bqt = self.sbn(es, "qt", [128, 512], BF16, 3)
        pt, bpt = self.sbn(es, "pt", [128, 512], BF16, 6)
        pss, bpss = self.psn(es, "pss", 4)
        pso, bpso = self.psn(es, "pso", 2)
        psb, bpsb = self.psn(es, "psb", 1)
        rs, brs = self.sbn(es, "rs", [128, 512], F32, 2)
        bc, bbc = self.sbn(es, "bc", [64, 512], F32, 2)
        oa, boa = self.sbn(es, "oa", [64, 512], BF16, 3)
        LAG = 2
        work = [(c, ti) for c in range(4) for ti in range(len(TILES)) if not (last and TILES[ti][0] < CTX)]
        qslot = {}

        def load_q(idx):
            c, ti = work[idx]
            s, n = TILES[ti]
            qs = self.nxt("qt", 3)
            self.dma("sp", qt[qs][:, 0:n], self.d_q[c, :, s:s + n], (), [bqt[qs]])
            qslot[idx] = qs

        pending = []

        def make_pv(c, kv, s, n, j, kc, nkc, po, pp):
            def pv():
                self.mm(pso[po][0:65, 0:n], self.va[:, kc, kv, 0:65], pt[pp][:, 0:n], kc == 0, kc == nkc - 1,
                        [self.b_va, bpt[pp]], [bpso[po]])
                if kc == nkc - 1:
                    r = self.nxt("rs", 2)
                    self.recip(rs[r][64:65, 0:n], pso[po][64:65, 0:n], [bpso[po]], [brs[r]])
                    self.mm(psb[0][0:64, 0:n], self.ones()[64:65, 0:64], rs[r][64:65, 0:n], True, True,
                            [self.b_cst, brs[r]], [bpsb[0]])
                    self.copy("act", bc[r][:, 0:n], psb[0][0:64, 0:n], [bpsb[0]], [bbc[r]])
                    o = self.nxt("oa", 3)
                    self.tt("dve", oa[o][:, 0:n], pso[po][0:64, 0:n], bc[r][:, 0:n], ALU.mult, [bpso[po], bbc[r]], [boa[o]])
                    self.dma("st", self.d_y[64 * j:64 * j + 64, 4 + c, s:s + n], oa[o][:, 0:n], [boa[o]], [])
            return pv

        load_q(0)
        for idx, (c, ti) in enumerate(work):
            if idx + 1 < len(work):
                load_q(idx + 1)
            kv = c // 2
            s, n = TILES[ti]
            nkc = 2 if s < CTX else 34
            qs = qslot[idx]
            for j in range(2):
                po = self.nxt("pso", 2)
                for kc in range(nkc):
                    p = self.nxt("pss", 4)
                    self.mm(pss[p][:, 0:n], self.kd[2 * kv + j][:, kc * 128:(kc + 1) * 128],
                            qt[qs][:, 0:n], True, True, [self.b_kd[2 * kv + j], bqt[qs]], [bpss[p]])
                    pp = self.nxt("pt", 6)
                    self.act(pt[pp][:, 0:n], pss[p][:, 0:n], AF.Exp, [bpss[p], self.b_nbias], [bpt[pp]],
                             scale=0.125, bias=self.nbias[:, 0:1])
                    pending.append(make_pv(c, kv, s, n, j, kc, nkc, po, pp))
                    if len(pending) > LAG:
                        pending.pop(0)()
        while pending:
            pending.pop(0)()

    def ln_fm_stages(self, r, br, n, gcol, bcol, outs, bouts, psA, bpsA, psB, bpsB, tmp):
        vec, bv = self.vec, self.b_vec
        sq, bsq, mu, bmu, rstd, brstd = tmp
        if not isinstance(br, (list, tuple)):
            br = [br] * 8
        if not isinstance(bouts, (list, tuple)):
            bouts = [bouts] * 8
        st = []

        def s1():
            for k in range(8):
                self.mm(psA[:, 0:n], self.onesD(), r[k], k == 0, k == 7, [self.b_cst, br[k]], [bpsA])
            for k in range(8):
                s = k % 2
                self.act(sq[s][:, 0:n], r[k], AF.Square, [br[k]], [bsq[s]])
                self.mm(psB[:, 0:n], self.onesD(), sq[s][:, 0:n], k == 0, k == 7, [self.b_cst, bsq[s]], [bpsB])
        st.append(s1)

        def s2():
            self.copy("act", mu[:, 0:n], psA[:, 0:n], [bpsA], [bmu])
            self.act(sq[0][:, 0:n], psA[:, 0:n], AF.Square, [bpsA], [bsq[0]])
            self.tt("dve", rstd[:, 0:n], psB[:, 0:n], sq[0][:, 0:n], ALU.subtract, [bpsB, bsq[0]], [brstd])
            self.act(rstd[:, 0:n], rstd[:, 0:n], AF.Sqrt, [brstd], [brstd], bias=self.cst[:, 5, 2:3])
            self.recip(rstd[:, 0:n], rstd[:, 0:n], [brstd], [brstd])
        st.append(s2)

        def mk(k):
            def f():
                s = k % 2
                self.tt("pool", sq[s][:, 0:n], r[k], mu[:, 0:n], ALU.subtract, [br[k], bmu], [bsq[s]])
                self.tt("dve", sq[s][:, 0:n], sq[s][:, 0:n], rstd[:, 0:n], ALU.mult, [bsq[s], brstd], [bsq[s]])
                self.act(outs[k], sq[s][:, 0:n], AF.Identity, [bsq[s], bv], [bouts[k]],
                         scale=vec[:, gcol + k:gcol + k + 1], bias=vec[:, bcol + k:bcol + k + 1])
            return f
        for k in range(8):
            st.append(mk(k))
        return st

    def ln_fm(self, *a):
        for f in self.ln_fm_stages(*a):
            f()

    def phase_merge(self, es, l, last):
        vec, bv = self.vec, self.b_vec
        wbr, bwbr = self.sb(es, "wbr", [128, 12, 1024], BF16)
        wo, bwo = self.sb(es, "wo", [128, 8, 1024], BF16)
        wgz, bwgz = self.sb(es, "wgz", [128, 8, 3072], BF16)
        xt, bxt = self.sb(es, "mxt", [128, 8, 512], F32)
        ht, bht = self.sb(es, "mht", [128, 8, 512], BF16)
        yt, byt = self.sb(es, "myt", [128, 12, 512], BF16)
        mg, bmg = self.sb(es, "mmg", [128, 8, 512], BF16)
        h2b, bh2b = self.sb(es, "h2b", [128, 8, 512], BF16)
        gs, bgs = self.sbn(es, "gs", [128, 512], F32, 3)
        pr, bpr = self.sbn(es, "pr", [128, 512], F32, 3)
        sq, bsq = self.sbn(es, "lsq", [128, 512], F32, 2)
        mu, bmu = self.sb(es, "lmu", [128, 512], F32)
        rstd, brstd = self.sb(es, "lrs", [128, 512], F32)
        tt_, btt = self.sbn(es, "mtt", [128, 512], F32, 2)
        pg, bpg = self.psn(es, "pg", 2)
        pb, bpb = self.psn(es, "pb", 2)
        po, bpo = self.psn(es, "po", 2)
        pl, bpl = self.psn(es, "pl", 2)
        if last:
            gTt, bgTt = self.sbn(es, "gTt", [8, 512], F32, 2)
            h2f, bh2f = self.sbn(es, "h2f", [128, 512], F32, 2)
            rt, brt = self.sb(es, "rt", [128, 8, 8], F32)
            lgT, blgT = self.sb(es, "lgT", [8, 512], F32)
            L, bL = self.sb(es, "L", [128, 4, 8], F32)
            sm, bsm = self.sb(es, "sm", [128, 64], F32)
            E1, bE1 = self.sb(es, "E1", [128, 4, 8], F32)
            E2, bE2 = self.sb(es, "E2", [128, 4, 8], F32)
            L2, bL2 = self.sb(es, "L2", [128, 4, 8], F32)
            self.dma("sp", rt[:], self.i_router.rearrange("(k p) e -> p k e", p=128), (), [brt])
        wsrc = self.i_win[l].rearrange("(k p) n -> p k n", p=128)
        for cc in range(12):
            self.dma("pool", wbr[:, cc, :], self.i_wbr[l, cc * 128:(cc + 1) * 128, :], (), [bwbr])
        for k in range(8):
            self.dma("pool", wo[:, k, :], self.i_wout[l, k * 128:(k + 1) * 128, :], (), [bwo])
        for k in range(8):
            for hh in range(2):
                self.dma("pool", wgz[:, k, hh * 1536:(hh + 1) * 1536], wsrc[:, k, 2304 + hh * 1536:2304 + (hh + 1) * 1536], (), [bwgz])
        nxt_ = 1 if last else 2
        if nxt_ == 2:
            xt2, _ = self.sb(es, "mxt2", [128, 8, 512], F32)
            xts = [xt, xt2]
        else:
            xts = [xt]
        bxtk = [[Buf(f"xt{i}_{k}") for k in range(8)] for i in range(nxt_)]
        pending = []

        def make_post(xt, bxk, s, n, w):
            r = [xt[:, k, 0:n] for k in range(8)]
            st = self.ln_fm_stages(r, bxk, n, V_LN1G, V_LN1B, r, bxk, pl[0], bpl[0], pl[1], bpl[1], (sq, bsq, mu, bmu, rstd, brstd))
            st.append(lambda: self.dma("st", self.d_x1[:, :, s:s + n], xt[:, :, 0:n], bxk, []))
            state = {}

            def h2k(k):
                def f():
                    if last:
                        if k == 0:
                            state["a"] = self.nxt("pl", 2)
                        a = state["a"]
                        hf = self.nxt("h2f", 2)
                        self.ts("dve", h2f[hf][:, 0:n], xt[:, k, 0:n], self.modp[:, 8 + k, w:w + 1], ALU.mult,
                                [bxk[k], self.b_mod, self.b_modp], [bh2f[hf]], s2=self.mod[:, 24 + k, w:w + 1], op1=ALU.add)
                        self.copy("pool", h2b[:, k, 0:n], h2f[hf][:, 0:n], [bh2f[hf]], [bh2b])
                        self.mm(pl[a][0:8, 0:n], rt[:, k, :], h2f[hf][:, 0:n], k == 0, k == 7, [brt, bh2f[hf]], [bpl[a]])
                    else:
                        self.ts("dve" if k % 2 else "pool", h2b[:, k, 0:n], xt[:, k, 0:n], self.modp[:, 8 + k, w:w + 1], ALU.mult,
                                [bxk[k], self.b_mod, self.b_modp], [bh2b], s2=self.mod[:, 24 + k, w:w + 1], op1=ALU.add)
                return f
            for k in range(8):
                st.append(h2k(k))
            st.append(lambda: self.dma("st", self.d_h2[:, :, s:s + n], h2b[:, :, 0:n], [bh2b], []))
            if last:
                def router():
                    a = state["a"]
                    self.copy("act", lgT[:, 0:n], pl[a][0:8, 0:n], [bpl[a]], [blgT])
                    b_ = self.nxt("pl", 2)
                    for sub in range(4):
                        self.tr(pl[b_][:, sub * 8:(sub + 1) * 8], lgT[0:8, sub * 128:(sub + 1) * 128], self.cst[0:8, 0, 0:8],
                                [blgT, self.b_cst], [bpl[b_]])
                    self.tt("dve", L[:], pl[b_][:, 0:32].rearrange("p (a b) -> p a b", a=4),
                            bass.AP(vec[:, V_RB:V_RB + 8].tensor, vec[:, V_RB:V_RB + 8].offset, [list(vec[:, V_RB:V_RB + 8].ap[0]), [0, 4], [1, 8]]),
                            ALU.add, [bpl[b_], bv], [bL])
                    m1, m2_, d_, e_, p1, p2 = (sm[:, 0:4], sm[:, 4:8], sm[:, 8:12], sm[:, 12:16], sm[:, 16:20], sm[:, 20:24])
                    self.rmax(m1, L[:], [bL], [bsm])
                    self.tt("dve", E1[:], L[:], bcast_last(m1, 8), ALU.is_equal, [bL, bsm], [bE1])
                    self.stt(L2[:], E1[:], -1e30, L[:], ALU.mult, ALU.add, [bE1, bL], [bL2])
                    self.rmax(m2_, L2[:], [bL2], [bsm])
                    self.tt("dve", E2[:], L2[:], bcast_last(m2_, 8), ALU.is_equal, [bL2, bsm], [bE2])
                    self.tt("dve", d_, m2_, m1, ALU.subtract, [bsm], [bsm])
                    self.act(e_, d_, AF.Exp, [bsm], [bsm])
                    self.ts("dve", p1, e_, 1.0, ALU.add, [bsm], [bsm])
                    self.recip(p1, p1, [bsm], [bsm])
                    self.tt("dve", p2, e_, p1, ALU.mult, [bsm], [bsm])
                    self.tt("dve", E1[:], E1[:], bcast_last(p1, 8), ALU.mult, [bE1, bsm], [bE1])
                    self.tt("dve", E2[:], E2[:], bcast_last(p2, 8), ALU.mult, [bE2, bsm], [bE2])
                    self.tt("dve", E1[:], E1[:], E2[:], ALU.add, [bE1, bE2], [bE1])
                    c_ = self.nxt("pl", 2)
                    for sub in range(4):
                        self.tr(pl[c_][0:8, sub * 128:(sub + 1) * 128], E1[:, sub, :], self.ident(), [bE1, self.b_cst], [bpl[c_]])
                    gs_ = self.nxt("gTt", 2)
                    self.copy("act", gTt[gs_][:, 0:n], pl[c_][0:8, 0:n], [bpl[c_]], [bgTt[gs_]])
                    self.dma("st", self.d_gate[:, s - CTX:s - CTX + n], gTt[gs_][:, 0:n], [bgTt[gs_]], [])
                st.append(router)
            return st

        tnum = 0
        for ti, (s, n) in enumerate(TILES):
            isctx = s < CTX
            if isctx and last:
                continue
            w = 1 if isctx else 0
            xi = tnum % nxt_
            tnum += 1
            xt, bxk = xts[xi], bxtk[xi]
            self.dma("sp", ht[:, :, 0:n], self.d_h[:, :, s:s + n], (), [bht])
            self.dma("sp", yt[:, :, 0:n], self.d_y[:, :, s:s + n], (), [byt])
            if nxt_ == 2:
                self.dma("sp", xt[:, :, 0:n], self.xsrc(l, s, n), (), bxk)
            for j in range(8):
                prods = []
                for nb in range(3):
                    a = self.nxt("pg", 2)
                    for k in range(8):
                        self.mm(pg[a][:, 0:n], wgz[:, k, nb * 1024 + j * 128:nb * 1024 + (j + 1) * 128], ht[:, k, 0:n], k == 0, k == 7, [bwgz, bht], [bpg[a]])
                    g_ = self.nxt("gs", 3)
                    self.act(gs[g_][:, 0:n], pg[a][:, 0:n], AF.Sigmoid, [bpg[a], bv], [bgs[g_]],
                             bias=vec[:, V_BMERGE + nb * 8 + j:V_BMERGE + nb * 8 + j + 1])
                    b_ = self.nxt("pb", 2)
                    for cc in range(4):
                        self.mm(pb[b_][:, 0:n], wbr[:, nb * 4 + cc, j * 128:(j + 1) * 128], yt[:, nb * 4 + cc, 0:n],
                                cc == 0, cc == 3, [bwbr, byt], [bpb[b_]])
                    p_ = self.nxt("pr", 3)
                    self.tt("dve", pr[p_][:, 0:n], pb[b_][:, 0:n], gs[g_][:, 0:n], ALU.mult, [bpb[b_], bgs[g_]], [bpr[p_]])
                    prods.append((pr[p_], bpr[p_]))
                self.tt("pool", prods[0][0][:, 0:n], prods[0][0][:, 0:n], prods[1][0][:, 0:n], ALU.add,
                        [prods[0][1], prods[1][1]], [prods[0][1]])
                self.tt("dve", mg[:, j, 0:n], prods[0][0][:, 0:n], prods[2][0][:, 0:n], ALU.add, [prods[0][1], prods[2][1]], [bmg])
                npop = -(-len(pending) // (8 - j))
                for _ in range(npop):
                    pending.pop(0)()
            assert not pending
            if nxt_ == 1:
                self.dma("sp", xt[:, :, 0:n], self.xsrc(l, s, n), (), bxk)
            for j2 in range(8):
                a = self.nxt("po", 2)
                for j in range(8):
                    self.mm(po[a][:, 0:n], wo[:, j, j2 * 128:(j2 + 1) * 128], mg[:, j, 0:n], j == 0, j == 7, [bwo, bmg], [bpo[a]])
                t_ = self.nxt("mtt", 2)
                self.act(tt_[t_][:, 0:n], po[a][:, 0:n], AF.Copy, [bpo[a], self.b_mod], [btt[t_]], scale=self.mod[:, 16 + j2, w:w + 1])
                self.stt(xt[:, j2, 0:n], xt[:, j2, 0:n], ALPHA, tt_[t_][:, 0:n], ALU.mult, ALU.add, [bxk[j2], btt[t_]], [bxk[j2]])
            pending.extend(make_post(xt, bxk, s, n, w))
        while pending:
            pending.pop(0)()

    def phase_ffn(self, es, l, last):
        vec, bv = self.vec, self.b_vec
        moe = (l % 2 == 1)
        assert not moe, "dense-evaluated MoE path removed (weights are host-laid-out for the sparse path)"
        nF = (D_EXP if moe else D_FF) // 128
        nE = N_EXP if moe else 1
        if last:
            sts = [(CTX + 1024 * i, 1024) for i in range(4)]
        else:
            sts = [(0, 256)] + [(CTX + 1024 * i, 1024) for i in range(4)]
        if moe:
            self.gT, self.b_gT = self.sb(es, "gT3", [8, SEQ], F32)
            self.dma("sp", self.gT[:], self.d_gate, (), [self.b_gT])
        h2, bh2 = self.sb(es, "fh2", [128, 8, 1024], BF16)
        A, bA = self.sb(es, "fA", [128, nF, 1024], BF16)
        acc, bacc = self.sb(es, "facc", [128, 8, 1024], F32)
        wgu, bwgu = self.sbn(es, "wgu", [128, 8, 2, 512], BF16, 2)
        wd, bwd = self.sbn(es, "wd", [128, nF, 128], BF16, 2)
        sg, bsg = self.sbn(es, "sg", [128, 512], F32, 2)
        gb, bgb = self.sbn(es, "gb", [128, 1024], F32, 2)
        x1, bx1 = self.sbn(es, "fx1", [128, 1024], F32, 2)
        tmp, btmp = self.sbn(es, "ftmp", [128, 512], F32, 2)
        sq, bsq = self.sbn(es, "fsq", [128, 512], F32, 2)
        mu, bmu = self.sb(es, "fmu", [128, 512], F32)
        rstd, brstd = self.sb(es, "frs", [128, 512], F32)
        pG, bpG = self.psn(es, "pG", 2)
        pU, bpU = self.psn(es, "pU", 2)
        pY, bpY = self.psn(es, "pY", 2)
        pL, bpL = self.psn(es, "pL", 2)
        for (s, N) in sts:
            isctx = s < CTX
            w = 1 if isctx else 0
            subs = [(o, min(512, N - o)) for o in range(0, N, 512)]
            self.dma("sp", h2[:, :, 0:N], self.d_h2[:, :, s:s + N], (), [bh2])
            for e in range(nE):
                if moe:
                    Wg, Wu, Wd = self.i_mg[e], self.i_mu[e], self.i_md[e]
                else:
                    Wg, Wu, Wd = self.i_fg, self.i_fu, self.i_fd
                Wg = Wg.rearrange("(k p) f -> p k f", p=128)
                Wu = Wu.rearrange("(k p) f -> p k f", p=128)
                Wd = Wd.rearrange("(g p) n -> p g n", p=128)
                if moe:
                    g_ = self.nxt("gb", 2)
                    for (o, n) in subs:
                        a = self.nxt("pL", 2)
                        self.mm(pL[a][:, 0:n], self.sel[0:8, e, :], self.gT[0:8, s - CTX + o:s - CTX + o + n], True, True,
                                [self.b_sel, self.b_gT], [bpL[a]])
                        self.copy("act", gb[g_][:, o:o + n], pL[a][:, 0:n], [bpL[a]], [bgb[g_]])
                for g0 in range(0, nF, 4):
                    ng = min(4, nF - g0)
                    ws = self.nxt("wgu", 2)
                    self.dma("pool", wgu[ws][:, :, 0, 0:ng * 128], Wg[:, :, g0 * 128:(g0 + ng) * 128], (), [bwgu[ws]])
                    self.dma("pool", wgu[ws][:, :, 1, 0:ng * 128], Wu[:, :, g0 * 128:(g0 + ng) * 128], (), [bwgu[ws]])
                    for fi in range(ng):
                        fg = g0 + fi
                        for (o, n) in subs:
                            a = self.nxt("pG", 2)
                            for k in range(8):
                                self.mm(pG[a][:, 0:n], wgu[ws][:, k, 0, fi * 128:(fi + 1) * 128], h2[:, k, o:o + n], k == 0, k == 7, [bwgu[ws], bh2], [bpG[a]])
                            b_ = self.nxt("pU", 2)
                            for k in range(8):
                                self.mm(pU[b_][:, 0:n], wgu[ws][:, k, 1, fi * 128:(fi + 1) * 128], h2[:, k, o:o + n], k == 0, k == 7, [bwgu[ws], bh2], [bpU[b_]])
                            s_ = self.nxt("sg", 2)
                            self.act(sg[s_][:, 0:n], pG[a][:, 0:n], AF.Silu, [bpG[a]], [bsg[s_]])
                            self.tt("dve", A[:, fg, o:o + n], pU[b_][:, 0:n], sg[s_][:, 0:n], ALU.mult, [bpU[b_], bsg[s_]], [bA])
                for j2 in range(8):
                    ds = self.nxt("wd", 2)
                    self.dma("pool", wd[ds][:], Wd[:, :, j2 * 128:(j2 + 1) * 128], (), [bwd[ds]])
                    for (o, n) in subs:
                        a = self.nxt("pY", 2)
                        for fg in range(nF):
                            self.mm(pY[a][:, 0:n], wd[ds][:, fg, :], A[:, fg, o:o + n], fg == 0, fg == nF - 1, [bwd[ds], bA], [bpY[a]])
                        if not moe:
                            self.copy("act", acc[:, j2, o:o + n], pY[a][:, 0:n], [bpY[a]], [bacc])
                        elif e == 0:
                            self.tt("dve", acc[:, j2, o:o + n], pY[a][:, 0:n], gb[g_][:, o:o + n], ALU.mult, [bpY[a], bgb[g_]], [bacc])
                        else:
                            t_ = self.nxt("ftmp", 2)
                            self.tt("dve", tmp[t_][:, 0:n], pY[a][:, 0:n], gb[g_][:, o:o + n], ALU.mult, [bpY[a], bgb[g_]], [btmp[t_]])
                            self.tt("pool", acc[:, j2, o:o + n], acc[:, j2, o:o + n], tmp[t_][:, 0:n], ALU.add, [bacc, btmp[t_]], [bacc])
            self.ffn_epilogue(s, N, subs, w, last, acc, bacc, x1, bx1, pL, bpL, (sq, bsq, mu, bmu, rstd, brstd))

    def ffn_epilogue(self, s, N, subs, w, last, acc, bacc, x1, bx1, pL, bpL, lnt):
        for j2 in range(8):
            xs_ = self.nxt("fx1", 2)
            self.dma("sp", x1[xs_][:, 0:N], self.d_x1[:, j2, s:s + N], (), [bx1[xs_]])
            self.ts("dve", acc[:, j2, 0:N], acc[:, j2, 0:N], self.mod[:, 40 + j2, w:w + 1], ALU.mult, [bacc, self.b_mod], [bacc])
            self.stt(acc[:, j2, 0:N], x1[xs_][:, 0:N], ALPHA, acc[:, j2, 0:N], ALU.mult, ALU.add, [bx1[xs_], bacc], [bacc])
        for (o, n) in subs:
            r = [acc[:, k, o:o + n] for k in range(8)]
            self.ln_fm(r, bacc, n, V_LN2G, V_LN2B, r, bacc, pL[0], bpL[0], pL[1], bpL[1], lnt)
        if last:
            self.dma("st", self.d_out[:, :, s - CTX:s - CTX + N], acc[:, :, 0:N], [bacc], [])
        else:
            self.dma("st", self.d_xs1[:, :, s:s + N], acc[:, :, 0:N], [bacc], [])

    def phase_route(self, es, l, last):
        gT, bgT = self.sb(es, "gT2", [8, SEQ], F32)
        self.dma("sp", gT[:], self.d_gate, (), [bgT])
        ones8, bo8 = self.sb(es, "ones8", [8, SEQ], F32)
        selT, bsel = self.sb(es, "selT", [8, SEQ], F32)
        incl, binc = self.sb(es, "incl", [8, SEQ], F32)
        dst, bdst = self.sb(es, "dstT", [8, SEQ], F32)
        mc, bmc = self.sb(es, "mc", [8, 64], F32)
        mc2, bmc2 = self.sb(es, "mc2", [128, 32], F32)
        sm, bsm = self.sb(es, "rsm", [8, 64], F32)
        ebf, bebf = self.sb(es, "ebf", [128, NBLK], F32)
        tf, btf = self.sb(es, "rtf", [128, NBLK * 28], F32)
        GT, bGT = self.sb(es, "GTk", [128, 32, 8], F32)
        DT, bDT = self.sb(es, "DTk", [128, 32, 8], F32)
        E1, bE1 = self.sb(es, "rE1", [128, 32, 8], F32)
        E2, bE2 = self.sb(es, "rE2", [128, 32, 8], F32)
        TM, bTM = self.sb(es, "rTM", [128, 32, 8], F32)
        r32, br32 = self.sb(es, "r32", [128, 4, 32], F32)
        ps, bps = self.psn(es, "rps", 3)
        self.dma("sp", mc[:], self.i_mc, (), [bmc])
        self.dma("sp", mc2[:], self.i_mc2, (), [bmc2])
        self.memset("dve", ones8[:], 1.0, [bo8])
        self.ts("dve", selT[:], gT[:], 0.0, ALU.is_gt, [bgT], [bsel])
        self.scan(incl[:], ones8[:], selT[:], 0.0, [bo8, bsel], [binc])
        cnt = incl[:, SEQ - 1:SEQ]
        self.ts("dve", sm[:, 0:8], mc[:, 0:8], cnt, ALU.is_lt, [bmc, binc], [bsm])
        self.S.add("dve", lambda e: e.tensor_reduce(out=sm[:, 8:9], in_=sm[:, 0:8], op=ALU.add, axis=AX.X), [bsm], [bsm])
        self.copy("dve", sm[:, 9:10], sm[:, 8:9], [bsm], [bsm])
        self.mm(ps[0][0:8, 0:2], mc[:, 32:40], sm[:, 8:10], True, True, [bmc, bsm], [bps[0]])
        self.ts("dve", sm[:, 10:11], ps[0][0:8, 0:1], float(MB), ALU.mult, [bps[0]], [bsm])
        self.stt(sm[:, 11:12], sm[:, 8:9], float(MB), sm[:, 10:11], ALU.mult, ALU.add, [bsm], [bsm])
        self.tt("dve", dst[:], incl[:], selT[:], ALU.subtract, [binc, bsel], [bdst])
        self.ts("dve", dst[:], dst[:], sm[:, 10:11], ALU.add, [bdst, bsm], [bdst])
        self.ts("dve", sm[:, 16:16 + NBLK], mc[:, 8:8 + NBLK], sm[:, 11:12], ALU.is_ge, [bmc, bsm], [bsm])
        self.mm(ps[1][:, 0:NBLK], self.ones()[0:8, :], sm[:, 16:16 + NBLK], True, True, [self.b_cst, bsm], [bps[1]])
        self.ts("dve", ebf[:], ps[1][:, 0:NBLK], 7.0, ALU.min, [bps[1]], [bebf])
        self.ts("dve", ebf[:], ebf[:], 2048.0, ALU.mult, [bebf, bmc2], [bebf], s2=mc2[:, 0:1], op1=ALU.add)
        m2ap = mc2[:, 2:18]
        self.tt("dve", tf[:, 0:NBLK * 16].rearrange("p (b f) -> p b f", f=16), bcast_last(ebf[:], 16),
                bass.AP(m2ap.tensor, m2ap.offset, [list(m2ap.ap[0]), [0, NBLK], [1, 16]]), ALU.add, [bebf, bmc2], [btf])
        self.copy("dve", self.IG[:].rearrange("p b f -> p (b f)"), tf[:, 0:NBLK * 16], [btf], [self.b_IG])
        for c in range(32):
            self.tr(ps[2][:, c * 8:(c + 1) * 8], gT[0:8, c * 128:(c + 1) * 128], self.cst[0:8, 0, 0:8], [bgT, self.b_cst], [bps[2]])
        self.copy("act", GT[:].rearrange("p a b -> p (a b)"), ps[2][:, 0:256], [bps[2]], [bGT])
        for c in range(32):
            self.tr(ps[0][:, c * 8:(c + 1) * 8], dst[0:8, c * 128:(c + 1) * 128], self.cst[0:8, 0, 0:8], [bdst, self.b_cst], [bps[0]])
        self.copy("act", DT[:].rearrange("p a b -> p (a b)"), ps[0][:, 0:256], [bps[0]], [bDT])
        m1 = r32[:, 0, :]
        self.rmax(m1, GT[:], [bGT], [br32])
        self.tt("dve", E1[:], GT[:], bcast_last(m1, 8), ALU.is_equal, [bGT, br32], [bE1])
        self.ts("dve", E2[:], GT[:], 0.0, ALU.is_gt, [bGT], [bE2])
        self.tt("dve", E2[:], E2[:], E1[:], ALU.subtract, [bE2, bE1], [bE2])
        rsum = lambda out, in_, rd: self.S.add("dve", lambda e: e.tensor_reduce(out=out, in_=in_, op=ALU.add, axis=AX.X), rd, [br32])
        self.copy("dve", self.P12[:, 0, :], m1, [br32], [self.b_P12])
        self.tt("dve", TM[:], GT[:], E2[:], ALU.mult, [bGT, bE2], [bTM])
        rsum(r32[:, 1, :], TM[:], [bTM])
        self.copy("dve", self.P12[:, 1, :], r32[:, 1, :], [br32], [self.b_P12])
        self.tt("dve", TM[:], DT[:], E1[:], ALU.mult, [bDT, bE1], [bTM])
        rsum(r32[:, 2, :], TM[:], [bTM])
        self.copy("dve", self.D12[:, 0, :], r32[:, 2, :], [br32], [self.b_D12])
        self.tt("dve", TM[:], DT[:], E2[:], ALU.mult, [bDT, bE2], [bTM])
        rsum(r32[:, 3, :], TM[:], [bTM])
        self.copy("dve", self.D12[:, 1, :], r32[:, 3, :], [br32], [self.b_D12])
        if self.dbg:
            dd = self.nc.dram_tensor("dbg_D12", [128, 64], I32, kind="ExternalOutput").ap()
            dg = self.nc.dram_tensor("dbg_IG", [128, 16 * NBLK], I32, kind="ExternalOutput").ap()
            dp = self.nc.dram_tensor("dbg_P12", [128, 64], F32, kind="ExternalOutput").ap()
            ds = self.nc.dram_tensor("dbg_sm", [8, 64], F32, kind="ExternalOutput").ap()
            self.dma("sp", dd, self.D12[:].rearrange("p a b -> p (a b)"), [self.b_D12], [])
            self.dma("sp", dg, self.IG[:].rearrange("p a b -> p (a b)"), [self.b_IG], [])
            self.dma("sp", dp, self.P12[:].rearrange("p a b -> p (a b)"), [self.b_P12], [])
            self.dma("sp", ds, sm[:], [bsm], [])

    def phase_blocks(self, es, l, last):
        nF = D_EXP // 128
        self.st_dve_q = "sp"
        idb, bidb = self.sb(es, "idb", [128, 128], BF16)
        rows, brows = self.sbn(es, "rows", [128, 1024], BF16, 2)
        XT, bXT = self.sbn(es, "XT", [128, 8, MB], BF16, 2)
        A, bA = self.sb(es, "bA", [128, nF, MB], BF16)
        wg, bwg = self.sbn(es, "bwg", [128, 8 * 896], BF16, 2)
        wu, bwu = self.sbn(es, "bwu", [128, 8 * 896], BF16, 2)
        wd, bwd = self.sbn(es, "bwd", [128, nF * 256], BF16, 2)
        Yr, bYr = self.sbn(es, "Yr", [128, 4, 1024], F32, 1)
        h4, bh4 = XT, bXT
        sg, bsg = self.sbn(es, "bsg", [128, MB], F32, 2)
        ptb, bptb = self.psn(es, "ptb", 2, shape=(128, 1024), dt=BF16)
        pG, bpG = self.psn(es, "bpG", 2)
        pU, bpU = self.psn(es, "bpU", 2)
        pY, bpY = self.psn(es, "bpY", 2)
        self.copy("dve", idb[:], self.ident(), [self.b_cst], [bidb])
        bxs = [Buf(f"xs{c}") for c in range(32)]

        def gather(dst_ap, src, idx, reads, writes):
            return self.S.add("pool", lambda e: e.indirect_dma_start(out=dst_ap, out_offset=None, in_=src,
                                                                     in_offset=IndirectOffsetOnAxis(ap=idx, axis=0)),
                              reads, writes, dma=True)

        wslot = {}

        def load_w1(b, cg):
            ws = self.nxt("bwg", 2)
            for kp in range(4):
                idx = self.IG[:, b, cg * 4 + kp:cg * 4 + kp + 1].bitcast(U32)
                gather(wg[ws][:, kp * 1792:(kp + 1) * 1792], self.i_mg, idx, [self.b_IG], [bwg[ws]])
                gather(wu[ws][:, kp * 1792:(kp + 1) * 1792], self.i_mu, idx, [self.b_IG], [bwu[ws]])
            wslot[(b, cg)] = ws

        load_w1(0, 0)
        load_w1(0, 1)
        for g4 in range(8):
            hs = g4 % 2
            self.dma("sp", h4[hs][:], self.d_h2[:, :, CTX + g4 * 512:CTX + (g4 + 1) * 512], (), [bh4[hs]])
            for c4 in range(4):
                c = g4 * 4 + c4
                pp = self.nxt("ptb", 2)
                for k in range(8):
                    self.tr(ptb[pp][:, k * 128:(k + 1) * 128], h4[hs][:, k, c4 * 128:(c4 + 1) * 128], idb[:], [bh4[hs], bidb], [bptb[pp]])
                rs_ = self.nxt("rows", 2)
                self.copy("act" if c % 2 else "dve", rows[rs_][:], ptb[pp][:], [bptb[pp]], [brows[rs_]])
                for t2 in range(2):
                    idx = self.D12[:, t2, c:c + 1].bitcast(U32)
                    self.S.add("pool", lambda e, idx=idx, src=rows[rs_]: e.indirect_dma_start(
                        out=self.d_xs, out_offset=IndirectOffsetOnAxis(ap=idx, axis=0), in_=src[:], in_offset=None),
                        [brows[rs_], self.b_D12], [bxs[c]], dma=True)
        bys = Buf("ys")
        for b in range(NBLK_RUN if BLK_STAGE > 0 else 0):
            xs_ = b % 2
            for c4 in range(4):
                rs_ = self.nxt("rows", 2)
                self.dma("sp", rows[rs_][:], self.d_xs[b * MB + c4 * 128:b * MB + (c4 + 1) * 128, :], bxs, [brows[rs_]])
                pp = self.nxt("ptb", 2)
                for k in range(8):
                    self.tr(ptb[pp][:, k * 128:(k + 1) * 128], rows[rs_][:, k * 128:(k + 1) * 128], idb[:], [brows[rs_], bidb], [bptb[pp]])
                self.copy("act" if c4 % 2 else "dve", XT[xs_][:, :, c4 * 128:(c4 + 1) * 128],
                          ptb[pp][:].rearrange("p (k t) -> p k t", k=8), [bptb[pp]], [bXT[xs_]])
            for cg in range(4 if BLK_STAGE > 1 else 0):
                if (b, cg) not in wslot:
                    load_w1(b, cg)
                ws = wslot[(b, cg)]
                for f7 in range(7):
                    fg = cg * 7 + f7
                    a = self.nxt("bpG", 2)
                    for k in range(8):
                        self.mm(pG[a][:, 0:MB], wg[ws][:, k * 896 + f7 * 128:k * 896 + (f7 + 1) * 128], XT[xs_][:, k, :], k == 0, k == 7, [bwg[ws], bXT[xs_]], [bpG[a]])
                    b_ = self.nxt("bpU", 2)
                    for k in range(8):
                        self.mm(pU[b_][:, 0:MB], wu[ws][:, k * 896 + f7 * 128:k * 896 + (f7 + 1) * 128], XT[xs_][:, k, :], k == 0, k == 7, [bwu[ws], bXT[xs_]], [bpU[b_]])
                    s_ = self.nxt("bsg", 2)
                    self.act(sg[s_][:], pG[a][:, 0:MB], AF.Silu, [bpG[a]], [bsg[s_]])
                    self.tt("dve", A[:, fg, :], pU[b_][:, 0:MB], sg[s_][:], ALU.mult, [bpU[b_], bsg[s_]], [bA])
            ys_ = 0
            for dq in range(4 if BLK_STAGE > 2 else 0):
                ds_ = self.nxt("bwd", 2)
                for fq in range(4):
                    idx = self.IG[:, b, dq * 4 + fq:dq * 4 + fq + 1].bitcast(U32)
                    gather(wd[ds_][:, fq * 1792:(fq + 1) * 1792], self.i_md, idx, [self.b_IG], [bwd[ds_]])
                for c4 in range(4):
                    a = self.nxt("bpY", 2)
                    for fg in range(nF):
                        self.mm(pY[a][:, 0:256], A[:, fg, c4 * 128:(c4 + 1) * 128], wd[ds_][:, fg * 256:(fg + 1) * 256], fg == 0, fg == nF - 1, [bA, bwd[ds_]], [bpY[a]])
                    self.copy("act" if c4 % 2 else "dve", Yr[ys_][:, c4, dq * 256:(dq + 1) * 256], pY[a][:, 0:256], [bpY[a]], [bYr[ys_]])
            self.dma("st", self.d_ys[b * MB:(b + 1) * MB, :].rearrange("(c p) d -> p c d", p=128), Yr[ys_][:], [bYr[ys_]], [bys])
        self.b_ys = bys
        self.st_dve_q = "pool"

    def phase_comb(self, es, l, last):
        acc, bacc = self.sb(es, "cacc", [128, 8, 1024], F32)
        Y1, bY1 = self.sbn(es, "cY1", [128, 1024], F32, 2)
        Y2, bY2 = self.sbn(es, "cY2", [128, 1024], F32, 2)
        x1, bx1 = self.sbn(es, "cx1", [128, 1024], F32, 2)
        sq, bsq = self.sbn(es, "csq", [128, 512], F32, 2)
        mu, bmu = self.sb(es, "cmu", [128, 512], F32)
        rstd, brstd = self.sb(es, "crs", [128, 512], F32)
        pT, bpT = self.psn(es, "cpT", 2)
        pL, bpL = self.psn(es, "cpL", 2)
        bys = Buf("ys2")
        for st in range(4):
            s, N = CTX + 1024 * st, 1024
            for c8 in range(8):
                c = st * 8 + c8
                ys_ = c % 2
                for t2, (Yt, bYt) in enumerate(((Y1, bY1), (Y2, bY2))):
                    idx = self.D12[:, t2, c:c + 1].bitcast(U32)
                    self.S.add("pool", lambda e, idx=idx, dst=Yt[ys_]: e.indirect_dma_start(
                        out=dst[:], out_offset=None, in_=self.d_ys, in_offset=IndirectOffsetOnAxis(ap=idx, axis=0)),
                        [self.b_D12], [bYt[ys_]], dma=True)
                self.ts("dve", Y1[ys_][:], Y1[ys_][:], self.P12[:, 0, c:c + 1], ALU.mult, [bY1[ys_], self.b_P12], [bY1[ys_]])
                self.stt(Y1[ys_][:], Y2[ys_][:], self.P12[:, 1, c:c + 1], Y1[ys_][:], ALU.mult, ALU.add,
                         [bY2[ys_], bY1[ys_], self.b_P12], [bY1[ys_]])
                for kq in range(2):
                    a = self.nxt("cpT", 2)
                    for k4 in range(4):
                        k = kq * 4 + k4
                        self.tr(pT[a][:, k4 * 128:(k4 + 1) * 128], Y1[ys_][:, k * 128:(k + 1) * 128], self.ident(), [bY1[ys_], self.b_cst], [bpT[a]])
                    self.copy("act", acc[:, kq * 4:(kq + 1) * 4, c8 * 128:(c8 + 1) * 128], pT[a][:].rearrange("p (k t) -> p k t", k=4), [bpT[a]], [bacc])
            subs = [(0, 512), (512, 512)]
            self.ffn_epilogue(s, N, subs, 0, last, acc, bacc, x1, bx1, pL, bpL, (sq, bsq, mu, bmu, rstd, brstd))

def fm(v):
    v = np.asarray(v, dtype=np.float32)
    n = v.shape[-1] // 128
    return np.swapaxes(v.reshape(v.shape[:-1] + (n, 128)), -1, -2)


def host_consts():
    c = np.zeros((6, 128, 128), np.float32)
    c[0] = np.eye(128)
    c[1] = 1.0 / 1024
    for h in range(2):
        c[2, h * 64:(h + 1) * 64, h * 64:(h + 1) * 64] = 1.0 / 64
    for m in range(128):
        d = m % 32
        partner = m + 16 if d < 16 else m - 16
        c[3, partner, m] = 1.0
    c[4] = 1.0
    c[5, :, 0] = 1.0
    c[5, :, 1] = RMS_EPS
    c[5, :, 2] = LN_EPS
    t = np.arange(SEQ)
    row = (t // 64).astype(np.float32)
    col = (t % 64).astype(np.float32)
    nf = 16
    inv = (10000.0 ** (-np.arange(nf, dtype=np.float32) / nf)).astype(np.float32)
    cosT = np.zeros((128, SEQ), np.float32)
    sinT = np.zeros((128, SEQ), np.float32)
    for p in range(128):
        d = p % 64
        pos = row if d < 32 else col
        ang = (pos * inv[d % 16]).astype(np.float32)
        cosT[p] = np.cos(ang)
        sinT[p] = -np.sin(ang) if (d % 32) < 16 else np.sin(ang)
    invcnt = np.zeros((4, PADW), np.float32)
    for g, wdw in enumerate(POOL_WINDOWS):
        lo = wdw // 2
        for (L, base) in ((CTX, 8), (SEQ, 8 + CTX + 16)):
            tt = np.arange(L)
            start = np.clip(tt - lo, 0, L)
            end = np.clip(tt - lo + wdw, 0, L)
            invcnt[g, base:base + L] = 1.0 / (end - start).astype(np.float32)
    sel8 = np.zeros((8, 8, 128), np.float32)
    for e in range(8):
        sel8[e, e, :] = 1.0
    mc = np.zeros((8, 64), np.float32)
    mc[:, 0:8] = (np.arange(8) * MB)[None, :]
    mc[:, 8:8 + NBLK] = (np.arange(NBLK) * MB)[None, :]
    for e1 in range(8):
        for e2 in range(8):
            mc[e1, 32 + e2] = 1.0 if e1 < e2 else 0.0
    mc2 = np.zeros((128, 32), np.float32)
    mc2[:, 0] = np.arange(128) * 16
    for j in range(16):
        mc2[:, 2 + j] = j
    return c, cosT, sinT, invcnt, sel8, mc, mc2


def relayout_gu(w):
    w = np.asarray(w, dtype=np.float32).reshape(N_EXP, 8, 128, 4, 896)
    w = np.transpose(w, (0, 2, 3, 1, 4))
    return np.ascontiguousarray(w).reshape(N_EXP * 128 * 16, 1792)


def relayout_d(w):
    w = np.asarray(w, dtype=np.float32).reshape(N_EXP, 28, 128, 4, 256)
    w = np.transpose(w, (0, 2, 3, 1, 4))
    return np.ascontiguousarray(w).reshape(N_EXP * 128 * 16, 1792)


def prep_inputs(inp):
    f32 = lambda a: np.ascontiguousarray(np.asarray(a, dtype=np.float32))
    consts, cosT, sinT, invcnt, sel8, mc, mc2 = host_consts()
    vecs = np.zeros((DEPTH, 128, NV), np.float32)
    for l in range(DEPTH):
        vecs[l, :, V_BMOD:V_BMOD + 48] = fm(inp["b_mod"][l])
        vecs[l, :, V_BMERGE:V_BMERGE + 24] = fm(np.asarray(inp["b_merge"][l]).reshape(-1))
        vecs[l, :, V_PSCALE:V_PSCALE + 4] = fm(inp["pool_scale"][l])
        cw = fm(inp["conv_w"][l])
        vecs[l, :, V_CONVW:V_CONVW + 16] = np.transpose(cw, (1, 2, 0)).reshape(128, 16)
        vecs[l, :, V_CONVB:V_CONVB + 4] = fm(inp["conv_b"][l])
        for nm, off in (("lru_ba", V_BA), ("lru_bx", V_BX), ("lru_lambda", V_LAM)):
            a = fm(inp[nm][l])
            vecs[l, :, off:off + 8] = np.transpose(a, (1, 0, 2)).reshape(128, 8)
        vecs[l, :, V_QN] = np.tile(np.asarray(inp["q_norm"][l], np.float32), 2)
        vecs[l, :, V_KN] = np.tile(np.asarray(inp["k_norm"][l], np.float32), 2)
        vecs[l, :, V_LN1G:V_LN1G + 8] = fm(inp["ln1_g"][l])
        vecs[l, :, V_LN1B:V_LN1B + 8] = fm(inp["ln1_b"][l])
        vecs[l, :, V_LN2G:V_LN2G + 8] = fm(inp["ln2_g"][l])
        vecs[l, :, V_LN2B:V_LN2B + 8] = fm(inp["ln2_b"][l])
        vecs[l, :, V_RB:V_RB + 8] = np.asarray(inp["moe_router_b"][0], np.float32)[None, :]
    lru_bd = np.zeros((DEPTH, 4, 4, 128, 128), np.float32)
    for l in range(DEPTH):
        for c in range(4):
            for dr in range(2):
                for wi, nm in enumerate(("lru_wa", "lru_wx")):
                    for hh in range(2):
                        lru_bd[l, c, 2 * dr + wi, hh * 64:(hh + 1) * 64, hh * 64:(hh + 1) * 64] = inp[nm][l][dr][2 * c + hh]
    shared = {
        "vecs": vecs, "w_mod": f32(inp["w_mod"]), "w_in": f32(inp["w_in"]), "pool_w": f32(inp["pool_w"]),
        "lru_bd": lru_bd, "w_branch": f32(np.asarray(inp["w_branch"]).reshape(DEPTH, 1536, D)), "w_out": f32(inp["w_out"]),
        "ffn_w_gate": f32(inp["ffn_w_gate"][0]), "ffn_w_up": f32(inp["ffn_w_up"][0]), "ffn_w_down": f32(inp["ffn_w_down"][0]),
        "moe_router": f32(inp["moe_router"][0]), "moe_g2": relayout_gu(inp["moe_w_gate"][0]), "moe_u2": relayout_gu(inp["moe_w_up"][0]),
        "moe_d2": relayout_d(inp["moe_w_down"][0]), "consts": consts, "cosT": cosT, "sinT": sinT, "invcnt": invcnt, "sel8": sel8, "mconst": mc, "mconst2": mc2,
    }
    maps = []
    cc = fm(inp["c_ctx"])
    for b in range(8):
        cvec = np.stack([fm(inp["c"][b]), cc], axis=-1)
        m = dict(shared)
        m["xT"] = f32(np.asarray(inp["x"][b]).T)
        m["ctxT"] = f32(np.asarray(inp["ctx"][b]).T)
        m["cvec"] = f32(cvec)
        maps.append(m)
    return maps


_NC_CACHE = {}


def kernel(**inputs):
    maps = prep_inputs(inputs)
    if "nc" not in _NC_CACHE:
        _NC_CACHE["nc"] = Ker().build()
    nc = _NC_CACHE["nc"]
    res = run_bass_kernel_spmd(nc, maps, core_ids=list(range(8)))
    out = np.empty((8, SEQ, D), np.float32)
    for b in range(8):
        o = np.asarray(res.results[b]["outT"]).reshape(128, 8, SEQ)
        out[b] = np.transpose(o, (2, 1, 0)).reshape(SEQ, D)
    return out
```

```python
import contextlib
import os
BLK_STAGE = int(os.environ.get('BLK_STAGE', '3'))
NBLK_RUN = int(os.environ.get('NBLK_RUN', '24'))
import numpy as np
import concourse.bass as bass
import concourse.mybir as mybir
from concourse.bass_utils import run_bass_kernel_spmd
from concourse.bass import IndirectOffsetOnAxis

F32 = mybir.dt.float32
BF16 = mybir.dt.bfloat16
I32 = mybir.dt.int32
U32 = mybir.dt.uint32
AF = mybir.ActivationFunctionType
ALU = mybir.AluOpType
AX = mybir.AxisListType

D = 1024
SEQ = 4096
CTX = 256
NT = SEQ + CTX
DEPTH = 2
W_IN = 5376
D_FF = 2816
D_EXP = 3584
N_EXP = 8
MB = 512
NBLK = 24
NSLOT = MB * NBLK
SPARSE_MOE = True
ALPHA = (2 * DEPTH) ** 0.25
LN_EPS = 1e-5
RMS_EPS = 1e-6
PADW = NT + 32
TILES = [(0, 256)] + [(256 + 512 * i, 512) for i in range(8)]
POOL_WINDOWS = (2, 4, 8, 16)

V_BMOD, V_BMERGE, V_PSCALE, V_CONVW, V_CONVB = 0, 48, 72, 76, 92
V_BA, V_BX, V_LAM, V_QN, V_KN = 96, 104, 112, 120, 121
V_LN1G, V_LN1B, V_LN2G, V_LN2B, V_RB = 122, 130, 138, 146, 154
NV = 162


def pad_pos(s):
    return s + 8 if s < CTX else s + 24


class Buf:
    __slots__ = ("name", "w", "r")

    def __init__(self, name=""):
        self.name = name
        self.w = None
        self.r = []


class Op:
    __slots__ = ("eng", "fn", "deps", "needs_inc", "count", "dma", "sem", "waits")

    def __init__(self, eng, fn, dma):
        self.eng = eng
        self.fn = fn
        self.deps = set()
        self.needs_inc = False
        self.count = 0
        self.dma = dma
        self.sem = None
        self.waits = []


ENGS = ["pe", "act", "dve", "pool", "sp"]


class Sched:
    def __init__(self, nc, es, n_dma_sems=48):
        self.nc = nc
        self.n_dma_sems = n_dma_sems
        self.esem = {e: es.enter_context(nc.semaphore(f"se_{e}")) for e in ENGS}
        self.dsem = [es.enter_context(nc.semaphore(f"sd_{i}")) for i in range(n_dma_sems)]
        self.ecount = {e: 0 for e in ENGS}
        self.dma_cnt = [0] * n_dma_sems
        self.dma_rr = 0
        self.total_ops = 0
        self._reset_phase()

    def _reset_phase(self):
        self.ops = {e: [] for e in ENGS}
        self.all = []
        self.dma_last = [None] * self.n_dma_sems
        self.touched = set()

    def add(self, eng, fn, reads=(), writes=(), dma=False):
        op = Op(eng, fn, dma)
        for b in reads:
            if b.w is not None:
                op.deps.add(b.w)
        for b in writes:
            if b.w is not None:
                op.deps.add(b.w)
            for r in b.r:
                op.deps.add(r)
        for b in reads:
            b.r.append(op)
            self.touched.add(b)
        for b in writes:
            b.w = op
            b.r = []
            self.touched.add(b)
        if dma:
            s = self.dma_rr
            self.dma_rr = (self.dma_rr + 1) % self.n_dma_sems
            prev = self.dma_last[s]
            if prev is not None:
                op.deps.add(prev)
            self.dma_last[s] = op
            self.dma_cnt[s] += 1
            op.sem = s
            op.count = self.dma_cnt[s] * 16
            op.needs_inc = True
        op.deps.discard(op)
        self.ops[eng].append(op)
        self.all.append(op)
        return op

    def flush(self):
        nc = self.nc
        for op in self.all:
            nd = set()
            for d in op.deps:
                if (not d.dma) and (not op.dma) and d.eng == "pe" and op.eng == "pe":
                    continue
                nd.add(d)
            op.deps = nd
            for d in nd:
                d.needs_inc = True
        for e in ENGS:
            for op in reversed(self.ops[e]):
                if not op.dma:
                    op.needs_inc = True
                    break
        for e in ENGS:
            c = self.ecount[e]
            for op in self.ops[e]:
                if op.dma:
                    continue
                if op.needs_inc:
                    c += 1
                    op.count = c
            self.ecount[e] = c
        seen = {e: {} for e in ENGS}
        for e in ENGS:
            sn = seen[e]
            for op in self.ops[e]:
                need = {}
                for d in op.deps:
                    key = ("d", d.sem) if d.dma else ("e", d.eng)
                    if d.count > need.get(key, 0):
                        need[key] = d.count
                for key, v in need.items():
                    if sn.get(key, 0) >= v:
                        continue
                    sn[key] = v
                    op.waits.append((key, v))
        bar = []
        for e in ENGS:
            if self.ecount[e] > 0:
                bar.append((("e", e), self.ecount[e]))
        for s in range(self.n_dma_sems):
            if self.dma_cnt[s] > 0:
                bar.append((("d", s), self.dma_cnt[s] * 16))
        handles = {"pe": "tensor", "act": "scalar", "dve": "vector", "pool": "gpsimd", "sp": "sync"}
        with nc.Block() as block:
            def run(ename, eng):
                sn = seen[ename]
                for op in self.ops[ename]:
                    for key, v in op.waits:
                        sem = self.dsem[key[1]] if key[0] == "d" else self.esem[key[1]]
                        eng.wait_ge(sem, v)
                    ins = op.fn(eng)
                    if op.dma:
                        ins.then_inc(self.dsem[op.sem], 16)
                    elif op.needs_inc:
                        ins.then_inc(self.esem[ename], 1)
                for key, v in bar:
                    if key == ("e", ename):
                        continue
                    if sn.get(key, 0) >= v:
                        continue
                    sem = self.dsem[key[1]] if key[0] == "d" else self.esem[key[1]]
                    eng.wait_ge(sem, v)

            for ename in ENGS:
                getattr(block, handles[ename])(lambda eng, ename=ename: run(ename, eng))
        self.total_ops += len(self.all)
        for b in self.touched:
            b.w = None
            b.r = []
        self._reset_phase()


def rev(ap2d):
    (ps, pn), (fs, fn) = ap2d.ap
    return bass.AP(ap2d.tensor, ap2d.offset + fs * (fn - 1), [[ps, pn], [-fs, fn]])


def bcast_last(ap2d, n):
    (ps, pn), (fs, fn) = ap2d.ap
    return bass.AP(ap2d.tensor, ap2d.offset, [[ps, pn], [fs, fn], [0, n]])


def pbcast(ap_row, nparts=128):
    dims = list(ap_row.ap)
    return bass.AP(ap_row.tensor, ap_row.offset, [[0, nparts]] + [list(d) for d in dims[1:]])


class Ker:
    def __init__(self, dbg=False, layers=(0, 1), stop_after=None):
        self.dbg = dbg
        self.layers = layers
        self.stop_after = stop_after
        self.nc = bass.Bass("TRN2", target_bir_lowering=False)
        self.rot = {}

    def dma(self, q, out, in_, reads=(), writes=()):
        if q == "st":
            q = "sp"
            for b in reads:
                if b.w is not None and (not b.w.dma) and b.w.eng in ("act", "dve", "pool"):
                    q = {"act": "act", "pool": "pool", "dve": getattr(self, "st_dve_q", "pool")}[b.w.eng]
                    break
        return self.S.add(q, lambda e: e.dma_start(out=out, in_=in_), reads, writes, dma=True)

    def mm(self, out, lhsT, rhs, start, stop, reads, writes):
        return self.S.add("pe", lambda e: e.matmul(out, lhsT=lhsT, rhs=rhs, start=start, stop=stop), reads, writes)

    def tr(self, out, in_, ident, reads, writes):
        return self.S.add("pe", lambda e: e.transpose(out, in_, ident), reads, writes)

    def act(self, out, in_, func, reads, writes, scale=1.0, bias=None):
        if bias is None:
            return self.S.add("act", lambda e: e.activation(out=out, in_=in_, func=func, scale=scale), reads, writes)
        return self.S.add("act", lambda e: e.activation(out=out, in_=in_, func=func, scale=scale, bias=bias), reads, writes)

    def tt(self, eng, out, in0, in1, op, reads, writes):
        return self.S.add(eng, lambda e: e.tensor_tensor(out=out, in0=in0, in1=in1, op=op), reads, writes)

    def ts(self, eng, out, in0, s1, op0, reads, writes, s2=None, op1=None):
        if op1 is None:
            return self.S.add(eng, lambda e: e.tensor_scalar(out, in0, s1, None, op0), reads, writes)
        return self.S.add(eng, lambda e: e.tensor_scalar(out=out, in0=in0, scalar1=s1, scalar2=s2, op0=op0, op1=op1), reads, writes)

    def stt(self, out, in0, scalar, in1, op0, op1, reads, writes):
        return self.S.add("dve", lambda e: e.scalar_tensor_tensor(out=out, in0=in0, scalar=scalar, in1=in1, op0=op0, op1=op1), reads, writes)

    def recip(self, out, in_, reads, writes):
        return self.S.add("dve", lambda e: e.reciprocal(out=out, in_=in_), reads, writes)

    def copy(self, eng, out, in_, reads, writes):
        if eng == "act":
            return self.act(out, in_, AF.Copy, reads, writes)
        return self.S.add(eng, lambda e: e.tensor_copy(out=out, in_=in_), reads, writes)

    def memset(self, eng, out, val, writes):
        return self.S.add(eng, lambda e: e.memset(out, val), (), writes)

    def scan(self, out, d0, d1, initial, reads, writes):
        return self.S.add("dve", lambda e: e.tensor_tensor_scan(out=out, data0=d0, data1=d1, initial=initial,
                                                                  op0=ALU.mult, op1=ALU.add), reads, writes)

    def rmax(self, out, in_, reads, writes):
        return self.S.add("dve", lambda e: e.tensor_reduce(out=out, in_=in_, op=ALU.max, axis=AX.X), reads, writes)

    def subflush(self, name):
        with self.nc.named_scope(name):
            self.S.flush()

    def un(self, name):
        self.uid = getattr(self, "uid", 0) + 1
        return f"{name}_{self.uid}"

    def sb(self, es, name, shape, dt):
        t = es.enter_context(self.nc.sbuf_tensor(self.un(name), shape, dt))
        return t, Buf(name)

    def sbn(self, es, name, shape, dt, n):
        ts_, bs = [], []
        for i in range(n):
            t, b = self.sb(es, f"{name}{i}", shape, dt)
            ts_.append(t)
            bs.append(b)
        return ts_, bs

    def psn(self, es, name, n, shape=(128, 512), dt=F32):
        ts_, bs = [], []
        for i in range(n):
            ts_.append(es.enter_context(self.nc.psum_tensor(self.un(f"{name}{i}"), list(shape), dt)))
            bs.append(Buf(f"{name}{i}"))
        return ts_, bs

    def nxt(self, key, n):
        v = self.rot.get(key, 0)
        self.rot[key] = v + 1
        return v % n

    def dram(self, name, shape, dt, out=False):
        kind = "ExternalOutput" if (out or self.dbg) else "Internal"
        return self.nc.dram_tensor(name, list(shape), dt, kind=kind).ap()

    def build(self):
        nc = self.nc
        I = lambda name, shape: nc.dram_tensor(name, list(shape), F32, kind="ExternalInput").ap()
        self.i_xT = I("xT", (D, SEQ))
        self.i_ctxT = I("ctxT", (D, CTX))
        self.i_cvec = I("cvec", (128, 8, 2))
        self.i_vecs = I("vecs", (DEPTH, 128, NV))
        self.i_wmod = I("w_mod", (DEPTH, D, 6 * D))
        self.i_win = I("w_in", (DEPTH, D, W_IN))
        self.i_poolw = I("pool_w", (DEPTH, 4, 128, 128))
        self.i_lrubd = I("lru_bd", (DEPTH, 4, 4, 128, 128))
        self.i_wbr = I("w_branch", (DEPTH, 1536, D))
        self.i_wout = I("w_out", (DEPTH, D, D))
        self.i_fg = I("ffn_w_gate", (D, D_FF))
        self.i_fu = I("ffn_w_up", (D, D_FF))
        self.i_fd = I("ffn_w_down", (D_FF, D))
        self.i_router = I("moe_router", (D, N_EXP))
        self.i_mg = I("moe_g2", (N_EXP * 128 * 16, 1792))
        self.i_mu = I("moe_u2", (N_EXP * 128 * 16, 1792))
        self.i_md = I("moe_d2", (N_EXP * 128 * 16, 1792))
        self.i_consts = I("consts", (6, 128, 128))
        self.i_cos = I("cosT", (128, SEQ))
        self.i_sin = I("sinT", (128, SEQ))
        self.i_invcnt = I("invcnt", (4, PADW))
        self.i_sel8 = I("sel8", (8, 8, 128))
        self.i_mc = I("mconst", (8, 64))
        self.i_mc2 = I("mconst2", (128, 32))
        self.d_xs1 = self.dram("xs1", (128, 8, NT), F32)
        self.d_h = self.dram("hT", (128, 8, NT), BF16)
        self.d_q = self.dram("qT", (4, 128, NT), BF16)
        self.d_y = self.dram("yT", (128, 12, NT), BF16)
        self.d_x1 = self.dram("x1T", (128, 8, NT), F32)
        self.d_h2 = self.dram("h2T", (128, 8, NT), BF16)
        self.d_out = self.dram("outT", (128, 8, SEQ), F32, out=True)
        self.d_gate = self.dram("gateT", (8, SEQ), F32)
        self.d_xs = self.dram("Xs", (NSLOT, D), BF16)
        self.d_ys = self.dram("Ys", (NSLOT, D), F32)
        if self.dbg:
            self.d_mod = self.dram("dbg_mod", (DEPTH, 128, 96), F32)
            self.d_k = self.dram("dbg_k", (2, 128, NT), BF16)
            self.d_v = self.dram("dbg_v", (128, 34 * 2 * 66), BF16)

        with contextlib.ExitStack() as es:
            self.S = Sched(nc, es)
            self.cst, self.b_cst = self.sb(es, "cst", [128, 6, 128], F32)
            self.vec, self.b_vec = self.sb(es, "vec", [128, NV], F32)
            self.mod, self.b_mod = self.sb(es, "mod", [128, 48, 2], F32)
            self.modp, self.b_modp = self.sb(es, "modp", [128, 16, 2], F32)
            self.lsc, self.b_lsc = self.sb(es, "lsc", [128, 2, 8], F32)
            self.nbias, self.b_nbias = self.sb(es, "nbias", [128, 1], F32)
            self.nbx, self.b_nbx = self.sb(es, "nbx", [128, 16], F32)
            self.sel, self.b_sel = self.sb(es, "sel", [8, 8, 128], F32)
            self.dma("sp", self.cst[:], self.i_consts.rearrange("c p n -> p c n"), (), [self.b_cst])
            self.dma("sp", self.sel[:], self.i_sel8, (), [self.b_sel])
            self.S.flush()
            for l in self.layers:
                last = l == DEPTH - 1
                if last and SPARSE_MOE:
                    tail = [("merge", self.phase_merge), ("route", self.phase_route), ("blk", self.phase_blocks),
                            ("ffn", self.phase_comb)]
                else:
                    tail = [("merge", self.phase_merge), ("ffn", self.phase_ffn)]
                groups = [[("mod", self.phase_mod)], [("h", self.phase_h)],
                          [("mix", self.phase_mix), ("attn", self.phase_attn)], tail]
                for gi, grp in enumerate(groups):
                    with contextlib.ExitStack() as ges:
                        if gi == 2:
                            self.kd, self.b_kd = self.sbn(ges, "kd", [128, NT], BF16, 4)
                            self.va, self.b_va = self.sb(ges, "va", [128, 34, 2, 66], BF16)
                        if gi == 3 and last:
                            self.P12, self.b_P12 = self.sb(ges, "P12", [128, 2, 32], F32)
                            self.D12, self.b_D12 = self.sb(ges, "D12", [128, 2, 32], I32)
                            self.IG, self.b_IG = self.sb(ges, "IG", [128, NBLK, 16], I32)
                        for name, fn in grp:
                            with contextlib.ExitStack() as pes:
                                fn(pes, l, last)
                                with nc.named_scope(f"L{l}_{name}"):
                                    self.S.flush()
                            if self.stop_after == (l, name):
                                return nc
        return nc

    def ident(self):
        return self.cst[:, 0, :]

    def onesD(self):
        return self.cst[:, 1, :]

    def blk64(self):
        return self.cst[:, 2, :]

    def perm(self):
        return self.cst[:, 3, :]

    def ones(self):
        return self.cst[:, 4, :]

    def xsrc(self, l, s, n):
        if l == 0:
            if s < CTX:
                return self.i_ctxT.rearrange("(k p) n -> p k n", p=128)[:, :, s:s + n]
            return self.i_xT.rearrange("(k p) n -> p k n", p=128)[:, :, s - CTX:s - CTX + n]
        return self.d_xs1[:, :, s:s + n]

    def phase_mod(self, es, l, last):
        vec, bv = self.vec, self.b_vec
        self.dma("sp", vec[:], self.i_vecs[l], (), [bv])
        cv, bcv = self.sb(es, "cv", [128, 8, 2], F32)
        sc, bsc = self.sb(es, "sc", [128, 8, 2], F32)
        self.dma("sp", cv[:], self.i_cvec, (), [bcv])
        self.act(sc[:], cv[:], AF.Silu, [bcv], [bsc])
        wm, bwm = self.sbn(es, "wm", [128, 8, 768], F32, 3)
        psm = es.enter_context(self.nc.psum_tensor(self.un("psm"), [128, 96], F32))
        bpsm = Buf("psm")
        wsrc = self.i_wmod[l].rearrange("(k p) n -> p k n", p=128)
        for g in range(8):
            sl = g % 3
            for hh in range(2):
                self.dma("sp" if hh == 0 else "act", wm[sl][:, hh * 4:(hh + 1) * 4, :], wsrc[:, hh * 4:(hh + 1) * 4, g * 768:(g + 1) * 768], (), [bwm[sl]])
            for jj in range(6):
                j = g * 6 + jj
                for k in range(8):
                    self.mm(psm[:, 2 * j:2 * j + 2], wm[sl][:, k, jj * 128:(jj + 1) * 128], sc[:, k, :],
                            k == 0, k == 7, [bwm[sl], bsc], [bpsm])
        psv = psm[:].rearrange("p (j w) -> p j w", w=2)
        for w in range(2):
            self.tt("dve", self.mod[:, :, w], psv[:, :, w], vec[:, V_BMOD:V_BMOD + 48], ALU.add, [bpsm, bv], [self.b_mod])
        self.ts("dve", self.modp[:, 0:8, :], self.mod[:, 8:16, :], 1.0, ALU.add, [self.b_mod], [self.b_modp])
        self.ts("dve", self.modp[:, 8:16, :], self.mod[:, 32:40, :], 1.0, ALU.add, [self.b_mod], [self.b_modp])
        if self.dbg:
            self.dma("sp", self.d_mod[l], self.mod[:].rearrange("p j w -> p (j w)"), [self.b_mod], [])
        lam = vec[:, V_LAM:V_LAM + 8]
        t = {}
        for nm in ["ab", "e", "y", "y2", "p", "r", "sp"]:
            t[nm], _ = self.sb(es, "sp_" + nm, [128, 8], F32)
        bt = Buf("sptmp")
        self.ts("dve", t["r"][:], lam, -1.0, ALU.mult, [bv], [bt])
        self.tt("dve", t["ab"][:], t["r"][:], lam, ALU.max, [bv, bt], [bt])
        self.act(t["e"][:], t["ab"][:], AF.Exp, [bt], [bt], scale=-1.0)
        self.ts("dve", t["y"][:], t["e"][:], 2.0, ALU.add, [bt], [bt])
        self.recip(t["y"][:], t["y"][:], [bt], [bt])
        self.tt("dve", t["y"][:], t["y"][:], t["e"][:], ALU.mult, [bt], [bt])
        self.tt("dve", t["y2"][:], t["y"][:], t["y"][:], ALU.mult, [bt], [bt])
        self.ts("dve", t["p"][:], t["y2"][:], 1.0 / 13, ALU.mult, [bt], [bt], s2=1.0 / 11, op1=ALU.add)
        for cf in [1.0 / 9, 1.0 / 7, 1.0 / 5, 1.0 / 3, 1.0]:
            self.tt("dve", t["p"][:], t["p"][:], t["y2"][:], ALU.mult, [bt], [bt])
            self.ts("dve", t["p"][:], t["p"][:], cf, ALU.add, [bt], [bt])
        self.tt("dve", t["p"][:], t["p"][:], t["y"][:], ALU.mult, [bt], [bt])
        self.ts("dve", t["r"][:], lam, -1.0, ALU.mult, [bv, bt], [bt], s2=0.0, op1=ALU.max)
        self.stt(t["sp"][:], t["p"][:], 2.0, t["r"][:], ALU.mult, ALU.add, [bt], [bt])
        lsv = self.lsc[:].rearrange("p a b -> p (a b)")
        self.ts("dve", self.lsc[:, 0, :], t["sp"][:], -8.0, ALU.mult, [bt], [self.b_lsc])
        self.ts("dve", self.lsc[:, 1, :], t["sp"][:], -16.0, ALU.mult, [bt], [self.b_lsc])
        self.ts("dve", self.nbx[:], vec[:, V_BA:V_BA + 16], -1.0, ALU.mult, [bv], [self.b_nbx])
        m2, _ = self.sb(es, "m2", [128, 2], F32)
        self.tt("dve", m2[:], vec[:, V_QN:V_QN + 2], vec[:, V_QN:V_QN + 2], ALU.mult, [bv], [bt])
        mx, _ = self.sb(es, "mx", [128, 2], F32)
        pst = es.enter_context(self.nc.psum_tensor(self.un("pst"), [128, 128], F32))
        bpst = Buf("pst")
        self.tr(pst[0:2, :], m2[:], self.ident(), [bt, self.b_cst], [bpst])
        r2, _ = self.sb(es, "r2", [2, 1], F32)
        self.rmax(r2[:], pst[0:2, :], [bpst], [bt])
        l2, _ = self.sb(es, "l2", [2, 1], F32)
        self.act(l2[:], r2[:], AF.Ln, [bt], [bt])
        psb = es.enter_context(self.nc.psum_tensor(self.un("psb"), [128, 2], F32))
        bpsb = Buf("psb")
        self.mm(psb[:, 0:1], self.ones()[0:2, :], l2[:], True, True, [bt, self.b_cst], [bpsb])
        self.act(mx[:, 0:1], psb[:, 0:1], AF.Exp, [bpsb], [bt], scale=0.5)
        self.ts("dve", self.nbias[:], mx[:, 0:1], -8.0, ALU.mult, [bt], [self.b_nbias])

    def phase_h(self, es, l, last):
        xt, bxt = self.sbn(es, "xt", [128, 8, 512], F32, 2)
        ht, bht = self.sbn(es, "ht", [128, 8, 512], BF16, 2)
        for ti, (s, n) in enumerate(TILES):
            w = 1 if s < CTX else 0
            sl = ti % 2
            self.dma("sp", xt[sl][:, :, 0:n], self.xsrc(l, s, n), (), [bxt[sl]])
            for k in range(8):
                if k % 2 == 0:
                    self.act(ht[sl][:, k, 0:n], xt[sl][:, k, 0:n], AF.Identity, [bxt[sl], self.b_mod, self.b_modp], [bht[sl]],
                             scale=self.modp[:, k, w:w + 1], bias=self.mod[:, k, w:w + 1])
                else:
                    self.ts("dve", ht[sl][:, k, 0:n], xt[sl][:, k, 0:n], self.modp[:, k, w:w + 1], ALU.mult,
                            [bxt[sl], self.b_mod, self.b_modp], [bht[sl]], s2=self.mod[:, k, w:w + 1], op1=ALU.add)
            self.dma("st", self.d_h[:, :, s:s + n], ht[sl][:, :, 0:n], [bht[sl]], [])

    def load_h(self, ti):
        s, n = TILES[ti]
        sl = self.nxt("hb", 3)
        self.dma("sp", self.hb[sl][:, :, 0:n], self.d_h[:, :, s:s + n], (), [self.b_hb[sl]])
        return self.hb[sl], self.b_hb[sl]

    def load_wz(self, l, cols):
        sl = self.nxt("wz", 4)
        wsrc = self.i_win[l].rearrange("(k p) n -> p k n", p=128)
        for (do, sc_, nn) in cols:
            self.dma("pool", self.wz[sl][:, :, do:do + nn], wsrc[:, :, sc_:sc_ + nn], (), [self.b_wz[sl]])
        return self.wz[sl], self.b_wz[sl]

    def zmm(self, ps, bps, wz, bwz, hb, bhb, n):
        for k in range(8):
            self.mm(ps[:, 0:n], wz[:, k, :], hb[:, k, 0:n], k == 0, k == 7, [bwz, bhb], [bps])

    def phase_mix(self, es, l, last):
        nc = self.nc
        vec, bv = self.vec, self.b_vec
        self.hb, self.b_hb = self.sbn(es, "hb", [128, 8, 512], BF16, 3)
        self.wz, self.b_wz = self.sbn(es, "wz", [128, 8, 128], BF16, 4)
        pz, bpz = self.psn(es, "pz", 3)
        pa, bpa = self.psn(es, "pa", 4)
        hl, bhl = self.sb(es, "hl", [128, 2], F32)
        tA, btA = self.sbn(es, "tA", [128, 544], F32, 2)
        tB, btB = self.sbn(es, "tB", [128, 544], F32, 2)
        tC, btC = self.sbn(es, "tC", [128, 512], F32, 2)
        tD, btD = self.sbn(es, "tD", [128, 512], F32, 2)
        tE, btE = self.sbn(es, "tE", [128, 512], F32, 2)
        tF, btF = self.sbn(es, "tF", [128, 512], F32, 2)
        ob, bob = self.sbn(es, "ob", [128, 512], BF16, 3)
        db, bdb = self.sbn(es, "db", [128, 512], BF16, 2)
        ic, bic = self.sbn(es, "ic", [128, 512], F32, 2)
        pw, bpw = self.sb(es, "pw", [128, 4, 128], BF16)
        bd, bbd = self.sbn(es, "bd", [128, 4, 128], BF16, 2)
        fes = contextlib.ExitStack()
        zp, bzp = self.sb(fes, "zp", [128, PADW], F32)
        xc, bxc = self.sb(fes, "xc", [128, NT], F32)
        xcb, bxcb = self.sb(fes, "xcb", [128, NT], BF16)
        hsum, bhs = self.sb(fes, "hsum", [128, NT], F32)
        gl, bgl = self.sb(fes, "gl", [128, NT], F32)
        self.memset("pool", zp[:], 0.0, [bzp])
        self.memset("pool", self.va[:, :, :, 64:66], 1.0, [self.b_va])
        for i_ in range(4):
            self.memset("pool", self.kd[i_][:], 0.0, [self.b_kd[i_]])
        self.dma("pool", pw[:], self.i_poolw[l].rearrange("g c d -> c g d"), (), [bpw])

        for g in range(4):
            wz, bwz = self.load_wz(l, [(0, g * 128, 128)])
            for ti, (s, n) in enumerate(TILES):
                hb, bhb = self.load_h(ti)
                p = self.nxt("pz", 3)
                self.zmm(pz[p], bpz[p], wz, bwz, hb, bhb, n)
                self.copy("act", zp[:, pad_pos(s):pad_pos(s) + n], pz[p][:, 0:n], [bpz[p]], [bzp])
            m = g + 1
            for ti, (s, n) in enumerate(TILES):
                if last and s < CTX:
                    continue
                p0 = pad_pos(s)
                a = p0 - (1 << (m - 1))
                lens = [n]
                for i in range(m, 0, -1):
                    lens.append(lens[-1] + (1 << (i - 1)))
                lens = lens[::-1]
                sl = ti % 2
                src, bsrc = zp[:, a:a + lens[0]], bzp
                cur = None
                for i in range(1, m + 1):
                    dst, bdst = (tA[sl], btA[sl]) if i % 2 == 1 else (tB[sl], btB[sl])
                    sh = 1 << (i - 1)
                    if i == 1:
                        in0, in1 = zp[:, a:a + lens[1]], zp[:, a + sh:a + sh + lens[1]]
                    else:
                        in0, in1 = cur[:, 0:lens[i]], cur[:, sh:sh + lens[i]]
                    self.tt("dve" if i % 2 else "pool", dst[:, 0:lens[i]], in0, in1, ALU.add, [bsrc], [bdst])
                    cur, bsrc = dst, bdst
                self.dma("sp", ic[sl][:, 0:n], pbcast(self.i_invcnt[g:g + 1, p0:p0 + n]), (), [bic[sl]])
                self.tt("pool", tC[sl][:, 0:n], cur[:, 0:n], ic[sl][:, 0:n], ALU.mult, [bsrc, bic[sl]], [btC[sl]])
                self.tt("dve", db[sl][:, 0:n], tC[sl][:, 0:n], zp[:, p0:p0 + n], ALU.subtract, [btC[sl], bzp], [bdb[sl]])
                p = self.nxt("pa", 4)
                self.mm(pa[p][:, 0:n], pw[:, g, :], db[sl][:, 0:n], True, True, [bpw, bdb[sl]], [bpa[p]])
                o = self.nxt("ob", 3)
                self.act(ob[o][:, 0:n], pa[p][:, 0:n], AF.Copy, [bpa[p], bv], [bob[o]], scale=vec[:, V_PSCALE + g:V_PSCALE + g + 1])
                self.dma("st", self.d_y[:, g, s:s + n], ob[o][:, 0:n], [bob[o]], [])

        self.subflush(f"L{l}_mixpool")
        for c in range(4):
            wzx, bwzx = self.load_wz(l, [(0, 1280 + c * 128, 128)])
            wzg, bwzg = self.load_wz(l, [(0, 1792 + c * 128, 128)])
            bsl = c % 2
            self.dma("pool", bd[bsl][:], self.i_lrubd[l, c].rearrange("m i j -> i m j"), (), [bbd[bsl]])
            for ti, (s, n) in enumerate(TILES):
                hb, bhb = self.load_h(ti)
                p = self.nxt("pz", 3)
                self.zmm(pz[p], bpz[p], wzx, bwzx, hb, bhb, n)
                self.copy("act", zp[:, pad_pos(s):pad_pos(s) + n], pz[p][:, 0:n], [bpz[p]], [bzp])
                if not (last and s < CTX):
                    p2 = self.nxt("pz", 3)
                    self.zmm(pz[p2], bpz[p2], wzg, bwzg, hb, bhb, n)
                    sl = ti % 2
                    self.act(tC[sl][:, 0:n], pz[p2][:, 0:n], AF.Square, [bpz[p2]], [btC[sl]])
                    self.ts("pool", tC[sl][:, 0:n], tC[sl][:, 0:n], 0.044715, ALU.mult, [btC[sl]], [btC[sl]], s2=1.0, op1=ALU.add)
                    self.tt("dve", tC[sl][:, 0:n], tC[sl][:, 0:n], pz[p2][:, 0:n], ALU.mult, [btC[sl], bpz[p2]], [btC[sl]])
                    self.act(tC[sl][:, 0:n], tC[sl][:, 0:n], AF.Sigmoid, [btC[sl]], [btC[sl]], scale=1.5957691216)
                    self.tt("dve", gl[:, s:s + n], tC[sl][:, 0:n], pz[p2][:, 0:n], ALU.mult, [btC[sl], bpz[p2]], [bgl])
            cw = lambda k: vec[:, V_CONVW + c * 4 + k:V_CONVW + c * 4 + k + 1]
            for ti, (s, n) in enumerate(TILES):
                p0 = pad_pos(s)
                self.ts("dve", xc[:, s:s + n], zp[:, p0 - 2:p0 - 2 + n], cw(0), ALU.mult, [bzp, bv], [bxc],
                        s2=vec[:, V_CONVB + c:V_CONVB + c + 1], op1=ALU.add)
                for k in range(1, 4):
                    self.stt(xc[:, s:s + n], zp[:, p0 - 2 + k:p0 - 2 + k + n], cw(k), xc[:, s:s + n], ALU.mult, ALU.add,
                             [bzp, bv, bxc], [bxc])
                self.copy("pool", xcb[:, s:s + n], xc[:, s:s + n], [bxc], [bxcb])
            for dr in (1, 0):
                order = [0] + list(range(8, 0, -1)) if dr == 1 else list(range(9))
                col = dr * 4 + c
                prev_tile = None
                for g0 in range(0, 9, 2):
                    grp = order[g0:g0 + 2]
                    info = []
                    for gi, ti in enumerate(grp):
                        s, n = TILES[ti]
                        pr = self.nxt("pa", 4)
                        self.mm(pa[pr][:, 0:n], bd[bsl][:, 2 * dr, :], xcb[:, s:s + n], True, True, [bbd[bsl], bxcb], [bpa[pr]])
                        pi = self.nxt("pa", 4)
                        self.mm(pa[pi][:, 0:n], bd[bsl][:, 2 * dr + 1, :], xcb[:, s:s + n], True, True, [bbd[bsl], bxcb], [bpa[pi]])
                        info.append((ti, gi, pr, pi))
                    for (ti, sl, pr, pi) in info:
                        s, n = TILES[ti]
                        self.act(tA[sl][:, 0:n], pa[pr][:, 0:n], AF.Sigmoid, [bpa[pr], bv], [btA[sl]],
                                 bias=vec[:, V_BA + col:V_BA + col + 1])
                        self.act(tB[sl][:, 0:n], pa[pi][:, 0:n], AF.Sigmoid, [bpa[pi], bv], [btB[sl]],
                                 bias=vec[:, V_BX + col:V_BX + col + 1])
                    for (ti, sl, pr, pi) in info:
                        s, n = TILES[ti]
                        self.act(tD[sl][:, 0:n], tA[sl][:, 0:n], AF.Exp, [btA[sl], self.b_lsc], [btD[sl]], scale=self.lsc[:, 0, col:col + 1])
                        self.act(tE[sl][:, 0:n], tA[sl][:, 0:n], AF.Exp, [btA[sl], self.b_lsc], [btE[sl]], scale=self.lsc[:, 1, col:col + 1])
                    for (ti, sl, pr, pi) in info:
                        s, n = TILES[ti]
                        self.act(tE[sl][:, 0:n], tE[sl][:, 0:n], AF.Sqrt, [btE[sl]], [btE[sl]], scale=-1.0, bias=self.cst[:, 5, 0:1])
                    for (ti, sl, pr, pi) in info:
                        s, n = TILES[ti]
                        self.tt("pool", tB[sl][:, 0:n], tB[sl][:, 0:n], tE[sl][:, 0:n], ALU.mult, [btB[sl], btE[sl]], [btB[sl]])
                        self.tt("dve", tB[sl][:, 0:n], tB[sl][:, 0:n], xc[:, s:s + n], ALU.mult, [btB[sl], bxc], [btB[sl]])
                        if dr == 1:
                            if prev_tile is None:
                                init, rd = 0.0, []
                            else:
                                ps_ = TILES[prev_tile[0]][0]
                                init, rd = hsum[:, ps_:ps_ + 1], [bhs]
                            self.scan(rev(hsum[:, s:s + n]), rev(tD[sl][:, 0:n]), rev(tB[sl][:, 0:n]), init,
                                      [btD[sl], btB[sl]] + rd, [bhs])
                        else:
                            if prev_tile is None:
                                init, rd = 0.0, []
                            else:
                                pn = TILES[prev_tile[0]][1]
                                psl = prev_tile[1]
                                init, rd = tF[psl][:, pn - 1:pn], [btF[psl]]
                            if prev_tile is not None and prev_tile[1] == sl:
                                self.copy("dve", hl[:, 0:1], tF[sl][:, TILES[prev_tile[0]][1] - 1:TILES[prev_tile[0]][1]], [btF[sl]], [bhl])
                                init, rd = hl[:, 0:1], [bhl]
                            self.scan(tF[sl][:, 0:n], tD[sl][:, 0:n], tB[sl][:, 0:n], init, [btD[sl], btB[sl]] + rd, [btF[sl]])
                            if not (last and s < CTX):
                                self.tt("dve", tC[sl][:, 0:n], tF[sl][:, 0:n], hsum[:, s:s + n], ALU.add, [btF[sl], bhs], [btC[sl]])
                                o = self.nxt("ob", 3)
                                self.tt("pool", ob[o][:, 0:n], tC[sl][:, 0:n], gl[:, s:s + n], ALU.mult, [btC[sl], bgl], [bob[o]])
                                self.dma("st", self.d_y[:, 8 + c, s:s + n], ob[o][:, 0:n], [bob[o]], [])
                        prev_tile = (ti, sl)

        self.subflush(f"L{l}_mixlru")
        fes.close()
        NQ = 4
        qA, bqA = self.sbn(es, "qA", [128, 512], F32, NQ)
        qB, bqB = self.sbn(es, "qB", [128, 512], F32, NQ)
        qC, bqC = self.sbn(es, "qC", [128, 512], F32, NQ)
        qD, bqD = self.sbn(es, "qD", [128, 512], F32, NQ)
        qE, bqE = self.sbn(es, "qE", [128, 512], F32, NQ)
        qF, bqF = self.sbn(es, "qF", [128, 512], F32, NQ)
        cs_, bcs = self.sbn(es, "cs4", [128, 512], F32, NQ)
        sn_, bsn = self.sbn(es, "sn4", [128, 512], F32, NQ)
        tA, btA, tB, btB, tC, btC, tD, btD, tE, btE, tF, btF = qA, bqA, qB, bqB, qC, bqC, qD, bqD, qE, bqE, qF, bqF
        qcnt = 0
        jobs = [("q", c) for c in range(4)] + [("k", kv) for kv in range(2)]
        wq, bwq = self.sbn(es, "wq", [128, 8, 128], BF16, 7)
        wsrc = self.i_win[l].rearrange("(k p) n -> p k n", p=128)
        for ji, (kind, c) in enumerate(jobs):
            if kind == "q":
                self.dma("pool", wq[ji][:], wsrc[:, :, 512 + c * 128:512 + (c + 1) * 128], (), [bwq[ji]])
            else:
                for hh in range(2):
                    self.dma("pool", wq[ji][:, :, 64 * hh:64 * hh + 64], wsrc[:, :, 1024 + c * 64:1024 + (c + 1) * 64], (), [bwq[ji]])
        self.dma("pool", wq[6][:], wsrc[:, :, 1152:1280], (), [bwq[6]])
        pend = []

        def stage2(sl, n, gcol):
            def f():
                p2 = self.nxt("pa", 4)
                self.mm(pa[p2][:, 0:n], self.blk64(), tB[sl][:, 0:n], True, True, [self.b_cst, btB[sl]], [bpa[p2]])
                self.act(tC[sl][:, 0:n], pa[p2][:, 0:n], AF.Sqrt, [bpa[p2]], [btC[sl]], bias=self.cst[:, 5, 1:2])
                self.recip(tC[sl][:, 0:n], tC[sl][:, 0:n], [btC[sl]], [btC[sl]])
                self.stt(tD[sl][:, 0:n], tA[sl][:, 0:n], vec[:, gcol:gcol + 1], tC[sl][:, 0:n], ALU.mult, ALU.mult,
                         [btA[sl], btC[sl], bv], [btD[sl]])
            return f

        def stage3(sl, n, s, kind, c, isctx, cq):
            def f():
                if kind == "q":
                    o = self.nxt("ob", 3)
                    dst, bdst = ob[o][:, 0:n], bob[o]
                if isctx:
                    if kind == "q":
                        self.copy("pool", dst, tD[sl][:, 0:n], [btD[sl]], [bdst])
                    else:
                        for hh in range(2):
                            self.copy("pool", self.kd[2 * c + hh][64 * hh:64 * hh + 64, s:s + n], tD[sl][64 * hh:64 * hh + 64, 0:n],
                                      [btD[sl]], [self.b_kd[2 * c + hh]])
                else:
                    p3 = self.nxt("pa", 4)
                    self.mm(pa[p3][:, 0:n], self.perm(), tD[sl][:, 0:n], True, True, [self.b_cst, btD[sl]], [bpa[p3]])
                    self.tt("pool", tE[sl][:, 0:n], tD[sl][:, 0:n], cs_[cq][:, 0:n], ALU.mult, [btD[sl], bcs[cq]], [btE[sl]])
                    self.tt("dve", tF[sl][:, 0:n], pa[p3][:, 0:n], sn_[cq][:, 0:n], ALU.mult, [bpa[p3], bsn[cq]], [btF[sl]])
                    if kind == "q":
                        self.tt("pool", dst, tE[sl][:, 0:n], tF[sl][:, 0:n], ALU.add, [btE[sl], btF[sl]], [bdst])
                    else:
                        for hh in range(2):
                            self.tt("dve" if hh else "pool", self.kd[2 * c + hh][64 * hh:64 * hh + 64, s:s + n], tE[sl][64 * hh:64 * hh + 64, 0:n],
                                    tF[sl][64 * hh:64 * hh + 64, 0:n], ALU.add, [btE[sl], btF[sl]], [self.b_kd[2 * c + hh]])
                if kind == "q":
                    self.dma("st", self.d_q[c, :, s:s + n], dst, [bdst], [])
            return f

        def advance():
            for ent in pend:
                ent[2] += 1
            for ent in pend:
                if ent[2] == 1:
                    ent[0]()
            while pend and pend[0][2] >= 2:
                pend.pop(0)[1]()

        tcnt = 0
        for ti, (s, n) in enumerate(TILES):
            isctx = s < CTX
            hb, bhb = self.load_h(ti)
            cq = tcnt % NQ
            tcnt += 1
            if not isctx:
                self.dma("sp", cs_[cq][:, 0:n], self.i_cos[:, s - CTX:s - CTX + n], (), [bcs[cq]])
                self.dma("sp", sn_[cq][:, 0:n], self.i_sin[:, s - CTX:s - CTX + n], (), [bsn[cq]])
            for ji, (kind, c) in enumerate(jobs):
                if kind == "q" and last and isctx:
                    continue
                gcol = V_QN if kind == "q" else V_KN
                wz, bwz = wq[ji], bwq[ji]
                sl = qcnt % NQ
                qcnt += 1
                p = self.nxt("pz", 3)
                self.zmm(pz[p], bpz[p], wz, bwz, hb, bhb, n)
                self.copy("act", tA[sl][:, 0:n], pz[p][:, 0:n], [bpz[p]], [btA[sl]])
                self.act(tB[sl][:, 0:n], pz[p][:, 0:n], AF.Square, [bpz[p]], [btB[sl]])
                pend.append([stage2(sl, n, gcol), stage3(sl, n, s, kind, c, isctx, cq), 0])
                advance()
            for sub in range(n // 128):
                tc = s // 128 + sub
                p = self.nxt("pa", 4)
                for k in range(8):
                    self.mm(pa[p][:, 0:128], hb[:, k, sub * 128:(sub + 1) * 128], wq[6][:, k, :], k == 0, k == 7, [bhb, bwq[6]], [bpa[p]])
                self.copy("act" if sub % 2 else "dve", self.va[:, tc, :, 0:64], pa[p][:, 0:128].rearrange("p (a b) -> p a b", a=2),
                          [bpa[p]], [self.b_va])
        advance()
        advance()
        assert not pend
        if self.dbg:
            for kv in range(2):
                for hh in range(2):
                    self.dma("sp", self.d_k[kv, 64 * hh:64 * hh + 64, :], self.kd[2 * kv + hh][64 * hh:64 * hh + 64, :], [self.b_kd[2 * kv + hh]], [])
            self.dma("sp", self.d_v, self.va[:].rearrange("p a b c -> p (a b c)"), [self.b_va], [])

    def phase_attn(self, es, l, last):
        qt, bqt = self.sbn(es, "qt", [128, 512], BF16, 3)
        pt, bpt = self.sbn(es, "pt", [128, 512], BF16, 6)
        pss, bpss = self.psn(es, "pss", 4)
        pso, bpso = self.psn(es, "pso", 2)
        psb, bpsb = self.psn(es, "psb", 1)
        rs, brs = self.sbn(es, "rs", [128, 512], F32, 2)
        bc, bbc = self.sbn(es, "bc", [64, 512], F32, 2)
        oa, boa = self.sbn(es, "oa", [64, 512], BF16, 3)
        LAG = 2
        work = [(c, ti) for c in range(4) for ti in range(len(TILES)) if not (last and TILES[ti][0] < CTX)]
        qslot = {}

        def load_q(idx):
            c, ti = work[idx]
            s, n = TILES[ti]
            qs = self.nxt("qt", 3)
            self.dma("sp", qt[qs][:, 0:n], self.d_q[c, :, s:s + n], (), [bqt[qs]])
            qslot[idx] = qs

        pending = []

        def make_pv(c, kv, s, n, j, kc, nkc, po, pp):
            def pv():
                self.mm(pso[po][0:65, 0:n], self.va[:, kc, kv, 0:65], pt[pp][:, 0:n], kc == 0, kc == nkc - 1,
                        [self.b_va, bpt[pp]], [bpso[po]])
                if kc == nkc - 1:
                    r = self.nxt("rs", 2)
                    self.recip(rs[r][64:65, 0:n], pso[po][64:65, 0:n], [bpso[po]], [brs[r]])
                    self.mm(psb[0][0:64, 0:n], self.ones()[64:65, 0:64], rs[r][64:65, 0:n], True, True,
                            [self.b_cst, brs[r]], [bpsb[0]])
                    self.copy("act", bc[r][:, 0:n], psb[0][0:64, 0:n], [bpsb[0]], [bbc[r]])
                    o = self.nxt("oa", 3)
                    self.tt("dve", oa[o][:, 0:n], pso[po][0:64, 0:n], bc[r][:, 0:n], ALU.mult, [bpso[po], bbc[r]], [boa[o]])
                    self.dma("st", self.d_y[64 * j:64 * j + 64, 4 + c, s:s + n], oa[o][:, 0:n], [boa[o]], [])
            return pv

        load_q(0)
        for idx, (c, ti) in enumerate(work):
            if idx + 1 < len(work):
                load_q(idx + 1)
            kv = c // 2
            s, n = TILES[ti]
            nkc = 2 if s < CTX else 34
            qs = qslot[idx]
            for j in range(2):
                po = self.nxt("pso", 2)
                for kc in range(nkc):
                    p = self.nxt("pss", 4)
                    self.mm(pss[p][:, 0:n], self.kd[2 * kv + j][:, kc * 128:(kc + 1) * 128],
                            qt[qs][:, 0:n], True, True, [self.b_kd[2 * kv + j], bqt[qs]], [bpss[p]])
                    pp = self.nxt("pt", 6)
                    self.act(pt[pp][:, 0:n], pss[p][:, 0:n], AF.Exp, [bpss[p], self.b_nbias], [bpt[pp]],
                             scale=0.125, bias=self.nbias[:, 0:1])
                    pending.append(make_pv(c, kv, s, n, j, kc, nkc, po, pp))
                    if len(pending) > LAG:
                        pending.pop(0)()
        while pending:
            pending.pop(0)()

    def ln_fm_stages(self, r, br, n, gcol, bcol, outs, bouts, psA, bpsA, psB, bpsB, tmp):
        vec, bv = self.vec, self.b_vec
        sq, bsq, mu, bmu, rstd, brstd = tmp
        if not isinstance(br, (list, tuple)):
            br = [br] * 8
        if not isinstance(bouts, (list, tuple)):
            bouts = [bouts] * 8
        st = []

        def s1():
            for k in range(8):
                self.mm(psA[:, 0:n], self.onesD(), r[k], k == 0, k == 7, [self.b_cst, br[k]], [bpsA])
            for k in range(8):
                s = k % 2
                self.act(sq[s][:, 0:n], r[k], AF.Square, [br[k]], [bsq[s]])
                self.mm(psB[:, 0:n], self.onesD(), sq[s][:, 0:n], k == 0, k == 7, [self.b_cst, bsq[s]], [bpsB])
        st.append(s1)

        def s2():
            self.copy("act", mu[:, 0:n], psA[:, 0:n], [bpsA], [bmu])
            self.act(sq[0][:, 0:n], psA[:, 0:n], AF.Square, [bpsA], [bsq[0]])
            self.tt("dve", rstd[:, 0:n], psB[:, 0:n], sq[0][:, 0:n], ALU.subtract, [bpsB, bsq[0]], [brstd])
            self.act(rstd[:, 0:n], rstd[:, 0:n], AF.Sqrt, [brstd], [brstd], bias=self.cst[:, 5, 2:3])
            self.recip(rstd[:, 0:n], rstd[:, 0:n], [brstd], [brstd])
        st.append(s2)

        def mk(k):
            def f():
                s = k % 2
                self.tt("pool", sq[s][:, 0:n], r[k], mu[:, 0:n], ALU.subtract, [br[k], bmu], [bsq[s]])
                self.tt("dve", sq[s][:, 0:n], sq[s][:, 0:n], rstd[:, 0:n], ALU.mult, [bsq[s], brstd], [bsq[s]])
                self.act(outs[k], sq[s][:, 0:n], AF.Identity, [bsq[s], bv], [bouts[k]],
                         scale=vec[:, gcol + k:gcol + k + 1], bias=vec[:, bcol + k:bcol + k + 1])
            return f
        for k in range(8):
            st.append(mk(k))
        return st

    def ln_fm(self, *a):
        for f in self.ln_fm_stages(*a):
            f()

    def phase_merge(self, es, l, last):
        vec, bv = self.vec, self.b_vec
        wbr, bwbr = self.sb(es, "wbr", [128, 12, 1024], BF16)
        wo, bwo = self.sb(es, "wo", [128, 8, 1024], BF16)
        wgz, bwgz = self.sb(es, "wgz", [128, 8, 3072], BF16)
        xt, bxt = self.sb(es, "mxt", [128, 8, 512], F32)
        ht, bht = self.sb(es, "mht", [128, 8, 512], BF16)
        yt, byt = self.sb(es, "myt", [128, 12, 512], BF16)
        mg, bmg = self.sb(es, "mmg", [128, 8, 512], BF16)
        h2b, bh2b = self.sb(es, "h2b", [128, 8, 512], BF16)
        gs, bgs = self.sbn(es, "gs", [128, 512], F32, 3)
        pr, bpr = self.sbn(es, "pr", [128, 512], F32, 3)
        sq, bsq = self.sbn(es, "lsq", [128, 512], F32, 2)
        mu, bmu = self.sb(es, "lmu", [128, 512], F32)
        rstd, brstd = self.sb(es, "lrs", [128, 512], F32)
        tt_, btt = self.sbn(es, "mtt", [128, 512], F32, 2)
        pg, bpg = self.psn(es, "pg", 2)
        pb, bpb = self.psn(es, "pb", 2)
        po, bpo = self.psn(es, "po", 2)
        pl, bpl = self.psn(es, "pl", 2)
        if last:
            gTt, bgTt = self.sbn(es, "gTt", [8, 512], F32, 2)
            h2f, bh2f = self.sbn(es, "h2f", [128, 512], F32, 2)
            rt, brt = self.sb(es, "rt", [128, 8, 8], F32)
            lgT, blgT = self.sb(es, "lgT", [8, 512], F32)
            L, bL = self.sb(es, "L", [128, 4, 8], F32)
            sm, bsm = self.sb(es, "sm", [128, 64], F32)
            E1, bE1 = self.sb(es, "E1", [128, 4, 8], F32)
            E2, bE2 = self.sb(es, "E2", [128, 4, 8], F32)
            L2, bL2 = self.sb(es, "L2", [128, 4, 8], F32)
            self.dma("sp", rt[:], self.i_router.rearrange("(k p) e -> p k e", p=128), (), [brt])
        wsrc = self.i_win[l].rearrange("(k p) n -> p k n", p=128)
        for cc in range(12):
            self.dma("pool", wbr[:, cc, :], self.i_wbr[l, cc * 128:(cc + 1) * 128, :], (), [bwbr])
        for k in range(8):
            self.dma("pool", wo[:, k, :], self.i_wout[l, k * 128:(k + 1) * 128, :], (), [bwo])
        for k in range(8):
            for hh in range(2):
                self.dma("pool", wgz[:, k, hh * 1536:(hh + 1) * 1536], wsrc[:, k, 2304 + hh * 1536:2304 + (hh + 1) * 1536], (), [bwgz])
        nxt_ = 1 if last else 2
        if nxt_ == 2:
            xt2, _ = self.sb(es, "mxt2", [128, 8, 512], F32)
            xts = [xt, xt2]
        else:
            xts = [xt]
        bxtk = [[Buf(f"xt{i}_{k}") for k in range(8)] for i in range(nxt_)]
        pending = []

        def make_post(xt, bxk, s, n, w):
            r = [xt[:, k, 0:n] for k in range(8)]
            st = self.ln_fm_stages(r, bxk, n, V_LN1G, V_LN1B, r, bxk, pl[0], bpl[0], pl[1], bpl[1], (sq, bsq, mu, bmu, rstd, brstd))
            st.append(lambda: self.dma("st", self.d_x1[:, :, s:s + n], xt[:, :, 0:n], bxk, []))
            state = {}

            def h2k(k):
                def f():
                    if last:
                        if k == 0:
                            state["a"] = self.nxt("pl", 2)
                        a = state["a"]
                        hf = self.nxt("h2f", 2)
                        self.ts("dve", h2f[hf][:, 0:n], xt[:, k, 0:n], self.modp[:, 8 + k, w:w + 1], ALU.mult,
                                [bxk[k], self.b_mod, self.b_modp], [bh2f[hf]], s2=self.mod[:, 24 + k, w:w + 1], op1=ALU.add)
                        self.copy("pool", h2b[:, k, 0:n], h2f[hf][:, 0:n], [bh2f[hf]], [bh2b])
                        self.mm(pl[a][0:8, 0:n], rt[:, k, :], h2f[hf][:, 0:n], k == 0, k == 7, [brt, bh2f[hf]], [bpl[a]])
                    else:
                        self.ts("dve" if k % 2 else "pool", h2b[:, k, 0:n], xt[:, k, 0:n], self.modp[:, 8 + k, w:w + 1], ALU.mult,
                                [bxk[k], self.b_mod, self.b_modp], [bh2b], s2=self.mod[:, 24 + k, w:w + 1], op1=ALU.add)
                return f
            for k in range(8):
                st.append(h2k(k))
            st.append(lambda: self.dma("st", self.d_h2[:, :, s:s + n], h2b[:, :, 0:n], [bh2b], []))
            if last:
                def router():
                    a = state["a"]
                    self.copy("act", lgT[:, 0:n], pl[a][0:8, 0:n], [bpl[a]], [blgT])
                    b_ = self.nxt("pl", 2)
                    for sub in range(4):
                        self.tr(pl[b_][:, sub * 8:(sub + 1) * 8], lgT[0:8, sub * 128:(sub + 1) * 128], self.cst[0:8, 0, 0:8],
                                [blgT, self.b_cst], [bpl[b_]])
                    self.tt("dve", L[:], pl[b_][:, 0:32].rearrange("p (a b) -> p a b", a=4),
                            bass.AP(vec[:, V_RB:V_RB + 8].tensor, vec[:, V_RB:V_RB + 8].offset, [list(vec[:, V_RB:V_RB + 8].ap[0]), [0, 4], [1, 8]]),
                            ALU.add, [bpl[b_], bv], [bL])
                    m1, m2_, d_, e_, p1, p2 = (sm[:, 0:4], sm[:, 4:8], sm[:, 8:12], sm[:, 12:16], sm[:, 16:20], sm[:, 20:24])
                    self.rmax(m1, L[:], [bL], [bsm])
                    self.tt("dve", E1[:], L[:], bcast_last(m1, 8), ALU.is_equal, [bL, bsm], [bE1])
                    self.stt(L2[:], E1[:], -1e30, L[:], ALU.mult, ALU.add, [bE1, bL], [bL2])
                    self.rmax(m2_, L2[:], [bL2], [bsm])
                    self.tt("dve", E2[:], L2[:], bcast_last(m2_, 8), ALU.is_equal, [bL2, bsm], [bE2])
                    self.tt("dve", d_, m2_, m1, ALU.subtract, [bsm], [bsm])
                    self.act(e_, d_, AF.Exp, [bsm], [bsm])
                    self.ts("dve", p1, e_, 1.0, ALU.add, [bsm], [bsm])
                    self.recip(p1, p1, [bsm], [bsm])
                    self.tt("dve", p2, e_, p1, ALU.mult, [bsm], [bsm])
                    self.tt("dve", E1[:], E1[:], bcast_last(p1, 8), ALU.mult, [bE1, bsm], [bE1])
                    self.tt("dve", E2[:], E2[:], bcast_last(p2, 8), ALU.mult, [bE2, bsm], [bE2])
                    self.tt("dve", E1[:], E1[:], E2[:], ALU.add, [bE1, bE2], [bE1])
                    c_ = self.nxt("pl", 2)
                    for sub in range(4):
                        self.tr(pl[c_][0:8, sub * 128:(sub + 1) * 128], E1[:, sub, :], self.ident(), [bE1, self.b_cst], [bpl[c_]])
                    gs_ = self.nxt("gTt", 2)
                    self.copy("act", gTt[gs_][:, 0:n], pl[c_][0:8, 0:n], [bpl[c_]], [bgTt[gs_]])
                    self.dma("st", self.d_gate[:, s - CTX:s - CTX + n], gTt[gs_][:, 0:n], [bgTt[gs_]], [])
                st.append(router)
            return st

        tnum = 0
        for ti, (s, n) in enumerate(TILES):
            isctx = s < CTX
            if isctx and last:
                continue
            w = 1 if isctx else 0
            xi = tnum % nxt_
            tnum += 1
            xt, bxk = xts[xi], bxtk[xi]
            self.dma("sp", ht[:, :, 0:n], self.d_h[:, :, s:s + n], (), [bht])
            self.dma("sp", yt[:, :, 0:n], self.d_y[:, :, s:s + n], (), [byt])
            if nxt_ == 2:
                self.dma("sp", xt[:, :, 0:n], self.xsrc(l, s, n), (), bxk)
            for j in range(8):
                prods = []
                for nb in range(3):
                    a = self.nxt("pg", 2)
                    for k in range(8):
                        self.mm(pg[a][:, 0:n], wgz[:, k, nb * 1024 + j * 128:nb * 1024 + (j + 1) * 128], ht[:, k, 0:n], k == 0, k == 7, [bwgz, bht], [bpg[a]])
                    g_ = self.nxt("gs", 3)
                    self.act(gs[g_][:, 0:n], pg[a][:, 0:n], AF.Sigmoid, [bpg[a], bv], [bgs[g_]],
                             bias=vec[:, V_BMERGE + nb * 8 + j:V_BMERGE + nb * 8 + j + 1])
                    b_ = self.nxt("pb", 2)
                    for cc in range(4):
                        self.mm(pb[b_][:, 0:n], wbr[:, nb * 4 + cc, j * 128:(j + 1) * 128], yt[:, nb * 4 + cc, 0:n],
                                cc == 0, cc == 3, [bwbr, byt], [bpb[b_]])
                    p_ = self.nxt("pr", 3)
                    self.tt("dve", pr[p_][:, 0:n], pb[b_][:, 0:n], gs[g_][:, 0:n], ALU.mult, [bpb[b_], bgs[g_]], [bpr[p_]])
                    prods.append((pr[p_], bpr[p_]))
                self.tt("pool", prods[0][0][:, 0:n], prods[0][0][:, 0:n], prods[1][0][:, 0:n], ALU.add,
                        [prods[0][1], prods[1][1]], [prods[0][1]])
                self.tt("dve", mg[:, j, 0:n], prods[0][0][:, 0:n], prods[2][0][:, 0:n], ALU.add, [prods[0][1], prods[2][1]], [bmg])
                npop = -(-len(pending) // (8 - j))
                for _ in range(npop):
                    pending.pop(0)()
            assert not pending
            if nxt_ == 1:
                self.dma("sp", xt[:, :, 0:n], self.xsrc(l, s, n), (), bxk)
            for j2 in range(8):
                a = self.nxt("po", 2)
                for j in range(8):
                    self.mm(po[a][:, 0:n], wo[:, j, j2 * 128:(j2 + 1) * 128], mg[:, j, 0:n], j == 0, j == 7, [bwo, bmg], [bpo[a]])
                t_ = self.nxt("mtt", 2)
                self.act(tt_[t_][:, 0:n], po[a][:, 0:n], AF.Copy, [bpo[a], self.b_mod], [btt[t_]], scale=self.mod[:, 16 + j2, w:w + 1])
                self.stt(xt[:, j2, 0:n], xt[:, j2, 0:n], ALPHA, tt_[t_][:, 0:n], ALU.mult, ALU.add, [bxk[j2], btt[t_]], [bxk[j2]])
            pending.extend(make_post(xt, bxk, s, n, w))
        while pending:
            pending.pop(0)()

    def phase_ffn(self, es, l, last):
        vec, bv = self.vec, self.b_vec
        moe = (l % 2 == 1)
        assert not moe, "dense-evaluated MoE path removed (weights are host-laid-out for the sparse path)"
        nF = (D_EXP if moe else D_FF) // 128
        nE = N_EXP if moe else 1
        if last:
            sts = [(CTX + 1024 * i, 1024) for i in range(4)]
        else:
            sts = [(0, 256)] + [(CTX + 1024 * i, 1024) for i in range(4)]
        if moe:
            self.gT, self.b_gT = self.sb(es, "gT3", [8, SEQ], F32)
            self.dma("sp", self.gT[:], self.d_gate, (), [self.b_gT])
        h2, bh2 = self.sb(es, "fh2", [128, 8, 1024], BF16)
        A, bA = self.sb(es, "fA", [128, nF, 1024], BF16)
        acc, _ = self.sb(es, "facc", [128, 8, 1024], F32)
        bacck = [Buf(f"facc{k}") for k in range(8)]
        fpend = []
        wgu, bwgu = self.sbn(es, "wgu", [128, 8, 2, 512], BF16, 2)
        wd, bwd = self.sbn(es, "wd", [128, nF, 128], BF16, 2)
        sg, bsg = self.sbn(es, "sg", [128, 512], F32, 2)
        gb, bgb = self.sbn(es, "gb", [128, 1024], F32, 2)
        x1, bx1 = self.sbn(es, "fx1", [128, 1024], F32, 2)
        tmp, btmp = self.sbn(es, "ftmp", [128, 512], F32, 2)
        sq, bsq = self.sbn(es, "fsq", [128, 512], F32, 2)
        mu, bmu = self.sb(es, "fmu", [128, 512], F32)
        rstd, brstd = self.sb(es, "frs", [128, 512], F32)
        pG, bpG = self.psn(es, "pG", 2)
        pU, bpU = self.psn(es, "pU", 2)
        pY, bpY = self.psn(es, "pY", 2)
        pL, bpL = self.psn(es, "pL", 2)
        for (s, N) in sts:
            isctx = s < CTX
            w = 1 if isctx else 0
            subs = [(o, min(512, N - o)) for o in range(0, N, 512)]
            self.dma("sp", h2[:, :, 0:N], self.d_h2[:, :, s:s + N], (), [bh2])
            for e in range(nE):
                if moe:
                    Wg, Wu, Wd = self.i_mg[e], self.i_mu[e], self.i_md[e]
                else:
                    Wg, Wu, Wd = self.i_fg, self.i_fu, self.i_fd
                Wg = Wg.rearrange("(k p) f -> p k f", p=128)
                Wu = Wu.rearrange("(k p) f -> p k f", p=128)
                Wd = Wd.rearrange("(g p) n -> p g n", p=128)
                if moe:
                    g_ = self.nxt("gb", 2)
                    for (o, n) in subs:
                        a = self.nxt("pL", 2)
                        self.mm(pL[a][:, 0:n], self.sel[0:8, e, :], self.gT[0:8, s - CTX + o:s - CTX + o + n], True, True,
                                [self.b_sel, self.b_gT], [bpL[a]])
                        self.copy("act", gb[g_][:, o:o + n], pL[a][:, 0:n], [bpL[a]], [bgb[g_]])
                for g0 in range(0, nF, 4):
                    ng = min(4, nF - g0)
                    ws = self.nxt("wgu", 2)
                    self.dma("pool", wgu[ws][:, :, 0, 0:ng * 128], Wg[:, :, g0 * 128:(g0 + ng) * 128], (), [bwgu[ws]])
                    self.dma("pool", wgu[ws][:, :, 1, 0:ng * 128], Wu[:, :, g0 * 128:(g0 + ng) * 128], (), [bwgu[ws]])
                    for fi in range(ng):
                        fg = g0 + fi
                        for (o, n) in subs:
                            a = self.nxt("pG", 2)
                            for k in range(8):
                                self.mm(pG[a][:, 0:n], wgu[ws][:, k, 0, fi * 128:(fi + 1) * 128], h2[:, k, o:o + n], k == 0, k == 7, [bwgu[ws], bh2], [bpG[a]])
                            b_ = self.nxt("pU", 2)
                            for k in range(8):
                                self.mm(pU[b_][:, 0:n], wgu[ws][:, k, 1, fi * 128:(fi + 1) * 128], h2[:, k, o:o + n], k == 0, k == 7, [bwgu[ws], bh2], [bpU[b_]])
                            s_ = self.nxt("sg", 2)
                            self.act(sg[s_][:, 0:n], pG[a][:, 0:n], AF.Silu, [bpG[a]], [bsg[s_]])
                            self.tt("dve", A[:, fg, o:o + n], pU[b_][:, 0:n], sg[s_][:, 0:n], ALU.mult, [bpU[b_], bsg[s_]], [bA])
                        if fpend:
                            for _ in range(-(-len(fpend) // max(1, (nF - 2 - fg)))):
                                if fpend:
                                    fpend.pop(0)()
                while fpend:
                    fpend.pop(0)()
                for j2 in range(8):
                    ds = self.nxt("wd", 2)
                    self.dma("pool", wd[ds][:], Wd[:, :, j2 * 128:(j2 + 1) * 128], (), [bwd[ds]])
                    for (o, n) in subs:
                        a = self.nxt("pY", 2)
                        for fg in range(nF):
                            self.mm(pY[a][:, 0:n], wd[ds][:, fg, :], A[:, fg, o:o + n], fg == 0, fg == nF - 1, [bwd[ds], bA], [bpY[a]])
                        if not moe:
                            self.copy("act", acc[:, j2, o:o + n], pY[a][:, 0:n], [bpY[a]], [bacck[j2]])
                        elif e == 0:
                            self.tt("dve", acc[:, j2, o:o + n], pY[a][:, 0:n], gb[g_][:, o:o + n], ALU.mult, [bpY[a], bgb[g_]], [bacc])
                        else:
                            t_ = self.nxt("ftmp", 2)
                            self.tt("dve", tmp[t_][:, 0:n], pY[a][:, 0:n], gb[g_][:, o:o + n], ALU.mult, [bpY[a], bgb[g_]], [btmp[t_]])
                            self.tt("pool", acc[:, j2, o:o + n], acc[:, j2, o:o + n], tmp[t_][:, 0:n], ALU.add, [bacc, btmp[t_]], [bacc])
            fpend.extend(self.ffn_epilogue_stages(s, N, subs, w, last, acc, bacck, x1, bx1, pL, bpL, (sq, bsq, mu, bmu, rstd, brstd)))
        while fpend:
            fpend.pop(0)()

    def ffn_epilogue_stages(self, s, N, subs, w, last, acc, bacc, x1, bx1, pL, bpL, lnt):
        if not isinstance(bacc, (list, tuple)):
            bacc = [bacc] * 8
        st = []

        def res(j2):
            def f():
                xs_ = self.nxt("fx1", 2)
                self.dma("sp", x1[xs_][:, 0:N], self.d_x1[:, j2, s:s + N], (), [bx1[xs_]])
                self.ts("dve", acc[:, j2, 0:N], acc[:, j2, 0:N], self.mod[:, 40 + j2, w:w + 1], ALU.mult, [bacc[j2], self.b_mod], [bacc[j2]])
                self.stt(acc[:, j2, 0:N], x1[xs_][:, 0:N], ALPHA, acc[:, j2, 0:N], ALU.mult, ALU.add, [bx1[xs_], bacc[j2]], [bacc[j2]])
            return f
        for j2 in range(8):
            st.append(res(j2))
        for (o, n) in subs:
            r = [acc[:, k, o:o + n] for k in range(8)]
            st.extend(self.ln_fm_stages(r, bacc, n, V_LN2G, V_LN2B, r, bacc, pL[0], bpL[0], pL[1], bpL[1], lnt))

        def store():
            if last:
                self.dma("st", self.d_out[:, :, s - CTX:s - CTX + N], acc[:, :, 0:N], bacc, [])
            else:
                self.dma("st", self.d_xs1[:, :, s:s + N], acc[:, :, 0:N], bacc, [])
        st.append(store)
        return st

    def ffn_epilogue(self, *a):
        for f in self.ffn_epilogue_stages(*a):
            f()

    def phase_route(self, es, l, last):
        gT, bgT = self.sb(es, "gT2", [8, SEQ], F32)
        self.dma("sp", gT[:], self.d_gate, (), [bgT])
        ones8, bo8 = self.sb(es, "ones8", [8, SEQ], F32)
        selT, bsel = self.sb(es, "selT", [8, SEQ], F32)
        incl, binc = self.sb(es, "incl", [8, SEQ], F32)
        dst, bdst = self.sb(es, "dstT", [8, SEQ], F32)
        mc, bmc = self.sb(es, "mc", [8, 64], F32)
        mc2, bmc2 = self.sb(es, "mc2", [128, 32], F32)
        sm, bsm = self.sb(es, "rsm", [8, 64], F32)
        ebf, bebf = self.sb(es, "ebf", [128, NBLK], F32)
        tf, btf = self.sb(es, "rtf", [128, NBLK * 28], F32)
        GT, bGT = self.sb(es, "GTk", [128, 32, 8], F32)
        DT, bDT = self.sb(es, "DTk", [128, 32, 8], F32)
        E1, bE1 = self.sb(es, "rE1", [128, 32, 8], F32)
        E2, bE2 = self.sb(es, "rE2", [128, 32, 8], F32)
        TM, bTM = self.sb(es, "rTM", [128, 32, 8], F32)
        r32, br32 = self.sb(es, "r32", [128, 4, 32], F32)
        ps, bps = self.psn(es, "rps", 3)
        self.dma("sp", mc[:], self.i_mc, (), [bmc])
        self.dma("sp", mc2[:], self.i_mc2, (), [bmc2])
        self.memset("dve", ones8[:], 1.0, [bo8])
        self.ts("dve", selT[:], gT[:], 0.0, ALU.is_gt, [bgT], [bsel])
        self.scan(incl[:], ones8[:], selT[:], 0.0, [bo8, bsel], [binc])
        cnt = incl[:, SEQ - 1:SEQ]
        self.ts("dve", sm[:, 0:8], mc[:, 0:8], cnt, ALU.is_lt, [bmc, binc], [bsm])
        self.S.add("dve", lambda e: e.tensor_reduce(out=sm[:, 8:9], in_=sm[:, 0:8], op=ALU.add, axis=AX.X), [bsm], [bsm])
        self.copy("dve", sm[:, 9:10], sm[:, 8:9], [bsm], [bsm])
        self.mm(ps[0][0:8, 0:2], mc[:, 32:40], sm[:, 8:10], True, True, [bmc, bsm], [bps[0]])
        self.ts("dve", sm[:, 10:11], ps[0][0:8, 0:1], float(MB), ALU.mult, [bps[0]], [bsm])
        self.stt(sm[:, 11:12], sm[:, 8:9], float(MB), sm[:, 10:11], ALU.mult, ALU.add, [bsm], [bsm])
        self.tt("dve", dst[:], incl[:], selT[:], ALU.subtract, [binc, bsel], [bdst])
        self.ts("dve", dst[:], dst[:], sm[:, 10:11], ALU.add, [bdst, bsm], [bdst])
        self.ts("dve", sm[:, 16:16 + NBLK], mc[:, 8:8 + NBLK], sm[:, 11:12], ALU.is_ge, [bmc, bsm], [bsm])
        self.mm(ps[1][:, 0:NBLK], self.ones()[0:8, :], sm[:, 16:16 + NBLK], True, True, [self.b_cst, bsm], [bps[1]])
        self.ts("dve", ebf[:], ps[1][:, 0:NBLK], 7.0, ALU.min, [bps[1]], [bebf])
        self.ts("dve", ebf[:], ebf[:], 2048.0, ALU.mult, [bebf, bmc2], [bebf], s2=mc2[:, 0:1], op1=ALU.add)
        m2ap = mc2[:, 2:18]
        self.tt("dve", tf[:, 0:NBLK * 16].rearrange("p (b f) -> p b f", f=16), bcast_last(ebf[:], 16),
                bass.AP(m2ap.tensor, m2ap.offset, [list(m2ap.ap[0]), [0, NBLK], [1, 16]]), ALU.add, [bebf, bmc2], [btf])
        self.copy("dve", self.IG[:].rearrange("p b f -> p (b f)"), tf[:, 0:NBLK * 16], [btf], [self.b_IG])
        for c in range(32):
            self.tr(ps[2][:, c * 8:(c + 1) * 8], gT[0:8, c * 128:(c + 1) * 128], self.cst[0:8, 0, 0:8], [bgT, self.b_cst], [bps[2]])
        self.copy("act", GT[:].rearrange("p a b -> p (a b)"), ps[2][:, 0:256], [bps[2]], [bGT])
        for c in range(32):
            self.tr(ps[0][:, c * 8:(c + 1) * 8], dst[0:8, c * 128:(c + 1) * 128], self.cst[0:8, 0, 0:8], [bdst, self.b_cst], [bps[0]])
        self.copy("act", DT[:].rearrange("p a b -> p (a b)"), ps[0][:, 0:256], [bps[0]], [bDT])
        m1 = r32[:, 0, :]
        self.rmax(m1, GT[:], [bGT], [br32])
        self.tt("dve", E1[:], GT[:], bcast_last(m1, 8), ALU.is_equal, [bGT, br32], [bE1])
        self.ts("dve", E2[:], GT[:], 0.0, ALU.is_gt, [bGT], [bE2])
        self.tt("dve", E2[:], E2[:], E1[:], ALU.subtract, [bE2, bE1], [bE2])
        rsum = lambda out, in_, rd: self.S.add("dve", lambda e: e.tensor_reduce(out=out, in_=in_, op=ALU.add, axis=AX.X), rd, [br32])
        self.copy("dve", self.P12[:, 0, :], m1, [br32], [self.b_P12])
        self.tt("dve", TM[:], GT[:], E2[:], ALU.mult, [bGT, bE2], [bTM])
        rsum(r32[:, 1, :], TM[:], [bTM])
        self.copy("dve", self.P12[:, 1, :], r32[:, 1, :], [br32], [self.b_P12])
        self.tt("dve", TM[:], DT[:], E1[:], ALU.mult, [bDT, bE1], [bTM])
        rsum(r32[:, 2, :], TM[:], [bTM])
        self.copy("dve", self.D12[:, 0, :], r32[:, 2, :], [br32], [self.b_D12])
        self.tt("dve", TM[:], DT[:], E2[:], ALU.mult, [bDT, bE2], [bTM])
        rsum(r32[:, 3, :], TM[:], [bTM])
        self.copy("dve", self.D12[:, 1, :], r32[:, 3, :], [br32], [self.b_D12])
        if self.dbg:
            dd = self.nc.dram_tensor("dbg_D12", [128, 64], I32, kind="ExternalOutput").ap()
            dg = self.nc.dram_tensor("dbg_IG", [128, 16 * NBLK], I32, kind="ExternalOutput").ap()
            dp = self.nc.dram_tensor("dbg_P12", [128, 64], F32, kind="ExternalOutput").ap()
            ds = self.nc.dram_tensor("dbg_sm", [8, 64], F32, kind="ExternalOutput").ap()
            self.dma("sp", dd, self.D12[:].rearrange("p a b -> p (a b)"), [self.b_D12], [])
            self.dma("sp", dg, self.IG[:].rearrange("p a b -> p (a b)"), [self.b_IG], [])
            self.dma("sp", dp, self.P12[:].rearrange("p a b -> p (a b)"), [self.b_P12], [])
            self.dma("sp", ds, sm[:], [bsm], [])

    def phase_blocks(self, es, l, last):
        nF = D_EXP // 128
        self.st_dve_q = "sp"
        idb, bidb = self.sb(es, "idb", [128, 128], BF16)
        rows, brows = self.sbn(es, "rows", [128, 1024], BF16, 2)
        XT, bXT = self.sbn(es, "XT", [128, 8, MB], BF16, 2)
        A, bA = self.sb(es, "bA", [128, nF, MB], BF16)
        wg, bwg = self.sbn(es, "bwg", [128, 8 * 896], BF16, 2)
        wu, bwu = self.sbn(es, "bwu", [128, 8 * 896], BF16, 2)
        wd, bwd = self.sbn(es, "bwd", [128, nF * 256], BF16, 2)
        Yr, bYr = self.sbn(es, "Yr", [128, 4, 1024], F32, 1)
        h4, bh4 = XT, bXT
        sg, bsg = self.sbn(es, "bsg", [128, MB], F32, 2)
        ptb, bptb = self.psn(es, "ptb", 2, shape=(128, 1024), dt=BF16)
        pG, bpG = self.psn(es, "bpG", 2)
        pU, bpU = self.psn(es, "bpU", 2)
        pY, bpY = self.psn(es, "bpY", 2)
        self.copy("dve", idb[:], self.ident(), [self.b_cst], [bidb])
        bxs = [Buf(f"xs{c}") for c in range(32)]

        def gather(dst_ap, src, idx, reads, writes):
            return self.S.add("pool", lambda e: e.indirect_dma_start(out=dst_ap, out_offset=None, in_=src,
                                                                     in_offset=IndirectOffsetOnAxis(ap=idx, axis=0)),
                              reads, writes, dma=True)

        wslot = {}

        def load_w1(b, cg):
            ws = self.nxt("bwg", 2)
            for kp in range(4):
                idx = self.IG[:, b, cg * 4 + kp:cg * 4 + kp + 1].bitcast(U32)
                gather(wg[ws][:, kp * 1792:(kp + 1) * 1792], self.i_mg, idx, [self.b_IG], [bwg[ws]])
                gather(wu[ws][:, kp * 1792:(kp + 1) * 1792], self.i_mu, idx, [self.b_IG], [bwu[ws]])
            wslot[(b, cg)] = ws

        load_w1(0, 0)
        load_w1(0, 1)
        for g4 in range(8):
            hs = g4 % 2
            self.dma("sp", h4[hs][:], self.d_h2[:, :, CTX + g4 * 512:CTX + (g4 + 1) * 512], (), [bh4[hs]])
            for c4 in range(4):
                c = g4 * 4 + c4
                pp = self.nxt("ptb", 2)
                for k in range(8):
                    self.tr(ptb[pp][:, k * 128:(k + 1) * 128], h4[hs][:, k, c4 * 128:(c4 + 1) * 128], idb[:], [bh4[hs], bidb], [bptb[pp]])
                rs_ = self.nxt("rows", 2)
                self.copy("act" if c % 2 else "dve", rows[rs_][:], ptb[pp][:], [bptb[pp]], [brows[rs_]])
                for t2 in range(2):
                    idx = self.D12[:, t2, c:c + 1].bitcast(U32)
                    self.S.add("pool", lambda e, idx=idx, src=rows[rs_]: e.indirect_dma_start(
                        out=self.d_xs, out_offset=IndirectOffsetOnAxis(ap=idx, axis=0), in_=src[:], in_offset=None),
                        [brows[rs_], self.b_D12], [bxs[c]], dma=True)
        bys = Buf("ys")
        for b in range(NBLK_RUN if BLK_STAGE > 0 else 0):
            xs_ = b % 2
            for c4 in range(4):
                rs_ = self.nxt("rows", 2)
                self.dma("sp", rows[rs_][:], self.d_xs[b * MB + c4 * 128:b * MB + (c4 + 1) * 128, :], bxs, [brows[rs_]])
                pp = self.nxt("ptb", 2)
                for k in range(8):
                    self.tr(ptb[pp][:, k * 128:(k + 1) * 128], rows[rs_][:, k * 128:(k + 1) * 128], idb[:], [brows[rs_], bidb], [bptb[pp]])
                self.copy("act" if c4 % 2 else "dve", XT[xs_][:, :, c4 * 128:(c4 + 1) * 128],
                          ptb[pp][:].rearrange("p (k t) -> p k t", k=8), [bptb[pp]], [bXT[xs_]])
            for cg in range(4 if BLK_STAGE > 1 else 0):
                if (b, cg) not in wslot:
                    load_w1(b, cg)
                ws = wslot[(b, cg)]
                for f7 in range(7):
                    fg = cg * 7 + f7
                    a = self.nxt("bpG", 2)
                    for k in range(8):
                        self.mm(pG[a][:, 0:MB], wg[ws][:, k * 896 + f7 * 128:k * 896 + (f7 + 1) * 128], XT[xs_][:, k, :], k == 0, k == 7, [bwg[ws], bXT[xs_]], [bpG[a]])
                    b_ = self.nxt("bpU", 2)
                    for k in range(8):
                        self.mm(pU[b_][:, 0:MB], wu[ws][:, k * 896 + f7 * 128:k * 896 + (f7 + 1) * 128], XT[xs_][:, k, :], k == 0, k == 7, [bwu[ws], bXT[xs_]], [bpU[b_]])
                    s_ = self.nxt("bsg", 2)
                    self.act(sg[s_][:], pG[a][:, 0:MB], AF.Silu, [bpG[a]], [bsg[s_]])
                    self.tt("dve", A[:, fg, :], pU[b_][:, 0:MB], sg[s_][:], ALU.mult, [bpU[b_], bsg[s_]], [bA])
            ys_ = 0
            for dq in range(4 if BLK_STAGE > 2 else 0):
                ds_ = self.nxt("bwd", 2)
                for fq in range(4):
                    idx = self.IG[:, b, dq * 4 + fq:dq * 4 + fq + 1].bitcast(U32)
                    gather(wd[ds_][:, fq * 1792:(fq + 1) * 1792], self.i_md, idx, [self.b_IG], [bwd[ds_]])
                for c4 in range(4):
                    a = self.nxt("bpY", 2)
                    for fg in range(nF):
                        self.mm(pY[a][:, 0:256], A[:, fg, c4 * 128:(c4 + 1) * 128], wd[ds_][:, fg * 256:(fg + 1) * 256], fg == 0, fg == nF - 1, [bA, bwd[ds_]], [bpY[a]])
                    self.copy("act" if c4 % 2 else "dve", Yr[ys_][:, c4, dq * 256:(dq + 1) * 256], pY[a][:, 0:256], [bpY[a]], [bYr[ys_]])
            self.dma("st", self.d_ys[b * MB:(b + 1) * MB, :].rearrange("(c p) d -> p c d", p=128), Yr[ys_][:], [bYr[ys_]], [bys])
        self.b_ys = bys
        self.st_dve_q = "pool"

    def phase_comb(self, es, l, last):
        accs = [self.sb(es, f"cacc{i}", [128, 8, 1024], F32)[0] for i in range(2)]
        baccs = [[Buf(f"cacc{i}_{k}") for k in range(8)] for i in range(2)]
        Y1, bY1 = self.sbn(es, "cY1", [128, 1024], F32, 2)
        Y2, bY2 = self.sbn(es, "cY2", [128, 1024], F32, 2)
        x1, bx1 = self.sbn(es, "cx1", [128, 1024], F32, 2)
        sq, bsq = self.sbn(es, "csq", [128, 512], F32, 2)
        mu, bmu = self.sb(es, "cmu", [128, 512], F32)
        rstd, brstd = self.sb(es, "crs", [128, 512], F32)
        pT, bpT = self.psn(es, "cpT", 2)
        pL, bpL = self.psn(es, "cpL", 2)
        pend = []
        for st in range(4):
            s, N = CTX + 1024 * st, 1024
            acc, bacc = accs[st % 2], baccs[st % 2]
            for c8 in range(8):
                c = st * 8 + c8
                ys_ = c % 2
                for t2, (Yt, bYt) in enumerate(((Y1, bY1), (Y2, bY2))):
                    idx = self.D12[:, t2, c:c + 1].bitcast(U32)
                    self.S.add("pool", lambda e, idx=idx, dst=Yt[ys_]: e.indirect_dma_start(
                        out=dst[:], out_offset=None, in_=self.d_ys, in_offset=IndirectOffsetOnAxis(ap=idx, axis=0)),
                        [self.b_D12], [bYt[ys_]], dma=True)
                self.ts("dve", Y1[ys_][:], Y1[ys_][:], self.P12[:, 0, c:c + 1], ALU.mult, [bY1[ys_], self.b_P12], [bY1[ys_]])
                self.stt(Y1[ys_][:], Y2[ys_][:], self.P12[:, 1, c:c + 1], Y1[ys_][:], ALU.mult, ALU.add,
                         [bY2[ys_], bY1[ys_], self.b_P12], [bY1[ys_]])
                for kq in range(2):
                    a = self.nxt("cpT", 2)
                    for k4 in range(4):
                        k = kq * 4 + k4
                        self.tr(pT[a][:, k4 * 128:(k4 + 1) * 128], Y1[ys_][:, k * 128:(k + 1) * 128], self.ident(), [bY1[ys_], self.b_cst], [bpT[a]])
                    self.copy("act", acc[:, kq * 4:(kq + 1) * 4, c8 * 128:(c8 + 1) * 128], pT[a][:].rearrange("p (k t) -> p k t", k=4),
                              [bpT[a]], bacc[kq * 4:(kq + 1) * 4])
                for _ in range(-(-len(pend) // (8 - c8))):
                    pend.pop(0)()
            subs = [(0, 512), (512, 512)]
            pend.extend(self.ffn_epilogue_stages(s, N, subs, 0, last, acc, bacc, x1, bx1, pL, bpL, (sq, bsq, mu, bmu, rstd, brstd)))
        while pend:
            pend.pop(0)()


def fm(v):
    v = np.asarray(v, dtype=np.float32)
    n = v.shape[-1] // 128
    return np.swapaxes(v.reshape(v.shape[:-1] + (n, 128)), -1, -2)


def host_consts():
    c = np.zeros((6, 128, 128), np.float32)
    c[0] = np.eye(128)
    c[1] = 1.0 / 1024
    for h in range(2):
        c[2, h * 64:(h + 1) * 64, h * 64:(h + 1) * 64] = 1.0 / 64
    for m in range(128):
        d = m % 32
        partner = m + 16 if d < 16 else m - 16
        c[3, partner, m] = 1.0
    c[4] = 1.0
    c[5, :, 0] = 1.0
    c[5, :, 1] = RMS_EPS
    c[5, :, 2] = LN_EPS
    t = np.arange(SEQ)
    row = (t // 64).astype(np.float32)
    col = (t % 64).astype(np.float32)
    nf = 16
    inv = (10000.0 ** (-np.arange(nf, dtype=np.float32) / nf)).astype(np.float32)
    cosT = np.zeros((128, SEQ), np.float32)
    sinT = np.zeros((128, SEQ), np.float32)
    for p in range(128):
        d = p % 64
        pos = row if d < 32 else col
        ang = (pos * inv[d % 16]).astype(np.float32)
        cosT[p] = np.cos(ang)
        sinT[p] = -np.sin(ang) if (d % 32) < 16 else np.sin(ang)
    invcnt = np.zeros((4, PADW), np.float32)
    for g, wdw in enumerate(POOL_WINDOWS):
        lo = wdw // 2
        for (L, base) in ((CTX, 8), (SEQ, 8 + CTX + 16)):
            tt = np.arange(L)
            start = np.clip(tt - lo, 0, L)
            end = np.clip(tt - lo + wdw, 0, L)
            invcnt[g, base:base + L] = 1.0 / (end - start).astype(np.float32)
    sel8 = np.zeros((8, 8, 128), np.float32)
    for e in range(8):
        sel8[e, e, :] = 1.0
    mc = np.zeros((8, 64), np.float32)
    mc[:, 0:8] = (np.arange(8) * MB)[None, :]
    mc[:, 8:8 + NBLK] = (np.arange(NBLK) * MB)[None, :]
    for e1 in range(8):
        for e2 in range(8):
            mc[e1, 32 + e2] = 1.0 if e1 < e2 else 0.0
    mc2 = np.zeros((128, 32), np.float32)
    mc2[:, 0] = np.arange(128) * 16
    for j in range(16):
        mc2[:, 2 + j] = j
    return c, cosT, sinT, invcnt, sel8, mc, mc2


def relayout_gu(w):
    w = np.asarray(w, dtype=np.float32).reshape(N_EXP, 8, 128, 4, 896)
    w = np.transpose(w, (0, 2, 3, 1, 4))
    return np.ascontiguousarray(w).reshape(N_EXP * 128 * 16, 1792)


def relayout_d(w):
    w = np.asarray(w, dtype=np.float32).reshape(N_EXP, 28, 128, 4, 256)
    w = np.transpose(w, (0, 2, 3, 1, 4))
    return np.ascontiguousarray(w).reshape(N_EXP * 128 * 16, 1792)


def prep_inputs(inp):
    f32 = lambda a: np.ascontiguousarray(np.asarray(a, dtype=np.float32))
    consts, cosT, sinT, invcnt, sel8, mc, mc2 = host_consts()
    vecs = np.zeros((DEPTH, 128, NV), np.float32)
    for l in range(DEPTH):
        vecs[l, :, V_BMOD:V_BMOD + 48] = fm(inp["b_mod"][l])
        vecs[l, :, V_BMERGE:V_BMERGE + 24] = fm(np.asarray(inp["b_merge"][l]).reshape(-1))
        vecs[l, :, V_PSCALE:V_PSCALE + 4] = fm(inp["pool_scale"][l])
        cw = fm(inp["conv_w"][l])
        vecs[l, :, V_CONVW:V_CONVW + 16] = np.transpose(cw, (1, 2, 0)).reshape(128, 16)
        vecs[l, :, V_CONVB:V_CONVB + 4] = fm(inp["conv_b"][l])
        for nm, off in (("lru_ba", V_BA), ("lru_bx", V_BX), ("lru_lambda", V_LAM)):
            a = fm(inp[nm][l])
            vecs[l, :, off:off + 8] = np.transpose(a, (1, 0, 2)).reshape(128, 8)
        vecs[l, :, V_QN] = np.tile(np.asarray(inp["q_norm"][l], np.float32), 2)
        vecs[l, :, V_KN] = np.tile(np.asarray(inp["k_norm"][l], np.float32), 2)
        vecs[l, :, V_LN1G:V_LN1G + 8] = fm(inp["ln1_g"][l])
        vecs[l, :, V_LN1B:V_LN1B + 8] = fm(inp["ln1_b"][l])
        vecs[l, :, V_LN2G:V_LN2G + 8] = fm(inp["ln2_g"][l])
        vecs[l, :, V_LN2B:V_LN2B + 8] = fm(inp["ln2_b"][l])
        vecs[l, :, V_RB:V_RB + 8] = np.asarray(inp["moe_router_b"][0], np.float32)[None, :]
    lru_bd = np.zeros((DEPTH, 4, 4, 128, 128), np.float32)
    for l in range(DEPTH):
        for c in range(4):
            for dr in range(2):
                for wi, nm in enumerate(("lru_wa", "lru_wx")):
                    for hh in range(2):
                        lru_bd[l, c, 2 * dr + wi, hh * 64:(hh + 1) * 64, hh * 64:(hh + 1) * 64] = inp[nm][l][dr][2 * c + hh]
    shared = {
        "vecs": vecs, "w_mod": f32(inp["w_mod"]), "w_in": f32(inp["w_in"]), "pool_w": f32(inp["pool_w"]),
        "lru_bd": lru_bd, "w_branch": f32(np.asarray(inp["w_branch"]).reshape(DEPTH, 1536, D)), "w_out": f32(inp["w_out"]),
        "ffn_w_gate": f32(inp["ffn_w_gate"][0]), "ffn_w_up": f32(inp["ffn_w_up"][0]), "ffn_w_down": f32(inp["ffn_w_down"][0]),
        "moe_router": f32(inp["moe_router"][0]), "moe_g2": relayout_gu(inp["moe_w_gate"][0]), "moe_u2": relayout_gu(inp["moe_w_up"][0]),
        "moe_d2": relayout_d(inp["moe_w_down"][0]), "consts": consts, "cosT": cosT, "sinT": sinT, "invcnt": invcnt, "sel8": sel8, "mconst": mc, "mconst2": mc2,
    }
    maps = []
    cc = fm(inp["c_ctx"])
    for b in range(8):
        cvec = np.stack([fm(inp["c"][b]), cc], axis=-1)
        m = dict(shared)
        m["xT"] = f32(np.asarray(inp["x"][b]).T)
        m["ctxT"] = f32(np.asarray(inp["ctx"][b]).T)
        m["cvec"] = f32(cvec)
        maps.append(m)
    return maps


_NC_CACHE = {}


def kernel(**inputs):
    maps = prep_inputs(inputs)
    if "nc" not in _NC_CACHE:
        _NC_CACHE["nc"] = Ker().build()
    nc = _NC_CACHE["nc"]
    res = run_bass_kernel_spmd(nc, maps, core_ids=list(range(8)))
    out = np.empty((8, SEQ, D), np.float32)
    for b in range(8):
        o = np.asarray(res.results[b]["outT"]).reshape(128, 8, SEQ)
        out[b] = np.transpose(o, (2, 1, 0)).reshape(SEQ, D)
    return out
```

```python
import contextlib
import os
BLK_STAGE = int(os.environ.get('BLK_STAGE', '3'))
NBLK_RUN = int(os.environ.get('NBLK_RUN', '23'))
import numpy as np
import concourse.bass as bass
import concourse.mybir as mybir
from concourse.bass_utils import run_bass_kernel_spmd
from concourse.bass import IndirectOffsetOnAxis

F32 = mybir.dt.float32
BF16 = mybir.dt.bfloat16
I32 = mybir.dt.int32
U32 = mybir.dt.uint32
AF = mybir.ActivationFunctionType
ALU = mybir.AluOpType
AX = mybir.AxisListType

D = 1024
SEQ = 4096
CTX = 256
NT = SEQ + CTX
DEPTH = 2
W_IN = 5376
D_FF = 2816
D_EXP = 3584
N_EXP = 8
MB = 512
NBLK = 23
NSLOT = MB * NBLK
SPARSE_MOE = True
ALPHA = (2 * DEPTH) ** 0.25
LN_EPS = 1e-5
RMS_EPS = 1e-6
PADW = NT + 32
TILES = [(0, 256)] + [(256 + 512 * i, 512) for i in range(8)]
POOL_WINDOWS = (2, 4, 8, 16)

V_BMOD, V_BMERGE, V_PSCALE, V_CONVW, V_CONVB = 0, 48, 72, 76, 92
V_BA, V_BX, V_LAM, V_QN, V_KN = 96, 104, 112, 120, 121
V_LN1G, V_LN1B, V_LN2G, V_LN2B, V_RB = 122, 130, 138, 146, 154
NV = 162


def pad_pos(s):
    return s + 8 if s < CTX else s + 24


class Buf:
    __slots__ = ("name", "w", "r")

    def __init__(self, name=""):
        self.name = name
        self.w = None
        self.r = []


class Op:
    __slots__ = ("eng", "fn", "deps", "needs_inc", "count", "dma", "sem", "waits")

    def __init__(self, eng, fn, dma):
        self.eng = eng
        self.fn = fn
        self.deps = set()
        self.needs_inc = False
        self.count = 0
        self.dma = dma
        self.sem = None
        self.waits = []


ENGS = ["pe", "act", "dve", "pool", "sp"]


class Sched:
    def __init__(self, nc, es, n_dma_sems=48):
        self.nc = nc
        self.n_dma_sems = n_dma_sems
        self.esem = {e: es.enter_context(nc.semaphore(f"se_{e}")) for e in ENGS}
        self.dsem = [es.enter_context(nc.semaphore(f"sd_{i}")) for i in range(n_dma_sems)]
        self.ecount = {e: 0 for e in ENGS}
        self.dma_cnt = [0] * n_dma_sems
        self.dma_rr = 0
        self.total_ops = 0
        self._reset_phase()

    def _reset_phase(self):
        self.ops = {e: [] for e in ENGS}
        self.all = []
        self.dma_last = [None] * self.n_dma_sems
        self.touched = set()

    def add(self, eng, fn, reads=(), writes=(), dma=False):
        op = Op(eng, fn, dma)
        for b in reads:
            if b.w is not None:
                op.deps.add(b.w)
        for b in writes:
            if b.w is not None:
                op.deps.add(b.w)
            for r in b.r:
                op.deps.add(r)
        for b in reads:
            b.r.append(op)
            self.touched.add(b)
        for b in writes:
            b.w = op
            b.r = []
            self.touched.add(b)
        if dma:
            s = self.dma_rr
            self.dma_rr = (self.dma_rr + 1) % self.n_dma_sems
            prev = self.dma_last[s]
            if prev is not None:
                op.deps.add(prev)
            self.dma_last[s] = op
            self.dma_cnt[s] += 1
            op.sem = s
            op.count = self.dma_cnt[s] * 16
            op.needs_inc = True
        op.deps.discard(op)
        self.ops[eng].append(op)
        self.all.append(op)
        return op

    def flush(self):
        nc = self.nc
        for op in self.all:
            nd = set()
            for d in op.deps:
                if (not d.dma) and (not op.dma) and d.eng == "pe" and op.eng == "pe":
                    continue
                nd.add(d)
            op.deps = nd
            for d in nd:
                d.needs_inc = True
        for e in ENGS:
            for op in reversed(self.ops[e]):
                if not op.dma:
                    op.needs_inc = True
                    break
        for e in ENGS:
            c = self.ecount[e]
            for op in self.ops[e]:
                if op.dma:
                    continue
                if op.needs_inc:
                    c += 1
                    op.count = c
            self.ecount[e] = c
        seen = {e: {} for e in ENGS}
        for e in ENGS:
            sn = seen[e]
            for op in self.ops[e]:
                need = {}
                for d in op.deps:
                    key = ("d", d.sem) if d.dma else ("e", d.eng)
                    if d.count > need.get(key, 0):
                        need[key] = d.count
                for key, v in need.items():
                    if sn.get(key, 0) >= v:
                        continue
                    sn[key] = v
                    op.waits.append((key, v))
        bar = []
        for e in ENGS:
            if self.ecount[e] > 0:
                bar.append((("e", e), self.ecount[e]))
        for s in range(self.n_dma_sems):
            if self.dma_cnt[s] > 0:
                bar.append((("d", s), self.dma_cnt[s] * 16))
        handles = {"pe": "tensor", "act": "scalar", "dve": "vector", "pool": "gpsimd", "sp": "sync"}
        with nc.Block() as block:
            def run(ename, eng):
                sn = seen[ename]
                for op in self.ops[ename]:
                    for key, v in op.waits:
                        sem = self.dsem[key[1]] if key[0] == "d" else self.esem[key[1]]
                        eng.wait_ge(sem, v)
                    ins = op.fn(eng)
                    if op.dma:
                        ins.then_inc(self.dsem[op.sem], 16)
                    elif op.needs_inc:
                        ins.then_inc(self.esem[ename], 1)
                for key, v in bar:
                    if key == ("e", ename):
                        continue
                    if sn.get(key, 0) >= v:
                        continue
                    sem = self.dsem[key[1]] if key[0] == "d" else self.esem[key[1]]
                    eng.wait_ge(sem, v)

            for ename in ENGS:
                getattr(block, handles[ename])(lambda eng, ename=ename: run(ename, eng))
        self.total_ops += len(self.all)
        for b in self.touched:
            b.w = None
            b.r = []
        self._reset_phase()


def rev(ap2d):
    (ps, pn), (fs, fn) = ap2d.ap
    return bass.AP(ap2d.tensor, ap2d.offset + fs * (fn - 1), [[ps, pn], [-fs, fn]])


def bcast_last(ap2d, n):
    (ps, pn), (fs, fn) = ap2d.ap
    return bass.AP(ap2d.tensor, ap2d.offset, [[ps, pn], [fs, fn], [0, n]])


def pbcast(ap_row, nparts=128):
    dims = list(ap_row.ap)
    return bass.AP(ap_row.tensor, ap_row.offset, [[0, nparts]] + [list(d) for d in dims[1:]])


class Ker:
    def __init__(self, dbg=False, layers=(0, 1), stop_after=None):
        self.dbg = dbg
        self.layers = layers
        self.stop_after = stop_after
        self.nc = bass.Bass("TRN2", target_bir_lowering=False)
        self.rot = {}

    def dma(self, q, out, in_, reads=(), writes=()):
        if q == "st":
            q = "sp"
            for b in reads:
                if b.w is not None and (not b.w.dma) and b.w.eng in ("act", "dve", "pool"):
                    q = {"act": "act", "pool": "pool", "dve": getattr(self, "st_dve_q", "pool")}[b.w.eng]
                    break
        return self.S.add(q, lambda e: e.dma_start(out=out, in_=in_), reads, writes, dma=True)

    def mm(self, out, lhsT, rhs, start, stop, reads, writes):
        return self.S.add("pe", lambda e: e.matmul(out, lhsT=lhsT, rhs=rhs, start=start, stop=stop), reads, writes)

    def tr(self, out, in_, ident, reads, writes):
        return self.S.add("pe", lambda e: e.transpose(out, in_, ident), reads, writes)

    def act(self, out, in_, func, reads, writes, scale=1.0, bias=None):
        if bias is None:
            return self.S.add("act", lambda e: e.activation(out=out, in_=in_, func=func, scale=scale), reads, writes)
        return self.S.add("act", lambda e: e.activation(out=out, in_=in_, func=func, scale=scale, bias=bias), reads, writes)

    def tt(self, eng, out, in0, in1, op, reads, writes):
        return self.S.add(eng, lambda e: e.tensor_tensor(out=out, in0=in0, in1=in1, op=op), reads, writes)

    def ts(self, eng, out, in0, s1, op0, reads, writes, s2=None, op1=None):
        if op1 is None:
            return self.S.add(eng, lambda e: e.tensor_scalar(out, in0, s1, None, op0), reads, writes)
        return self.S.add(eng, lambda e: e.tensor_scalar(out=out, in0=in0, scalar1=s1, scalar2=s2, op0=op0, op1=op1), reads, writes)

    def stt(self, out, in0, scalar, in1, op0, op1, reads, writes):
        return self.S.add("dve", lambda e: e.scalar_tensor_tensor(out=out, in0=in0, scalar=scalar, in1=in1, op0=op0, op1=op1), reads, writes)

    def recip(self, out, in_, reads, writes):
        return self.S.add("dve", lambda e: e.reciprocal(out=out, in_=in_), reads, writes)

    def copy(self, eng, out, in_, reads, writes):
        if eng == "act":
            return self.act(out, in_, AF.Copy, reads, writes)
        return self.S.add(eng, lambda e: e.tensor_copy(out=out, in_=in_), reads, writes)

    def memset(self, eng, out, val, writes):
        return self.S.add(eng, lambda e: e.memset(out, val), (), writes)

    def scan(self, out, d0, d1, initial, reads, writes):
        return self.S.add("dve", lambda e: e.tensor_tensor_scan(out=out, data0=d0, data1=d1, initial=initial,
                                                                  op0=ALU.mult, op1=ALU.add), reads, writes)

    def rmax(self, out, in_, reads, writes):
        return self.S.add("dve", lambda e: e.tensor_reduce(out=out, in_=in_, op=ALU.max, axis=AX.X), reads, writes)

    def subflush(self, name):
        with self.nc.named_scope(name):
            self.S.flush()

    def un(self, name):
        self.uid = getattr(self, "uid", 0) + 1
        return f"{name}_{self.uid}"

    def sb(self, es, name, shape, dt):
        t = es.enter_context(self.nc.sbuf_tensor(self.un(name), shape, dt))
        return t, Buf(name)

    def sbn(self, es, name, shape, dt, n):
        ts_, bs = [], []
        for i in range(n):
            t, b = self.sb(es, f"{name}{i}", shape, dt)
            ts_.append(t)
            bs.append(b)
        return ts_, bs

    def psn(self, es, name, n, shape=(128, 512), dt=F32):
        ts_, bs = [], []
        for i in range(n):
            ts_.append(es.enter_context(self.nc.psum_tensor(self.un(f"{name}{i}"), list(shape), dt)))
            bs.append(Buf(f"{name}{i}"))
        return ts_, bs

    def nxt(self, key, n):
        v = self.rot.get(key, 0)
        self.rot[key] = v + 1
        return v % n

    def dram(self, name, shape, dt, out=False):
        kind = "ExternalOutput" if (out or self.dbg) else "Internal"
        return self.nc.dram_tensor(name, list(shape), dt, kind=kind).ap()

    def build(self):
        nc = self.nc
        I = lambda name, shape: nc.dram_tensor(name, list(shape), F32, kind="ExternalInput").ap()
        self.i_xT = I("xT", (D, SEQ))
        self.i_ctxT = I("ctxT", (D, CTX))
        self.i_cvec = I("cvec", (128, 8, 2))
        self.i_vecs = I("vecs", (DEPTH, 128, NV))
        self.i_wmod = I("w_mod", (DEPTH, D, 6 * D))
        self.i_win = I("w_in", (DEPTH, D, W_IN))
        self.i_poolw = I("pool_w", (DEPTH, 4, 128, 128))
        self.i_lrubd = I("lru_bd", (DEPTH, 4, 4, 128, 128))
        self.i_wbr = I("w_branch", (DEPTH, 1536, D))
        self.i_wout = I("w_out", (DEPTH, D, D))
        self.i_fg = I("ffn_w_gate", (D, D_FF))
        self.i_fu = I("ffn_w_up", (D, D_FF))
        self.i_fd = I("ffn_w_down", (D_FF, D))
        self.i_router = I("moe_router", (D, N_EXP))
        self.i_mg = I("moe_g2", (N_EXP * 128 * 16, 1792))
        self.i_mu = I("moe_u2", (N_EXP * 128 * 16, 1792))
        self.i_md = I("moe_d2", (N_EXP * 128 * 16, 1792))
        self.i_consts = I("consts", (6, 128, 128))
        self.i_cos = I("cosT", (128, SEQ))
        self.i_sin = I("sinT", (128, SEQ))
        self.i_invcnt = I("invcnt", (4, PADW))
        self.i_sel8 = I("sel8", (8, 8, 128))
        self.i_mc = I("mconst", (8, 64))
        self.i_mc2 = I("mconst2", (128, 32))
        self.d_xs1 = self.dram("xs1", (128, 8, NT), F32)
        self.d_h = self.dram("hT", (128, 8, NT), BF16)
        self.d_q = self.dram("qT", (4, 128, NT), BF16)
        self.d_y = self.dram("yT", (128, 12, NT), BF16)
        self.d_x1 = self.dram("x1T", (128, 8, NT), F32)
        self.d_h2 = self.dram("h2T", (128, 8, NT), BF16)
        self.d_out = self.dram("outT", (128, 8, SEQ), F32, out=True)
        self.d_gate = self.dram("gateT", (8, SEQ), F32)
        self.d_xs = self.dram("Xs", (NSLOT, D), BF16)
        self.d_ys = self.dram("Ys", (NSLOT, D), F32)
        if self.dbg:
            self.d_mod = self.dram("dbg_mod", (DEPTH, 128, 96), F32)
            self.d_k = self.dram("dbg_k", (2, 128, NT), BF16)
            self.d_v = self.dram("dbg_v", (128, 34 * 2 * 66), BF16)

        with contextlib.ExitStack() as es:
            self.S = Sched(nc, es)
            self.cst, self.b_cst = self.sb(es, "cst", [128, 6, 128], F32)
            self.vec, self.b_vec = self.sb(es, "vec", [128, NV], F32)
            self.mod, self.b_mod = self.sb(es, "mod", [128, 48, 2], F32)
            self.modp, self.b_modp = self.sb(es, "modp", [128, 16, 2], F32)
            self.lsc, self.b_lsc = self.sb(es, "lsc", [128, 2, 8], F32)
            self.nbias, self.b_nbias = self.sb(es, "nbias", [128, 1], F32)
            self.nbx, self.b_nbx = self.sb(es, "nbx", [128, 16], F32)
            self.sel, self.b_sel = self.sb(es, "sel", [8, 8, 128], F32)
            self.dma("sp", self.cst[:], self.i_consts.rearrange("c p n -> p c n"), (), [self.b_cst])
            self.dma("sp", self.sel[:], self.i_sel8, (), [self.b_sel])
            self.S.flush()
            for l in self.layers:
                last = l == DEPTH - 1
                if last and SPARSE_MOE:
                    tail = [("merge", self.phase_merge), ("route", self.phase_route), ("blk", self.phase_blocks),
                            ("ffn", self.phase_comb)]
                else:
                    tail = [("merge", self.phase_merge), ("ffn", self.phase_ffn)]
                groups = [[("mod", self.phase_mod)], [("h", self.phase_h)],
                          [("mix", self.phase_mix), ("attn", self.phase_attn)], tail]
                for gi, grp in enumerate(groups):
                    with contextlib.ExitStack() as ges:
                        if gi == 2:
                            self.kd, self.b_kd = self.sbn(ges, "kd", [128, NT], BF16, 4)
                            self.va, self.b_va = self.sb(ges, "va", [128, 34, 2, 66], BF16)
                        if gi == 3 and last:
                            self.P12, self.b_P12 = self.sb(ges, "P12", [128, 2, 32], F32)
                            self.D12, self.b_D12 = self.sb(ges, "D12", [128, 2, 32], I32)
                            self.IG, self.b_IG = self.sb(ges, "IG", [128, NBLK, 16], I32)
                        for name, fn in grp:
                            with contextlib.ExitStack() as pes:
                                fn(pes, l, last)
                                with nc.named_scope(f"L{l}_{name}"):
                                    self.S.flush()
                            if self.stop_after == (l, name):
                                return nc
        return nc

    def ident(self):
        return self.cst[:, 0, :]

    def onesD(self):
        return self.cst[:, 1, :]

    def blk64(self):
        return self.cst[:, 2, :]

    def perm(self):
        return self.cst[:, 3, :]

    def ones(self):
        return self.cst[:, 4, :]

    def xsrc(self, l, s, n):
        if l == 0:
            if s < CTX:
                return self.i_ctxT.rearrange("(k p) n -> p k n", p=128)[:, :, s:s + n]
            return self.i_xT.rearrange("(k p) n -> p k n", p=128)[:, :, s - CTX:s - CTX + n]
        return self.d_xs1[:, :, s:s + n]

    def phase_mod(self, es, l, last):
        vec, bv = self.vec, self.b_vec
        self.dma("sp", vec[:], self.i_vecs[l], (), [bv])
        cv, bcv = self.sb(es, "cv", [128, 8, 2], F32)
        sc, bsc = self.sb(es, "sc", [128, 8, 2], F32)
        self.dma("sp", cv[:], self.i_cvec, (), [bcv])
        self.act(sc[:], cv[:], AF.Silu, [bcv], [bsc])
        wm, bwm = self.sbn(es, "wm", [128, 8, 768], F32, 3)
        psm = es.enter_context(self.nc.psum_tensor(self.un("psm"), [128, 96], F32))
        bpsm = Buf("psm")
        wsrc = self.i_wmod[l].rearrange("(k p) n -> p k n", p=128)
        for g in range(8):
            sl = g % 3
            for hh in range(2):
                self.dma("sp" if hh == 0 else "act", wm[sl][:, hh * 4:(hh + 1) * 4, :], wsrc[:, hh * 4:(hh + 1) * 4, g * 768:(g + 1) * 768], (), [bwm[sl]])
            for jj in range(6):
                j = g * 6 + jj
                for k in range(8):
                    self.mm(psm[:, 2 * j:2 * j + 2], wm[sl][:, k, jj * 128:(jj + 1) * 128], sc[:, k, :],
                            k == 0, k == 7, [bwm[sl], bsc], [bpsm])
        psv = psm[:].rearrange("p (j w) -> p j w", w=2)
        for w in range(2):
            self.tt("dve", self.mod[:, :, w], psv[:, :, w], vec[:, V_BMOD:V_BMOD + 48], ALU.add, [bpsm, bv], [self.b_mod])
        self.ts("dve", self.modp[:, 0:8, :], self.mod[:, 8:16, :], 1.0, ALU.add, [self.b_mod], [self.b_modp])
        self.ts("dve", self.modp[:, 8:16, :], self.mod[:, 32:40, :], 1.0, ALU.add, [self.b_mod], [self.b_modp])
        if self.dbg:
            self.dma("sp", self.d_mod[l], self.mod[:].rearrange("p j w -> p (j w)"), [self.b_mod], [])
        lam = vec[:, V_LAM:V_LAM + 8]
        t = {}
        for nm in ["ab", "e", "y", "y2", "p", "r", "sp"]:
            t[nm], _ = self.sb(es, "sp_" + nm, [128, 8], F32)
        bt = Buf("sptmp")
        self.ts("dve", t["r"][:], lam, -1.0, ALU.mult, [bv], [bt])
        self.tt("dve", t["ab"][:], t["r"][:], lam, ALU.max, [bv, bt], [bt])
        self.act(t["e"][:], t["ab"][:], AF.Exp, [bt], [bt], scale=-1.0)
        self.ts("dve", t["y"][:], t["e"][:], 2.0, ALU.add, [bt], [bt])
        self.recip(t["y"][:], t["y"][:], [bt], [bt])
        self.tt("dve", t["y"][:], t["y"][:], t["e"][:], ALU.mult, [bt], [bt])
        self.tt("dve", t["y2"][:], t["y"][:], t["y"][:], ALU.mult, [bt], [bt])
        self.ts("dve", t["p"][:], t["y2"][:], 1.0 / 13, ALU.mult, [bt], [bt], s2=1.0 / 11, op1=ALU.add)
        for cf in [1.0 / 9, 1.0 / 7, 1.0 / 5, 1.0 / 3, 1.0]:
            self.tt("dve", t["p"][:], t["p"][:], t["y2"][:], ALU.mult, [bt], [bt])
            self.ts("dve", t["p"][:], t["p"][:], cf, ALU.add, [bt], [bt])
        self.tt("dve", t["p"][:], t["p"][:], t["y"][:], ALU.mult, [bt], [bt])
        self.ts("dve", t["r"][:], lam, -1.0, ALU.mult, [bv, bt], [bt], s2=0.0, op1=ALU.max)
        self.stt(t["sp"][:], t["p"][:], 2.0, t["r"][:], ALU.mult, ALU.add, [bt], [bt])
        lsv = self.lsc[:].rearrange("p a b -> p (a b)")
        self.ts("dve", self.lsc[:, 0, :], t["sp"][:], -8.0, ALU.mult, [bt], [self.b_lsc])
        self.ts("dve", self.lsc[:, 1, :], t["sp"][:], -16.0, ALU.mult, [bt], [self.b_lsc])
        self.ts("dve", self.nbx[:], vec[:, V_BA:V_BA + 16], -1.0, ALU.mult, [bv], [self.b_nbx])
        m2, _ = self.sb(es, "m2", [128, 2], F32)
        self.tt("dve", m2[:], vec[:, V_QN:V_QN + 2], vec[:, V_QN:V_QN + 2], ALU.mult, [bv], [bt])
        mx, _ = self.sb(es, "mx", [128, 2], F32)
        pst = es.enter_context(self.nc.psum_tensor(self.un("pst"), [128, 128], F32))
        bpst = Buf("pst")
        self.tr(pst[0:2, :], m2[:], self.ident(), [bt, self.b_cst], [bpst])
        r2, _ = self.sb(es, "r2", [2, 1], F32)
        self.rmax(r2[:], pst[0:2, :], [bpst], [bt])
        l2, _ = self.sb(es, "l2", [2, 1], F32)
        self.act(l2[:], r2[:], AF.Ln, [bt], [bt])
        psb = es.enter_context(self.nc.psum_tensor(self.un("psb"), [128, 2], F32))
        bpsb = Buf("psb")
        self.mm(psb[:, 0:1], self.ones()[0:2, :], l2[:], True, True, [bt, self.b_cst], [bpsb])
        self.act(mx[:, 0:1], psb[:, 0:1], AF.Exp, [bpsb], [bt], scale=0.5)
        self.ts("dve", self.nbias[:], mx[:, 0:1], -8.0, ALU.mult, [bt], [self.b_nbias])

    def phase_h(self, es, l, last):
        xt, bxt = self.sbn(es, "xt", [128, 8, 512], F32, 2)
        ht, bht = self.sbn(es, "ht", [128, 8, 512], BF16, 2)
        for ti, (s, n) in enumerate(TILES):
            w = 1 if s < CTX else 0
            sl = ti % 2
            self.dma("sp", xt[sl][:, :, 0:n], self.xsrc(l, s, n), (), [bxt[sl]])
            for k in range(8):
                if k % 2 == 0:
                    self.act(ht[sl][:, k, 0:n], xt[sl][:, k, 0:n], AF.Identity, [bxt[sl], self.b_mod, self.b_modp], [bht[sl]],
                             scale=self.modp[:, k, w:w + 1], bias=self.mod[:, k, w:w + 1])
                else:
                    self.ts("dve", ht[sl][:, k, 0:n], xt[sl][:, k, 0:n], self.modp[:, k, w:w + 1], ALU.mult,
                            [bxt[sl], self.b_mod, self.b_modp], [bht[sl]], s2=self.mod[:, k, w:w + 1], op1=ALU.add)
            self.dma("st", self.d_h[:, :, s:s + n], ht[sl][:, :, 0:n], [bht[sl]], [])

    def load_h(self, ti):
        s, n = TILES[ti]
        sl = self.nxt("hb", 3)
        self.dma("sp", self.hb[sl][:, :, 0:n], self.d_h[:, :, s:s + n], (), [self.b_hb[sl]])
        return self.hb[sl], self.b_hb[sl]

    def load_wz(self, l, cols):
        sl = self.nxt("wz", 4)
        wsrc = self.i_win[l].rearrange("(k p) n -> p k n", p=128)
        for (do, sc_, nn) in cols:
            self.dma("pool", self.wz[sl][:, :, do:do + nn], wsrc[:, :, sc_:sc_ + nn], (), [self.b_wz[sl]])
        return self.wz[sl], self.b_wz[sl]

    def zmm(self, ps, bps, wz, bwz, hb, bhb, n):
        for k in range(8):
            self.mm(ps[:, 0:n], wz[:, k, :], hb[:, k, 0:n], k == 0, k == 7, [bwz, bhb], [bps])

    def phase_mix(self, es, l, last):
        nc = self.nc
        vec, bv = self.vec, self.b_vec
        self.hb, self.b_hb = self.sbn(es, "hb", [128, 8, 512], BF16, 3)
        self.wz, self.b_wz = self.sbn(es, "wz", [128, 8, 128], BF16, 4)
        pz, bpz = self.psn(es, "pz", 3)
        pa, bpa = self.psn(es, "pa", 4)
        hl, bhl = self.sb(es, "hl", [128, 2], F32)
        tA, btA = self.sbn(es, "tA", [128, 544], F32, 2)
        tB, btB = self.sbn(es, "tB", [128, 544], F32, 2)
        tC, btC = self.sbn(es, "tC", [128, 512], F32, 2)
        tD, btD = self.sbn(es, "tD", [128, 512], F32, 2)
        tE, btE = self.sbn(es, "tE", [128, 512], F32, 2)
        tF, btF = self.sbn(es, "tF", [128, 512], F32, 2)
        ob, bob = self.sbn(es, "ob", [128, 512], BF16, 3)
        db, bdb = self.sbn(es, "db", [128, 512], BF16, 2)
        ic, bic = self.sbn(es, "ic", [128, 512], F32, 2)
        pw, bpw = self.sb(es, "pw", [128, 4, 128], BF16)
        bd, bbd = self.sbn(es, "bd", [128, 4, 128], BF16, 2)
        fes = contextlib.ExitStack()
        zp, bzp = self.sb(fes, "zp", [128, PADW], F32)
        xc, bxc = self.sb(fes, "xc", [128, NT], F32)
        xcb, bxcb = self.sb(fes, "xcb", [128, NT], BF16)
        hsum, bhs = self.sb(fes, "hsum", [128, NT], F32)
        gl, bgl = self.sb(fes, "gl", [128, NT], F32)
        self.memset("pool", zp[:], 0.0, [bzp])
        self.memset("pool", self.va[:, :, :, 64:66], 1.0, [self.b_va])
        for i_ in range(4):
            self.memset("pool", self.kd[i_][:], 0.0, [self.b_kd[i_]])
        self.dma("pool", pw[:], self.i_poolw[l].rearrange("g c d -> c g d"), (), [bpw])

        for g in range(4):
            wz, bwz = self.load_wz(l, [(0, g * 128, 128)])
            for ti, (s, n) in enumerate(TILES):
                hb, bhb = self.load_h(ti)
                p = self.nxt("pz", 3)
                self.zmm(pz[p], bpz[p], wz, bwz, hb, bhb, n)
                self.copy("act", zp[:, pad_pos(s):pad_pos(s) + n], pz[p][:, 0:n], [bpz[p]], [bzp])
            m = g + 1
            for ti, (s, n) in enumerate(TILES):
                if last and s < CTX:
                    continue
                p0 = pad_pos(s)
                a = p0 - (1 << (m - 1))
                lens = [n]
                for i in range(m, 0, -1):
                    lens.append(lens[-1] + (1 << (i - 1)))
                lens = lens[::-1]
                sl = ti % 2
                src, bsrc = zp[:, a:a + lens[0]], bzp
                cur = None
                for i in range(1, m + 1):
                    dst, bdst = (tA[sl], btA[sl]) if i % 2 == 1 else (tB[sl], btB[sl])
                    sh = 1 << (i - 1)
                    if i == 1:
                        in0, in1 = zp[:, a:a + lens[1]], zp[:, a + sh:a + sh + lens[1]]
                    else:
                        in0, in1 = cur[:, 0:lens[i]], cur[:, sh:sh + lens[i]]
                    self.tt("dve" if i % 2 else "pool", dst[:, 0:lens[i]], in0, in1, ALU.add, [bsrc], [bdst])
                    cur, bsrc = dst, bdst
                self.dma("sp", ic[sl][:, 0:n], pbcast(self.i_invcnt[g:g + 1, p0:p0 + n]), (), [bic[sl]])
                self.tt("pool", tC[sl][:, 0:n], cur[:, 0:n], ic[sl][:, 0:n], ALU.mult, [bsrc, bic[sl]], [btC[sl]])
                self.tt("dve", db[sl][:, 0:n], tC[sl][:, 0:n], zp[:, p0:p0 + n], ALU.subtract, [btC[sl], bzp], [bdb[sl]])
                p = self.nxt("pa", 4)
                self.mm(pa[p][:, 0:n], pw[:, g, :], db[sl][:, 0:n], True, True, [bpw, bdb[sl]], [bpa[p]])
                o = self.nxt("ob", 3)
                self.act(ob[o][:, 0:n], pa[p][:, 0:n], AF.Copy, [bpa[p], bv], [bob[o]], scale=vec[:, V_PSCALE + g:V_PSCALE + g + 1])
                self.dma("st", self.d_y[:, g, s:s + n], ob[o][:, 0:n], [bob[o]], [])

        self.subflush(f"L{l}_mixpool")
        for c in range(4):
            wzx, bwzx = self.load_wz(l, [(0, 1280 + c * 128, 128)])
            wzg, bwzg = self.load_wz(l, [(0, 1792 + c * 128, 128)])
            bsl = c % 2
            self.dma("pool", bd[bsl][:], self.i_lrubd[l, c].rearrange("m i j -> i m j"), (), [bbd[bsl]])
            for ti, (s, n) in enumerate(TILES):
                hb, bhb = self.load_h(ti)
                p = self.nxt("pz", 3)
                self.zmm(pz[p], bpz[p], wzx, bwzx, hb, bhb, n)
                self.copy("act", zp[:, pad_pos(s):pad_pos(s) + n], pz[p][:, 0:n], [bpz[p]], [bzp])
                if not (last and s < CTX):
                    p2 = self.nxt("pz", 3)
                    self.zmm(pz[p2], bpz[p2], wzg, bwzg, hb, bhb, n)
                    sl = ti % 2
                    self.act(tC[sl][:, 0:n], pz[p2][:, 0:n], AF.Square, [bpz[p2]], [btC[sl]])
                    self.ts("pool", tC[sl][:, 0:n], tC[sl][:, 0:n], 0.044715, ALU.mult, [btC[sl]], [btC[sl]], s2=1.0, op1=ALU.add)
                    self.tt("dve", tC[sl][:, 0:n], tC[sl][:, 0:n], pz[p2][:, 0:n], ALU.mult, [btC[sl], bpz[p2]], [btC[sl]])
                    self.act(tC[sl][:, 0:n], tC[sl][:, 0:n], AF.Sigmoid, [btC[sl]], [btC[sl]], scale=1.5957691216)
                    self.tt("dve", gl[:, s:s + n], tC[sl][:, 0:n], pz[p2][:, 0:n], ALU.mult, [btC[sl], bpz[p2]], [bgl])
            cw = lambda k: vec[:, V_CONVW + c * 4 + k:V_CONVW + c * 4 + k + 1]
            for ti, (s, n) in enumerate(TILES):
                p0 = pad_pos(s)
                self.ts("dve", xc[:, s:s + n], zp[:, p0 - 2:p0 - 2 + n], cw(0), ALU.mult, [bzp, bv], [bxc],
                        s2=vec[:, V_CONVB + c:V_CONVB + c + 1], op1=ALU.add)
                for k in range(1, 4):
                    self.stt(xc[:, s:s + n], zp[:, p0 - 2 + k:p0 - 2 + k + n], cw(k), xc[:, s:s + n], ALU.mult, ALU.add,
                             [bzp, bv, bxc], [bxc])
                self.copy("pool", xcb[:, s:s + n], xc[:, s:s + n], [bxc], [bxcb])
            for dr in (1, 0):
                order = [0] + list(range(8, 0, -1)) if dr == 1 else list(range(9))
                col = dr * 4 + c
                prev_tile = None
                for g0 in range(0, 9, 2):
                    grp = order[g0:g0 + 2]
                    info = []
                    for gi, ti in enumerate(grp):
                        s, n = TILES[ti]
                        pr = self.nxt("pa", 4)
                        self.mm(pa[pr][:, 0:n], bd[bsl][:, 2 * dr, :], xcb[:, s:s + n], True, True, [bbd[bsl], bxcb], [bpa[pr]])
                        pi = self.nxt("pa", 4)
                        self.mm(pa[pi][:, 0:n], bd[bsl][:, 2 * dr + 1, :], xcb[:, s:s + n], True, True, [bbd[bsl], bxcb], [bpa[pi]])
                        info.append((ti, gi, pr, pi))
                    for (ti, sl, pr, pi) in info:
                        s, n = TILES[ti]
                        self.act(tA[sl][:, 0:n], pa[pr][:, 0:n], AF.Sigmoid, [bpa[pr], bv], [btA[sl]],
                                 bias=vec[:, V_BA + col:V_BA + col + 1])
                        self.act(tB[sl][:, 0:n], pa[pi][:, 0:n], AF.Sigmoid, [bpa[pi], bv], [btB[sl]],
                                 bias=vec[:, V_BX + col:V_BX + col + 1])
                    for (ti, sl, pr, pi) in info:
                        s, n = TILES[ti]
                        self.act(tD[sl][:, 0:n], tA[sl][:, 0:n], AF.Exp, [btA[sl], self.b_lsc], [btD[sl]], scale=self.lsc[:, 0, col:col + 1])
                        self.act(tE[sl][:, 0:n], tA[sl][:, 0:n], AF.Exp, [btA[sl], self.b_lsc], [btE[sl]], scale=self.lsc[:, 1, col:col + 1])
                    for (ti, sl, pr, pi) in info:
                        s, n = TILES[ti]
                        self.act(tE[sl][:, 0:n], tE[sl][:, 0:n], AF.Sqrt, [btE[sl]], [btE[sl]], scale=-1.0, bias=self.cst[:, 5, 0:1])
                    for (ti, sl, pr, pi) in info:
                        s, n = TILES[ti]
                        self.tt("pool", tB[sl][:, 0:n], tB[sl][:, 0:n], tE[sl][:, 0:n], ALU.mult, [btB[sl], btE[sl]], [btB[sl]])
                        self.tt("dve", tB[sl][:, 0:n], tB[sl][:, 0:n], xc[:, s:s + n], ALU.mult, [btB[sl], bxc], [btB[sl]])
                        if dr == 1:
                            if prev_tile is None:
                                init, rd = 0.0, []
                            else:
                                ps_ = TILES[prev_tile[0]][0]
                                init, rd = hsum[:, ps_:ps_ + 1], [bhs]
                            self.scan(rev(hsum[:, s:s + n]), rev(tD[sl][:, 0:n]), rev(tB[sl][:, 0:n]), init,
                                      [btD[sl], btB[sl]] + rd, [bhs])
                        else:
                            if prev_tile is None:
                                init, rd = 0.0, []
                            else:
                                pn = TILES[prev_tile[0]][1]
                                psl = prev_tile[1]
                                init, rd = tF[psl][:, pn - 1:pn], [btF[psl]]
                            if prev_tile is not None and prev_tile[1] == sl:
                                self.copy("dve", hl[:, 0:1], tF[sl][:, TILES[prev_tile[0]][1] - 1:TILES[prev_tile[0]][1]], [btF[sl]], [bhl])
                                init, rd = hl[:, 0:1], [bhl]
                            self.scan(tF[sl][:, 0:n], tD[sl][:, 0:n], tB[sl][:, 0:n], init, [btD[sl], btB[sl]] + rd, [btF[sl]])
                            if not (last and s < CTX):
                                self.tt("dve", tC[sl][:, 0:n], tF[sl][:, 0:n], hsum[:, s:s + n], ALU.add, [btF[sl], bhs], [btC[sl]])
                                o = self.nxt("ob", 3)
                                self.tt("pool", ob[o][:, 0:n], tC[sl][:, 0:n], gl[:, s:s + n], ALU.mult, [btC[sl], bgl], [bob[o]])
                                self.dma("st", self.d_y[:, 8 + c, s:s + n], ob[o][:, 0:n], [bob[o]], [])
                        prev_tile = (ti, sl)

        self.subflush(f"L{l}_mixlru")
        fes.close()
        NQ = 4
        qA, bqA = self.sbn(es, "qA", [128, 512], F32, NQ)
        qB, bqB = self.sbn(es, "qB", [128, 512], F32, NQ)
        qC, bqC = self.sbn(es, "qC", [128, 512], F32, NQ)
        qD, bqD = self.sbn(es, "qD", [128, 512], F32, NQ)
        qE, bqE = self.sbn(es, "qE", [128, 512], F32, NQ)
        qF, bqF = self.sbn(es, "qF", [128, 512], F32, NQ)
        cs_, bcs = self.sbn(es, "cs4", [128, 512], F32, NQ)
        sn_, bsn = self.sbn(es, "sn4", [128, 512], F32, NQ)
        tA, btA, tB, btB, tC, btC, tD, btD, tE, btE, tF, btF = qA, bqA, qB, bqB, qC, bqC, qD, bqD, qE, bqE, qF, bqF
        qcnt = 0
        jobs = [("q", c) for c in range(4)] + [("k", kv) for kv in range(2)]
        wq, bwq = self.sbn(es, "wq", [128, 8, 128], BF16, 7)
        wsrc = self.i_win[l].rearrange("(k p) n -> p k n", p=128)
        for ji, (kind, c) in enumerate(jobs):
            if kind == "q":
                self.dma("pool", wq[ji][:], wsrc[:, :, 512 + c * 128:512 + (c + 1) * 128], (), [bwq[ji]])
            else:
                for hh in range(2):
                    self.dma("pool", wq[ji][:, :, 64 * hh:64 * hh + 64], wsrc[:, :, 1024 + c * 64:1024 + (c + 1) * 64], (), [bwq[ji]])
        self.dma("pool", wq[6][:], wsrc[:, :, 1152:1280], (), [bwq[6]])
        pend = []

        def stage2(sl, n, gcol):
            def f():
                p2 = self.nxt("pa", 4)
                self.mm(pa[p2][:, 0:n], self.blk64(), tB[sl][:, 0:n], True, True, [self.b_cst, btB[sl]], [bpa[p2]])
                self.act(tC[sl][:, 0:n], pa[p2][:, 0:n], AF.Sqrt, [bpa[p2]], [btC[sl]], bias=self.cst[:, 5, 1:2])
                self.recip(tC[sl][:, 0:n], tC[sl][:, 0:n], [btC[sl]], [btC[sl]])
                self.stt(tD[sl][:, 0:n], tA[sl][:, 0:n], vec[:, gcol:gcol + 1], tC[sl][:, 0:n], ALU.mult, ALU.mult,
                         [btA[sl], btC[sl], bv], [btD[sl]])
            return f

        def stage3(sl, n, s, kind, c, isctx, cq):
            def f():
                if kind == "q":
                    o = self.nxt("ob", 3)
                    dst, bdst = ob[o][:, 0:n], bob[o]
                if isctx:
                    if kind == "q":
                        self.copy("pool", dst, tD[sl][:, 0:n], [btD[sl]], [bdst])
                    else:
                        for hh in range(2):
                            self.copy("pool", self.kd[2 * c + hh][64 * hh:64 * hh + 64, s:s + n], tD[sl][64 * hh:64 * hh + 64, 0:n],
                                      [btD[sl]], [self.b_kd[2 * c + hh]])
                else:
                    p3 = self.nxt("pa", 4)
                    self.mm(pa[p3][:, 0:n], self.perm(), tD[sl][:, 0:n], True, True, [self.b_cst, btD[sl]], [bpa[p3]])
                    self.tt("pool", tE[sl][:, 0:n], tD[sl][:, 0:n], cs_[cq][:, 0:n], ALU.mult, [btD[sl], bcs[cq]], [btE[sl]])
                    self.tt("dve", tF[sl][:, 0:n], pa[p3][:, 0:n], sn_[cq][:, 0:n], ALU.mult, [bpa[p3], bsn[cq]], [btF[sl]])
                    if kind == "q":
                        self.tt("pool", dst, tE[sl][:, 0:n], tF[sl][:, 0:n], ALU.add, [btE[sl], btF[sl]], [bdst])
                    else:
                        for hh in range(2):
                            self.tt("dve" if hh else "pool", self.kd[2 * c + hh][64 * hh:64 * hh + 64, s:s + n], tE[sl][64 * hh:64 * hh + 64, 0:n],
                                    tF[sl][64 * hh:64 * hh + 64, 0:n], ALU.add, [btE[sl], btF[sl]], [self.b_kd[2 * c + hh]])
                if kind == "q":
                    self.dma("st", self.d_q[c, :, s:s + n], dst, [bdst], [])
            return f

        def advance():
            for ent in pend:
                ent[2] += 1
            for ent in pend:
                if ent[2] == 1:
                    ent[0]()
            while pend and pend[0][2] >= 2:
                pend.pop(0)[1]()

        tcnt = 0
        for ti, (s, n) in enumerate(TILES):
            isctx = s < CTX
            hb, bhb = self.load_h(ti)
            cq = tcnt % NQ
            tcnt += 1
            if not isctx:
                self.dma("sp", cs_[cq][:, 0:n], self.i_cos[:, s - CTX:s - CTX + n], (), [bcs[cq]])
                self.dma("sp", sn_[cq][:, 0:n], self.i_sin[:, s - CTX:s - CTX + n], (), [bsn[cq]])
            for ji, (kind, c) in enumerate(jobs):
                if kind == "q" and last and isctx:
                    continue
                gcol = V_QN if kind == "q" else V_KN
                wz, bwz = wq[ji], bwq[ji]
                sl = qcnt % NQ
                qcnt += 1
                p = self.nxt("pz", 3)
                self.zmm(pz[p], bpz[p], wz, bwz, hb, bhb, n)
                self.copy("act", tA[sl][:, 0:n], pz[p][:, 0:n], [bpz[p]], [btA[sl]])
                self.act(tB[sl][:, 0:n], pz[p][:, 0:n], AF.Square, [bpz[p]], [btB[sl]])
                pend.append([stage2(sl, n, gcol), stage3(sl, n, s, kind, c, isctx, cq), 0])
                advance()
            for sub in range(n // 128):
                tc = s // 128 + sub
                p = self.nxt("pa", 4)
                for k in range(8):
                    self.mm(pa[p][:, 0:128], hb[:, k, sub * 128:(sub + 1) * 128], wq[6][:, k, :], k == 0, k == 7, [bhb, bwq[6]], [bpa[p]])
                self.copy("act" if sub % 2 else "dve", self.va[:, tc, :, 0:64], pa[p][:, 0:128].rearrange("p (a b) -> p a b", a=2),
                          [bpa[p]], [self.b_va])
        advance()
        advance()
        assert not pend
        if self.dbg:
            for kv in range(2):
                for hh in range(2):
                    self.dma("sp", self.d_k[kv, 64 * hh:64 * hh + 64, :], self.kd[2 * kv + hh][64 * hh:64 * hh + 64, :], [self.b_kd[2 * kv + hh]], [])
            self.dma("sp", self.d_v, self.va[:].rearrange("p a b c -> p (a b c)"), [self.b_va], [])

    def phase_attn(self, es, l, last):
        qt, bqt = self.sbn(es, "qt", [128, 512], BF16, 3)
        pt, bpt = self.sbn(es, "pt", [128, 512], BF16, 6)
        pss, bpss = self.psn(es, "pss", 4)
        pso, bpso = self.psn(es, "pso", 2)
        psb, bpsb = self.psn(es, "psb", 1)
        rs, brs = self.sbn(es, "rs", [128, 512], F32, 2)
        bc, bbc = self.sbn(es, "bc", [64, 512], F32, 2)
        oa, boa = self.sbn(es, "oa", [64, 512], BF16, 3)
        LAG = 2
        work = [(c, ti) for c in range(4) for ti in range(len(TILES)) if not (last and TILES[ti][0] < CTX)]
        qslot = {}

        def load_q(idx):
            c, ti = work[idx]
            s, n = TILES[ti]
            qs = self.nxt("qt", 3)
            self.dma("sp", qt[qs][:, 0:n], self.d_q[c, :, s:s + n], (), [bqt[qs]])
            qslot[idx] = qs

        pending = []

        def make_pv(c, kv, s, n, j, kc, nkc, po, pp):
            def pv():
                self.mm(pso[po][0:65, 0:n], self.va[:, kc, kv, 0:65], pt[pp][:, 0:n], kc == 0, kc == nkc - 1,
                        [self.b_va, bpt[pp]], [bpso[po]])
                if kc == nkc - 1:
                    r = self.nxt("rs", 2)
                    self.recip(rs[r][64:65, 0:n], pso[po][64:65, 0:n], [bpso[po]], [brs[r]])
                    self.mm(psb[0][0:64, 0:n], self.ones()[64:65, 0:64], rs[r][64:65, 0:n], True, True,
                            [self.b_cst, brs[r]], [bpsb[0]])
                    self.copy("act", bc[r][:, 0:n], psb[0][0:64, 0:n], [bpsb[0]], [bbc[r]])
                    o = self.nxt("oa", 3)
                    self.tt("dve", oa[o][:, 0:n], pso[po][0:64, 0:n], bc[r][:, 0:n], ALU.mult, [bpso[po], bbc[r]], [boa[o]])
                    self.dma("st", self.d_y[64 * j:64 * j + 64, 4 + c, s:s + n], oa[o][:, 0:n], [boa[o]], [])
            return pv

        load_q(0)
        for idx, (c, ti) in enumerate(work):
            if idx + 1 < len(work):
                load_q(idx + 1)
            kv = c // 2
            s, n = TILES[ti]
            nkc = 2 if s < CTX else 34
            qs = qslot[idx]
            for j in range(2):
                po = self.nxt("pso", 2)
                for kc in range(nkc):
                    p = self.nxt("pss", 4)
                    self.mm(pss[p][:, 0:n], self.kd[2 * kv + j][:, kc * 128:(kc + 1) * 128],
                            qt[qs][:, 0:n], True, True, [self.b_kd[2 * kv + j], bqt[qs]], [bpss[p]])
                    pp = self.nxt("pt", 6)
                    self.act(pt[pp][:, 0:n], pss[p][:, 0:n], AF.Exp, [bpss[p], self.b_nbias], [bpt[pp]],
                             scale=0.125, bias=self.nbias[:, 0:1])
                    pending.append(make_pv(c, kv, s, n, j, kc, nkc, po, pp))
                    if len(pending) > LAG:
                        pending.pop(0)()
        while pending:
            pending.pop(0)()

    def ln_fm_stages(self, r, br, n, gcol, bcol, outs, bouts, psA, bpsA, psB, bpsB, tmp):
        vec, bv = self.vec, self.b_vec
        sq, bsq, mu, bmu, rstd, brstd = tmp
        if not isinstance(br, (list, tuple)):
            br = [br] * 8
        if not isinstance(bouts, (list, tuple)):
            bouts = [bouts] * 8
        st = []

        def s1():
            for k in range(8):
                self.mm(psA[:, 0:n], self.onesD(), r[k], k == 0, k == 7, [self.b_cst, br[k]], [bpsA])
            for k in range(8):
                s = k % 2
                self.act(sq[s][:, 0:n], r[k], AF.Square, [br[k]], [bsq[s]])
                self.mm(psB[:, 0:n], self.onesD(), sq[s][:, 0:n], k == 0, k == 7, [self.b_cst, bsq[s]], [bpsB])
        st.append(s1)

        def s2():
            self.copy("act", mu[:, 0:n], psA[:, 0:n], [bpsA], [bmu])
            self.act(sq[0][:, 0:n], psA[:, 0:n], AF.Square, [bpsA], [bsq[0]])
            self.tt("dve", rstd[:, 0:n], psB[:, 0:n], sq[0][:, 0:n], ALU.subtract, [bpsB, bsq[0]], [brstd])
            self.act(rstd[:, 0:n], rstd[:, 0:n], AF.Sqrt, [brstd], [brstd], bias=self.cst[:, 5, 2:3])
            self.recip(rstd[:, 0:n], rstd[:, 0:n], [brstd], [brstd])
        st.append(s2)

        def mk(k):
            def f():
                s = k % 2
                self.tt("pool", sq[s][:, 0:n], r[k], mu[:, 0:n], ALU.subtract, [br[k], bmu], [bsq[s]])
                self.tt("dve", sq[s][:, 0:n], sq[s][:, 0:n], rstd[:, 0:n], ALU.mult, [bsq[s], brstd], [bsq[s]])
                self.act(outs[k], sq[s][:, 0:n], AF.Identity, [bsq[s], bv], [bouts[k]],
                         scale=vec[:, gcol + k:gcol + k + 1], bias=vec[:, bcol + k:bcol + k + 1])
            return f
        for k in range(8):
            st.append(mk(k))
        return st

    def ln_fm(self, *a):
        for f in self.ln_fm_stages(*a):
            f()

    def phase_merge(self, es, l, last):
        vec, bv = self.vec, self.b_vec
        wbr, bwbr = self.sb(es, "wbr", [128, 12, 1024], BF16)
        wo, bwo = self.sb(es, "wo", [128, 8, 1024], BF16)
        wgz, bwgz = self.sb(es, "wgz", [128, 8, 3072], BF16)
        xt, bxt = self.sb(es, "mxt", [128, 8, 512], F32)
        ht, bht = self.sb(es, "mht", [128, 8, 512], BF16)
        yt, byt = self.sb(es, "myt", [128, 12, 512], BF16)
        mg, bmg = self.sb(es, "mmg", [128, 8, 512], BF16)
        h2b, bh2b = self.sb(es, "h2b", [128, 8, 512], BF16)
        gs, bgs = self.sbn(es, "gs", [128, 512], F32, 3)
        pr, bpr = self.sbn(es, "pr", [128, 512], F32, 3)
        sq, bsq = self.sbn(es, "lsq", [128, 512], F32, 2)
        mu, bmu = self.sb(es, "lmu", [128, 512], F32)
        rstd, brstd = self.sb(es, "lrs", [128, 512], F32)
        tt_, btt = self.sbn(es, "mtt", [128, 512], F32, 2)
        pg, bpg = self.psn(es, "pg", 2)
        pb, bpb = self.psn(es, "pb", 2)
        po, bpo = self.psn(es, "po", 2)
        pl, bpl = self.psn(es, "pl", 2)
        if last:
            gTt, bgTt = self.sbn(es, "gTt", [8, 512], F32, 2)
            h2f, bh2f = self.sbn(es, "h2f", [128, 512], F32, 2)
            rt, brt = self.sb(es, "rt", [128, 8, 8], F32)
            lgT, blgT = self.sb(es, "lgT", [8, 512], F32)
            L, bL = self.sb(es, "L", [128, 4, 8], F32)
            sm, bsm = self.sb(es, "sm", [128, 64], F32)
            E1, bE1 = self.sb(es, "E1", [128, 4, 8], F32)
            E2, bE2 = self.sb(es, "E2", [128, 4, 8], F32)
            L2, bL2 = self.sb(es, "L2", [128, 4, 8], F32)
            self.dma("sp", rt[:], self.i_router.rearrange("(k p) e -> p k e", p=128), (), [brt])
        wsrc = self.i_win[l].rearrange("(k p) n -> p k n", p=128)
        for cc in range(12):
            self.dma("pool", wbr[:, cc, :], self.i_wbr[l, cc * 128:(cc + 1) * 128, :], (), [bwbr])
        for k in range(8):
            self.dma("pool", wo[:, k, :], self.i_wout[l, k * 128:(k + 1) * 128, :], (), [bwo])
        for k in range(8):
            for hh in range(2):
                self.dma("pool", wgz[:, k, hh * 1536:(hh + 1) * 1536], wsrc[:, k, 2304 + hh * 1536:2304 + (hh + 1) * 1536], (), [bwgz])
        nxt_ = 1 if last else 2
        if nxt_ == 2:
            xt2, _ = self.sb(es, "mxt2", [128, 8, 512], F32)
            xts = [xt, xt2]
        else:
            xts = [xt]
        bxtk = [[Buf(f"xt{i}_{k}") for k in range(8)] for i in range(nxt_)]
        pending = []

        def make_post(xt, bxk, s, n, w):
            r = [xt[:, k, 0:n] for k in range(8)]
            st = self.ln_fm_stages(r, bxk, n, V_LN1G, V_LN1B, r, bxk, pl[0], bpl[0], pl[1], bpl[1], (sq, bsq, mu, bmu, rstd, brstd))
            st.append(lambda: self.dma("st", self.d_x1[:, :, s:s + n], xt[:, :, 0:n], bxk, []))
            state = {}

            def h2k(k):
                def f():
                    if last:
                        if k == 0:
                            state["a"] = self.nxt("pl", 2)
                        a = state["a"]
                        hf = self.nxt("h2f", 2)
                        self.ts("dve", h2f[hf][:, 0:n], xt[:, k, 0:n], self.modp[:, 8 + k, w:w + 1], ALU.mult,
                                [bxk[k], self.b_mod, self.b_modp], [bh2f[hf]], s2=self.mod[:, 24 + k, w:w + 1], op1=ALU.add)
                        self.copy("pool", h2b[:, k, 0:n], h2f[hf][:, 0:n], [bh2f[hf]], [bh2b])
                        self.mm(pl[a][0:8, 0:n], rt[:, k, :], h2f[hf][:, 0:n], k == 0, k == 7, [brt, bh2f[hf]], [bpl[a]])
                    else:
                        self.ts("dve" if k % 2 else "pool", h2b[:, k, 0:n], xt[:, k, 0:n], self.modp[:, 8 + k, w:w + 1], ALU.mult,
                                [bxk[k], self.b_mod, self.b_modp], [bh2b], s2=self.mod[:, 24 + k, w:w + 1], op1=ALU.add)
                return f
            for k in range(8):
                st.append(h2k(k))
            st.append(lambda: self.dma("st", self.d_h2[:, :, s:s + n], h2b[:, :, 0:n], [bh2b], []))
            if last:
                def router():
                    a = state["a"]
                    self.copy("act", lgT[:, 0:n], pl[a][0:8, 0:n], [bpl[a]], [blgT])
                    b_ = self.nxt("pl", 2)
                    for sub in range(4):
                        self.tr(pl[b_][:, sub * 8:(sub + 1) * 8], lgT[0:8, sub * 128:(sub + 1) * 128], self.cst[0:8, 0, 0:8],
                                [blgT, self.b_cst], [bpl[b_]])
                    self.tt("dve", L[:], pl[b_][:, 0:32].rearrange("p (a b) -> p a b", a=4),
                            bass.AP(vec[:, V_RB:V_RB + 8].tensor, vec[:, V_RB:V_RB + 8].offset, [list(vec[:, V_RB:V_RB + 8].ap[0]), [0, 4], [1, 8]]),
                            ALU.add, [bpl[b_], bv], [bL])
                    m1, m2_, d_, e_, p1, p2 = (sm[:, 0:4], sm[:, 4:8], sm[:, 8:12], sm[:, 12:16], sm[:, 16:20], sm[:, 20:24])
                    self.rmax(m1, L[:], [bL], [bsm])
                    self.tt("dve", E1[:], L[:], bcast_last(m1, 8), ALU.is_equal, [bL, bsm], [bE1])
                    self.stt(L2[:], E1[:], -1e30, L[:], ALU.mult, ALU.add, [bE1, bL], [bL2])
                    self.rmax(m2_, L2[:], [bL2], [bsm])
                    self.tt("dve", E2[:], L2[:], bcast_last(m2_, 8), ALU.is_equal, [bL2, bsm], [bE2])
                    self.tt("dve", d_, m2_, m1, ALU.subtract, [bsm], [bsm])
                    self.act(e_, d_, AF.Exp, [bsm], [bsm])
                    self.ts("dve", p1, e_, 1.0, ALU.add, [bsm], [bsm])
                    self.recip(p1, p1, [bsm], [bsm])
                    self.tt("dve", p2, e_, p1, ALU.mult, [bsm], [bsm])
                    self.tt("dve", E1[:], E1[:], bcast_last(p1, 8), ALU.mult, [bE1, bsm], [bE1])
                    self.tt("dve", E2[:], E2[:], bcast_last(p2, 8), ALU.mult, [bE2, bsm], [bE2])
                    self.tt("dve", E1[:], E1[:], E2[:], ALU.add, [bE1, bE2], [bE1])
                    c_ = self.nxt("pl", 2)
                    for sub in range(4):
                        self.tr(pl[c_][0:8, sub * 128:(sub + 1) * 128], E1[:, sub, :], self.ident(), [bE1, self.b_cst], [bpl[c_]])
                    gs_ = self.nxt("gTt", 2)
                    self.copy("act", gTt[gs_][:, 0:n], pl[c_][0:8, 0:n], [bpl[c_]], [bgTt[gs_]])
                    self.dma("st", self.d_gate[:, s - CTX:s - CTX + n], gTt[gs_][:, 0:n], [bgTt[gs_]], [])
                st.append(router)
            return st

        tnum = 0
        for ti, (s, n) in enumerate(TILES):
            isctx = s < CTX
            if isctx and last:
                continue
            w = 1 if isctx else 0
            xi = tnum % nxt_
            tnum += 1
            xt, bxk = xts[xi], bxtk[xi]
            self.dma("sp", ht[:, :, 0:n], self.d_h[:, :, s:s + n], (), [bht])
            self.dma("sp", yt[:, :, 0:n], self.d_y[:, :, s:s + n], (), [byt])
            if nxt_ == 2:
                self.dma("sp", xt[:, :, 0:n], self.xsrc(l, s, n), (), bxk)
            for j in range(8):
                prods = []
                for nb in range(3):
                    a = self.nxt("pg", 2)
                    for k in range(8):
                        self.mm(pg[a][:, 0:n], wgz[:, k, nb * 1024 + j * 128:nb * 1024 + (j + 1) * 128], ht[:, k, 0:n], k == 0, k == 7, [bwgz, bht], [bpg[a]])
                    g_ = self.nxt("gs", 3)
                    self.act(gs[g_][:, 0:n], pg[a][:, 0:n], AF.Sigmoid, [bpg[a], bv], [bgs[g_]],
                             bias=vec[:, V_BMERGE + nb * 8 + j:V_BMERGE + nb * 8 + j + 1])
                    b_ = self.nxt("pb", 2)
                    for cc in range(4):
                        self.mm(pb[b_][:, 0:n], wbr[:, nb * 4 + cc, j * 128:(j + 1) * 128], yt[:, nb * 4 + cc, 0:n],
                                cc == 0, cc == 3, [bwbr, byt], [bpb[b_]])
                    p_ = self.nxt("pr", 3)
                    self.tt("dve", pr[p_][:, 0:n], pb[b_][:, 0:n], gs[g_][:, 0:n], ALU.mult, [bpb[b_], bgs[g_]], [bpr[p_]])
                    prods.append((pr[p_], bpr[p_]))
                self.tt("pool", prods[0][0][:, 0:n], prods[0][0][:, 0:n], prods[1][0][:, 0:n], ALU.add,
                        [prods[0][1], prods[1][1]], [prods[0][1]])
                self.tt("dve", mg[:, j, 0:n], prods[0][0][:, 0:n], prods[2][0][:, 0:n], ALU.add, [prods[0][1], prods[2][1]], [bmg])
                npop = -(-len(pending) // (8 - j))
                for _ in range(npop):
                    pending.pop(0)()
            assert not pending
            if nxt_ == 1:
                self.dma("sp", xt[:, :, 0:n], self.xsrc(l, s, n), (), bxk)
            for j2 in range(8):
                a = self.nxt("po", 2)
                for j in range(8):
                    self.mm(po[a][:, 0:n], wo[:, j, j2 * 128:(j2 + 1) * 128], mg[:, j, 0:n], j == 0, j == 7, [bwo, bmg], [bpo[a]])
                t_ = self.nxt("mtt", 2)
                self.act(tt_[t_][:, 0:n], po[a][:, 0:n], AF.Copy, [bpo[a], self.b_mod], [btt[t_]], scale=self.mod[:, 16 + j2, w:w + 1])
                self.stt(xt[:, j2, 0:n], xt[:, j2, 0:n], ALPHA, tt_[t_][:, 0:n], ALU.mult, ALU.add, [bxk[j2], btt[t_]], [bxk[j2]])
            pending.extend(make_post(xt, bxk, s, n, w))
        while pending:
            pending.pop(0)()

    def phase_ffn(self, es, l, last):
        vec, bv = self.vec, self.b_vec
        moe = (l % 2 == 1)
        assert not moe, "dense-evaluated MoE path removed (weights are host-laid-out for the sparse path)"
        nF = (D_EXP if moe else D_FF) // 128
        nE = N_EXP if moe else 1
        if last:
            sts = [(CTX + 1024 * i, 1024) for i in range(4)]
        else:
            sts = [(0, 256)] + [(CTX + 1024 * i, 1024) for i in range(4)]
        if moe:
            self.gT, self.b_gT = self.sb(es, "gT3", [8, SEQ], F32)
            self.dma("sp", self.gT[:], self.d_gate, (), [self.b_gT])
        h2, bh2 = self.sb(es, "fh2", [128, 8, 1024], BF16)
        A, bA = self.sb(es, "fA", [128, nF, 1024], BF16)
        acc, _ = self.sb(es, "facc", [128, 8, 1024], F32)
        bacck = [Buf(f"facc{k}") for k in range(8)]
        fpend = []
        wgu, bwgu = self.sbn(es, "wgu", [128, 8, 2, 512], BF16, 2)
        wd, bwd = self.sbn(es, "wd", [128, nF, 128], BF16, 2)
        sg, bsg = self.sbn(es, "sg", [128, 512], F32, 2)
        gb, bgb = self.sbn(es, "gb", [128, 1024], F32, 2)
        x1, bx1 = self.sbn(es, "fx1", [128, 1024], F32, 2)
        tmp, btmp = self.sbn(es, "ftmp", [128, 512], F32, 2)
        sq, bsq = self.sbn(es, "fsq", [128, 512], F32, 2)
        mu, bmu = self.sb(es, "fmu", [128, 512], F32)
        rstd, brstd = self.sb(es, "frs", [128, 512], F32)
        pG, bpG = self.psn(es, "pG", 2)
        pU, bpU = self.psn(es, "pU", 2)
        pY, bpY = self.psn(es, "pY", 2)
        pL, bpL = self.psn(es, "pL", 2)
        for (s, N) in sts:
            isctx = s < CTX
            w = 1 if isctx else 0
            subs = [(o, min(512, N - o)) for o in range(0, N, 512)]
            self.dma("sp", h2[:, :, 0:N], self.d_h2[:, :, s:s + N], (), [bh2])
            for e in range(nE):
                if moe:
                    Wg, Wu, Wd = self.i_mg[e], self.i_mu[e], self.i_md[e]
                else:
                    Wg, Wu, Wd = self.i_fg, self.i_fu, self.i_fd
                Wg = Wg.rearrange("(k p) f -> p k f", p=128)
                Wu = Wu.rearrange("(k p) f -> p k f", p=128)
                Wd = Wd.rearrange("(g p) n -> p g n", p=128)
                if moe:
                    g_ = self.nxt("gb", 2)
                    for (o, n) in subs:
                        a = self.nxt("pL", 2)
                        self.mm(pL[a][:, 0:n], self.sel[0:8, e, :], self.gT[0:8, s - CTX + o:s - CTX + o + n], True, True,
                                [self.b_sel, self.b_gT], [bpL[a]])
                        self.copy("act", gb[g_][:, o:o + n], pL[a][:, 0:n], [bpL[a]], [bgb[g_]])
                for g0 in range(0, nF, 4):
                    ng = min(4, nF - g0)
                    ws = self.nxt("wgu", 2)
                    self.dma("pool", wgu[ws][:, :, 0, 0:ng * 128], Wg[:, :, g0 * 128:(g0 + ng) * 128], (), [bwgu[ws]])
                    self.dma("pool", wgu[ws][:, :, 1, 0:ng * 128], Wu[:, :, g0 * 128:(g0 + ng) * 128], (), [bwgu[ws]])
                    for fi in range(ng):
                        fg = g0 + fi
                        for (o, n) in subs:
                            a = self.nxt("pG", 2)
                            for k in range(8):
                                self.mm(pG[a][:, 0:n], wgu[ws][:, k, 0, fi * 128:(fi + 1) * 128], h2[:, k, o:o + n], k == 0, k == 7, [bwgu[ws], bh2], [bpG[a]])
                            b_ = self.nxt("pU", 2)
                            for k in range(8):
                                self.mm(pU[b_][:, 0:n], wgu[ws][:, k, 1, fi * 128:(fi + 1) * 128], h2[:, k, o:o + n], k == 0, k == 7, [bwgu[ws], bh2], [bpU[b_]])
                            s_ = self.nxt("sg", 2)
                            self.act(sg[s_][:, 0:n], pG[a][:, 0:n], AF.Silu, [bpG[a]], [bsg[s_]])
                            self.tt("dve", A[:, fg, o:o + n], pU[b_][:, 0:n], sg[s_][:, 0:n], ALU.mult, [bpU[b_], bsg[s_]], [bA])
                        if fpend:
                            for _ in range(-(-len(fpend) // max(1, (nF - 2 - fg)))):
                                if fpend:
                                    fpend.pop(0)()
                while fpend:
                    fpend.pop(0)()
                for j2 in range(8):
                    ds = self.nxt("wd", 2)
                    self.dma("pool", wd[ds][:], Wd[:, :, j2 * 128:(j2 + 1) * 128], (), [bwd[ds]])
                    for (o, n) in subs:
                        a = self.nxt("pY", 2)
                        for fg in range(nF):
                            self.mm(pY[a][:, 0:n], wd[ds][:, fg, :], A[:, fg, o:o + n], fg == 0, fg == nF - 1, [bwd[ds], bA], [bpY[a]])
                        if not moe:
                            self.copy("act", acc[:, j2, o:o + n], pY[a][:, 0:n], [bpY[a]], [bacck[j2]])
                        elif e == 0:
                            self.tt("dve", acc[:, j2, o:o + n], pY[a][:, 0:n], gb[g_][:, o:o + n], ALU.mult, [bpY[a], bgb[g_]], [bacc])
                        else:
                            t_ = self.nxt("ftmp", 2)
                            self.tt("dve", tmp[t_][:, 0:n], pY[a][:, 0:n], gb[g_][:, o:o + n], ALU.mult, [bpY[a], bgb[g_]], [btmp[t_]])
                            self.tt("pool", acc[:, j2, o:o + n], acc[:, j2, o:o + n], tmp[t_][:, 0:n], ALU.add, [bacc, btmp[t_]], [bacc])
            fpend.extend(self.ffn_epilogue_stages(s, N, subs, w, last, acc, bacck, x1, bx1, pL, bpL, (sq, bsq, mu, bmu, rstd, brstd)))
        while fpend:
            fpend.pop(0)()

    def ffn_epilogue_stages(self, s, N, subs, w, last, acc, bacc, x1, bx1, pL, bpL, lnt):
        if not isinstance(bacc, (list, tuple)):
            bacc = [bacc] * 8
        st = []

        def res(j2):
            def f():
                xs_ = self.nxt("fx1", 2)
                self.dma("sp", x1[xs_][:, 0:N], self.d_x1[:, j2, s:s + N], (), [bx1[xs_]])
                self.ts("dve", acc[:, j2, 0:N], acc[:, j2, 0:N], self.mod[:, 40 + j2, w:w + 1], ALU.mult, [bacc[j2], self.b_mod], [bacc[j2]])
                self.stt(acc[:, j2, 0:N], x1[xs_][:, 0:N], ALPHA, acc[:, j2, 0:N], ALU.mult, ALU.add, [bx1[xs_], bacc[j2]], [bacc[j2]])
            return f
        for j2 in range(8):
            st.append(res(j2))
        for (o, n) in subs:
            r = [acc[:, k, o:o + n] for k in range(8)]
            st.extend(self.ln_fm_stages(r, bacc, n, V_LN2G, V_LN2B, r, bacc, pL[0], bpL[0], pL[1], bpL[1], lnt))

        def store():
            if last:
                self.dma("st", self.d_out[:, :, s - CTX:s - CTX + N], acc[:, :, 0:N], bacc, [])
            else:
                self.dma("st", self.d_xs1[:, :, s:s + N], acc[:, :, 0:N], bacc, [])
        st.append(store)
        return st

    def ffn_epilogue(self, *a):
        for f in self.ffn_epilogue_stages(*a):
            f()

    def phase_route(self, es, l, last):
        gT, bgT = self.sb(es, "gT2", [8, SEQ], F32)
        self.dma("sp", gT[:], self.d_gate, (), [bgT])
        ones8, bo8 = self.sb(es, "ones8", [8, SEQ], F32)
        selT, bsel = self.sb(es, "selT", [8, SEQ], F32)
        incl, binc = self.sb(es, "incl", [8, SEQ], F32)
        dst, bdst = self.sb(es, "dstT", [8, SEQ], F32)
        mc, bmc = self.sb(es, "mc", [8, 64], F32)
        mc2, bmc2 = self.sb(es, "mc2", [128, 32], F32)
        sm, bsm = self.sb(es, "rsm", [8, 64], F32)
        ebf, bebf = self.sb(es, "ebf", [128, NBLK], F32)
        tf, btf = self.sb(es, "rtf", [128, NBLK * 28], F32)
        GT, bGT = self.sb(es, "GTk", [128, 32, 8], F32)
        DT, bDT = self.sb(es, "DTk", [128, 32, 8], F32)
        E1, bE1 = self.sb(es, "rE1", [128, 32, 8], F32)
        E2, bE2 = self.sb(es, "rE2", [128, 32, 8], F32)
        TM, bTM = self.sb(es, "rTM", [128, 32, 8], F32)
        r32, br32 = self.sb(es, "r32", [128, 4, 32], F32)
        ps, bps = self.psn(es, "rps", 3)
        self.dma("sp", mc[:], self.i_mc, (), [bmc])
        self.dma("sp", mc2[:], self.i_mc2, (), [bmc2])
        self.memset("dve", ones8[:], 1.0, [bo8])
        self.ts("dve", selT[:], gT[:], 0.0, ALU.is_gt, [bgT], [bsel])
        self.scan(incl[:], ones8[:], selT[:], 0.0, [bo8, bsel], [binc])
        cnt = incl[:, SEQ - 1:SEQ]
        self.ts("dve", sm[:, 0:8], mc[:, 0:8], cnt, ALU.is_lt, [bmc, binc], [bsm])
        self.S.add("dve", lambda e: e.tensor_reduce(out=sm[:, 8:9], in_=sm[:, 0:8], op=ALU.add, axis=AX.X), [bsm], [bsm])
        self.copy("dve", sm[:, 9:10], sm[:, 8:9], [bsm], [bsm])
        self.mm(ps[0][0:8, 0:2], mc[:, 32:40], sm[:, 8:10], True, True, [bmc, bsm], [bps[0]])
        self.ts("dve", sm[:, 10:11], ps[0][0:8, 0:1], float(MB), ALU.mult, [bps[0]], [bsm])
        self.stt(sm[:, 11:12], sm[:, 8:9], float(MB), sm[:, 10:11], ALU.mult, ALU.add, [bsm], [bsm])
        self.tt("dve", dst[:], incl[:], selT[:], ALU.subtract, [binc, bsel], [bdst])
        self.ts("dve", dst[:], dst[:], sm[:, 10:11], ALU.add, [bdst, bsm], [bdst])
        self.ts("dve", sm[:, 16:16 + NBLK], mc[:, 8:8 + NBLK], sm[:, 11:12], ALU.is_ge, [bmc, bsm], [bsm])
        self.mm(ps[1][:, 0:NBLK], self.ones()[0:8, :], sm[:, 16:16 + NBLK], True, True, [self.b_cst, bsm], [bps[1]])
        self.ts("dve", ebf[:], ps[1][:, 0:NBLK], 7.0, ALU.min, [bps[1]], [bebf])
        self.ts("dve", ebf[:], ebf[:], 2048.0, ALU.mult, [bebf, bmc2], [bebf], s2=mc2[:, 0:1], op1=ALU.add)
        m2ap = mc2[:, 2:18]
        self.tt("dve", tf[:, 0:NBLK * 16].rearrange("p (b f) -> p b f", f=16), bcast_last(ebf[:], 16),
                bass.AP(m2ap.tensor, m2ap.offset, [list(m2ap.ap[0]), [0, NBLK], [1, 16]]), ALU.add, [bebf, bmc2], [btf])
        self.copy("dve", self.IG[:].rearrange("p b f -> p (b f)"), tf[:, 0:NBLK * 16], [btf], [self.b_IG])
        for c in range(32):
            self.tr(ps[2][:, c * 8:(c + 1) * 8], gT[0:8, c * 128:(c + 1) * 128], self.cst[0:8, 0, 0:8], [bgT, self.b_cst], [bps[2]])
        self.copy("act", GT[:].rearrange("p a b -> p (a b)"), ps[2][:, 0:256], [bps[2]], [bGT])
        for c in range(32):
            self.tr(ps[0][:, c * 8:(c + 1) * 8], dst[0:8, c * 128:(c + 1) * 128], self.cst[0:8, 0, 0:8], [bdst, self.b_cst], [bps[0]])
        self.copy("act", DT[:].rearrange("p a b -> p (a b)"), ps[0][:, 0:256], [bps[0]], [bDT])
        m1 = r32[:, 0, :]
        self.rmax(m1, GT[:], [bGT], [br32])
        self.tt("dve", E1[:], GT[:], bcast_last(m1, 8), ALU.is_equal, [bGT, br32], [bE1])
        self.ts("dve", E2[:], GT[:], 0.0, ALU.is_gt, [bGT], [bE2])
        self.tt("dve", E2[:], E2[:], E1[:], ALU.subtract, [bE2, bE1], [bE2])
        rsum = lambda out, in_, rd: self.S.add("dve", lambda e: e.tensor_reduce(out=out, in_=in_, op=ALU.add, axis=AX.X), rd, [br32])
        self.copy("dve", self.P12[:, 0, :], m1, [br32], [self.b_P12])
        self.tt("dve", TM[:], GT[:], E2[:], ALU.mult, [bGT, bE2], [bTM])
        rsum(r32[:, 1, :], TM[:], [bTM])
        self.copy("dve", self.P12[:, 1, :], r32[:, 1, :], [br32], [self.b_P12])
        self.tt("dve", TM[:], DT[:], E1[:], ALU.mult, [bDT, bE1], [bTM])
        rsum(r32[:, 2, :], TM[:], [bTM])
        self.copy("dve", self.D12[:, 0, :], r32[:, 2, :], [br32], [self.b_D12])
        self.tt("dve", TM[:], DT[:], E2[:], ALU.mult, [bDT, bE2], [bTM])
        rsum(r32[:, 3, :], TM[:], [bTM])
        self.copy("dve", self.D12[:, 1, :], r32[:, 3, :], [br32], [self.b_D12])
        if self.dbg:
            dd = self.nc.dram_tensor("dbg_D12", [128, 64], I32, kind="ExternalOutput").ap()
            dg = self.nc.dram_tensor("dbg_IG", [128, 16 * NBLK], I32, kind="ExternalOutput").ap()
            dp = self.nc.dram_tensor("dbg_P12", [128, 64], F32, kind="ExternalOutput").ap()
            ds = self.nc.dram_tensor("dbg_sm", [8, 64], F32, kind="ExternalOutput").ap()
            self.dma("sp", dd, self.D12[:].rearrange("p a b -> p (a b)"), [self.b_D12], [])
            self.dma("sp", dg, self.IG[:].rearrange("p a b -> p (a b)"), [self.b_IG], [])
            self.dma("sp", dp, self.P12[:].rearrange("p a b -> p (a b)"), [self.b_P12], [])
            self.dma("sp", ds, sm[:], [bsm], [])

    def phase_blocks(self, es, l, last):
        nF = D_EXP // 128
        self.st_dve_q = "sp"
        idb, bidb = self.sb(es, "idb", [128, 128], BF16)
        rows, brows = self.sbn(es, "rows", [128, 1024], BF16, 2)
        XT, bXT = self.sbn(es, "XT", [128, 8, MB], BF16, 2)
        A, bA = self.sb(es, "bA", [128, nF, MB], BF16)
        wg, bwg = self.sbn(es, "bwg", [128, 8 * 896], BF16, 2)
        wu, bwu = self.sbn(es, "bwu", [128, 8 * 896], BF16, 2)
        wd, bwd = self.sbn(es, "bwd", [128, nF * 256], BF16, 2)
        Yr, bYr = self.sbn(es, "Yr", [128, 4, 1024], F32, 1)
        h4, bh4 = XT, bXT
        sg, bsg = self.sbn(es, "bsg", [128, MB], F32, 2)
        ptb, bptb = self.psn(es, "ptb", 2, shape=(128, 1024), dt=BF16)
        pG, bpG = self.psn(es, "bpG", 2)
        pU, bpU = self.psn(es, "bpU", 2)
        pY, bpY = self.psn(es, "bpY", 2)
        self.copy("dve", idb[:], self.ident(), [self.b_cst], [bidb])
        bxs = [Buf(f"xs{c}") for c in range(32)]

        def gather(dst_ap, src, idx, reads, writes):
            return self.S.add("pool", lambda e: e.indirect_dma_start(out=dst_ap, out_offset=None, in_=src,
                                                                     in_offset=IndirectOffsetOnAxis(ap=idx, axis=0)),
                              reads, writes, dma=True)

        wslot = {}

        def load_w1(b, cg):
            ws = self.nxt("bwg", 2)
            for kp in range(4):
                idx = self.IG[:, b, cg * 4 + kp:cg * 4 + kp + 1].bitcast(U32)
                gather(wg[ws][:, kp * 1792:(kp + 1) * 1792], self.i_mg, idx, [self.b_IG], [bwg[ws]])
                gather(wu[ws][:, kp * 1792:(kp + 1) * 1792], self.i_mu, idx, [self.b_IG], [bwu[ws]])
            wslot[(b, cg)] = ws

        load_w1(0, 0)
        load_w1(0, 1)
        for g4 in range(8):
            hs = g4 % 2
            self.dma("sp", h4[hs][:], self.d_h2[:, :, CTX + g4 * 512:CTX + (g4 + 1) * 512], (), [bh4[hs]])
            for c4 in range(4):
                c = g4 * 4 + c4
                pp = self.nxt("ptb", 2)
                for k in range(8):
                    self.tr(ptb[pp][:, k * 128:(k + 1) * 128], h4[hs][:, k, c4 * 128:(c4 + 1) * 128], idb[:], [bh4[hs], bidb], [bptb[pp]])
                rs_ = self.nxt("rows", 2)
                self.copy("act" if c % 2 else "dve", rows[rs_][:], ptb[pp][:], [bptb[pp]], [brows[rs_]])
                for t2 in range(2):
                    idx = self.D12[:, t2, c:c + 1].bitcast(U32)
                    self.S.add("pool", lambda e, idx=idx, src=rows[rs_]: e.indirect_dma_start(
                        out=self.d_xs, out_offset=IndirectOffsetOnAxis(ap=idx, axis=0), in_=src[:], in_offset=None),
                        [brows[rs_], self.b_D12], [bxs[c]], dma=True)
        bys = Buf("ys")
        for b in range(NBLK_RUN if BLK_STAGE > 0 else 0):
            xs_ = b % 2
            for c4 in range(4):
                rs_ = self.nxt("rows", 2)
                self.dma("sp", rows[rs_][:], self.d_xs[b * MB + c4 * 128:b * MB + (c4 + 1) * 128, :], bxs, [brows[rs_]])
                pp = self.nxt("ptb", 2)
                for k in range(8):
                    self.tr(ptb[pp][:, k * 128:(k + 1) * 128], rows[rs_][:, k * 128:(k + 1) * 128], idb[:], [brows[rs_], bidb], [bptb[pp]])
                self.copy("act" if c4 % 2 else "dve", XT[xs_][:, :, c4 * 128:(c4 + 1) * 128],
                          ptb[pp][:].rearrange("p (k t) -> p k t", k=8), [bptb[pp]], [bXT[xs_]])
            for cg in range(4 if BLK_STAGE > 1 else 0):
                if (b, cg) not in wslot:
                    load_w1(b, cg)
                ws = wslot[(b, cg)]
                for f7 in range(7):
                    fg = cg * 7 + f7
                    a = self.nxt("bpG", 2)
                    for k in range(8):
                        self.mm(pG[a][:, 0:MB], wg[ws][:, k * 896 + f7 * 128:k * 896 + (f7 + 1) * 128], XT[xs_][:, k, :], k == 0, k == 7, [bwg[ws], bXT[xs_]], [bpG[a]])
                    b_ = self.nxt("bpU", 2)
                    for k in range(8):
                        self.mm(pU[b_][:, 0:MB], wu[ws][:, k * 896 + f7 * 128:k * 896 + (f7 + 1) * 128], XT[xs_][:, k, :], k == 0, k == 7, [bwu[ws], bXT[xs_]], [bpU[b_]])
                    s_ = self.nxt("bsg", 2)
                    self.act(sg[s_][:], pG[a][:, 0:MB], AF.Silu, [bpG[a]], [bsg[s_]])
                    self.tt("dve", A[:, fg, :], pU[b_][:, 0:MB], sg[s_][:], ALU.mult, [bpU[b_], bsg[s_]], [bA])
            ys_ = 0
            for dq in range(4 if BLK_STAGE > 2 else 0):
                ds_ = self.nxt("bwd", 2)
                for fq in range(4):
                    idx = self.IG[:, b, dq * 4 + fq:dq * 4 + fq + 1].bitcast(U32)
                    gather(wd[ds_][:, fq * 1792:(fq + 1) * 1792], self.i_md, idx, [self.b_IG], [bwd[ds_]])
                for c4 in range(4):
                    a = self.nxt("bpY", 2)
                    for fg in range(nF):
                        self.mm(pY[a][:, 0:256], A[:, fg, c4 * 128:(c4 + 1) * 128], wd[ds_][:, fg * 256:(fg + 1) * 256], fg == 0, fg == nF - 1, [bA, bwd[ds_]], [bpY[a]])
                    self.copy("act" if c4 % 2 else "dve", Yr[ys_][:, c4, dq * 256:(dq + 1) * 256], pY[a][:, 0:256], [bpY[a]], [bYr[ys_]])
            self.dma("st", self.d_ys[b * MB:(b + 1) * MB, :].rearrange("(c p) d -> p c d", p=128), Yr[ys_][:], [bYr[ys_]], [bys])
        self.b_ys = bys
        self.st_dve_q = "pool"

    def phase_comb(self, es, l, last):
        accs = [self.sb(es, f"cacc{i}", [128, 8, 1024], F32)[0] for i in range(2)]
        baccs = [[Buf(f"cacc{i}_{k}") for k in range(8)] for i in range(2)]
        Y1, bY1 = self.sbn(es, "cY1", [128, 1024], F32, 2)
        Y2, bY2 = self.sbn(es, "cY2", [128, 1024], F32, 2)
        x1, bx1 = self.sbn(es, "cx1", [128, 1024], F32, 2)
        sq, bsq = self.sbn(es, "csq", [128, 512], F32, 2)
        mu, bmu = self.sb(es, "cmu", [128, 512], F32)
        rstd, brstd = self.sb(es, "crs", [128, 512], F32)
        pT, bpT = self.psn(es, "cpT", 2)
        pL, bpL = self.psn(es, "cpL", 2)
        pend = []
        for st in range(4):
            s, N = CTX + 1024 * st, 1024
            acc, bacc = accs[st % 2], baccs[st % 2]
            for c8 in range(8):
                c = st * 8 + c8
                ys_ = c % 2
                for t2, (Yt, bYt) in enumerate(((Y1, bY1), (Y2, bY2))):
                    idx = self.D12[:, t2, c:c + 1].bitcast(U32)
                    self.S.add("pool", lambda e, idx=idx, dst=Yt[ys_]: e.indirect_dma_start(
                        out=dst[:], out_offset=None, in_=self.d_ys, in_offset=IndirectOffsetOnAxis(ap=idx, axis=0)),
                        [self.b_D12], [bYt[ys_]], dma=True)
                self.ts("dve", Y1[ys_][:], Y1[ys_][:], self.P12[:, 0, c:c + 1], ALU.mult, [bY1[ys_], self.b_P12], [bY1[ys_]])
                self.stt(Y1[ys_][:], Y2[ys_][:], self.P12[:, 1, c:c + 1], Y1[ys_][:], ALU.mult, ALU.add,
                         [bY2[ys_], bY1[ys_], self.b_P12], [bY1[ys_]])
                for kq in range(2):
                    a = self.nxt("cpT", 2)
                    for k4 in range(4):
                        k = kq * 4 + k4
                        self.tr(pT[a][:, k4 * 128:(k4 + 1) * 128], Y1[ys_][:, k * 128:(k + 1) * 128], self.ident(), [bY1[ys_], self.b_cst], [bpT[a]])
                    self.copy("act", acc[:, kq * 4:(kq + 1) * 4, c8 * 128:(c8 + 1) * 128], pT[a][:].rearrange("p (k t) -> p k t", k=4),
                              [bpT[a]], bacc[kq * 4:(kq + 1) * 4])
                for _ in range(-(-len(pend) // (8 - c8))):
                    pend.pop(0)()
            subs = [(0, 512), (512, 512)]
            pend.extend(self.ffn_epilogue_stages(s, N, subs, 0, last, acc, bacc, x1, bx1, pL, bpL, (sq, bsq, mu, bmu, rstd, brstd)))
        while pend:
            pend.pop(0)()


def fm(v):
    v = np.asarray(v, dtype=np.float32)
    n = v.shape[-1] // 128
    return np.swapaxes(v.reshape(v.shape[:-1] + (n, 128)), -1, -2)


def host_consts():
    c = np.zeros((6, 128, 128), np.float32)
    c[0] = np.eye(128)
    c[1] = 1.0 / 1024
    for h in range(2):
        c[2, h * 64:(h + 1) * 64, h * 64:(h + 1) * 64] = 1.0 / 64
    for m in range(128):
        d = m % 32
        partner = m + 16 if d < 16 else m - 16
        c[3, partner, m] = 1.0
    c[4] = 1.0
    c[5, :, 0] = 1.0
    c[5, :, 1] = RMS_EPS
    c[5, :, 2] = LN_EPS
    t = np.arange(SEQ)
    row = (t // 64).astype(np.float32)
    col = (t % 64).astype(np.float32)
    nf = 16
    inv = (10000.0 ** (-np.arange(nf, dtype=np.float32) / nf)).astype(np.float32)
    cosT = np.zeros((128, SEQ), np.float32)
    sinT = np.zeros((128, SEQ), np.float32)
    for p in range(128):
        d = p % 64
        pos = row if d < 32 else col
        ang = (pos * inv[d % 16]).astype(np.float32)
        cosT[p] = np.cos(ang)
        sinT[p] = -np.sin(ang) if (d % 32) < 16 else np.sin(ang)
    invcnt = np.zeros((4, PADW), np.float32)
    for g, wdw in enumerate(POOL_WINDOWS):
        lo = wdw // 2
        for (L, base) in ((CTX, 8), (SEQ, 8 + CTX + 16)):
            tt = np.arange(L)
            start = np.clip(tt - lo, 0, L)
            end = np.clip(tt - lo + wdw, 0, L)
            invcnt[g, base:base + L] = 1.0 / (end - start).astype(np.float32)
    sel8 = np.zeros((8, 8, 128), np.float32)
    for e in range(8):
        sel8[e, e, :] = 1.0
    mc = np.zeros((8, 64), np.float32)
    mc[:, 0:8] = (np.arange(8) * MB)[None, :]
    mc[:, 8:8 + NBLK] = (np.arange(NBLK) * MB)[None, :]
    for e1 in range(8):
        for e2 in range(8):
            mc[e1, 32 + e2] = 1.0 if e1 < e2 else 0.0
    mc2 = np.zeros((128, 32), np.float32)
    mc2[:, 0] = np.arange(128) * 16
    for j in range(16):
        mc2[:, 2 + j] = j
    return c, cosT, sinT, invcnt, sel8, mc, mc2


def relayout_gu(w):
    w = np.asarray(w, dtype=np.float32).reshape(N_EXP, 8, 128, 4, 896)
    w = np.transpose(w, (0, 2, 3, 1, 4))
    return np.ascontiguousarray(w).reshape(N_EXP * 128 * 16, 1792)


def relayout_d(w):
    w = np.asarray(w, dtype=np.float32).reshape(N_EXP, 28, 128, 4, 256)
    w = np.transpose(w, (0, 2, 3, 1, 4))
    return np.ascontiguousarray(w).reshape(N_EXP * 128 * 16, 1792)


def prep_inputs(inp):
    f32 = lambda a: np.ascontiguousarray(np.asarray(a, dtype=np.float32))
    consts, cosT, sinT, invcnt, sel8, mc, mc2 = host_consts()
    vecs = np.zeros((DEPTH, 128, NV), np.float32)
    for l in range(DEPTH):
        vecs[l, :, V_BMOD:V_BMOD + 48] = fm(inp["b_mod"][l])
        vecs[l, :, V_BMERGE:V_BMERGE + 24] = fm(np.asarray(inp["b_merge"][l]).reshape(-1))
        vecs[l, :, V_PSCALE:V_PSCALE + 4] = fm(inp["pool_scale"][l])
        cw = fm(inp["conv_w"][l])
        vecs[l, :, V_CONVW:V_CONVW + 16] = np.transpose(cw, (1, 2, 0)).reshape(128, 16)
        vecs[l, :, V_CONVB:V_CONVB + 4] = fm(inp["conv_b"][l])
        for nm, off in (("lru_ba", V_BA), ("lru_bx", V_BX), ("lru_lambda", V_LAM)):
            a = fm(inp[nm][l])
            vecs[l, :, off:off + 8] = np.transpose(a, (1, 0, 2)).reshape(128, 8)
        vecs[l, :, V_QN] = np.tile(np.asarray(inp["q_norm"][l], np.float32), 2)
        vecs[l, :, V_KN] = np.tile(np.asarray(inp["k_norm"][l], np.float32), 2)
        vecs[l, :, V_LN1G:V_LN1G + 8] = fm(inp["ln1_g"][l])
        vecs[l, :, V_LN1B:V_LN1B + 8] = fm(inp["ln1_b"][l])
        vecs[l, :, V_LN2G:V_LN2G + 8] = fm(inp["ln2_g"][l])
        vecs[l, :, V_LN2B:V_LN2B + 8] = fm(inp["ln2_b"][l])
        vecs[l, :, V_RB:V_RB + 8] = np.asarray(inp["moe_router_b"][0], np.float32)[None, :]
    lru_bd = np.zeros((DEPTH, 4, 4, 128, 128), np.float32)
    for l in range(DEPTH):
        for c in range(4):
            for dr in range(2):
                for wi, nm in enumerate(("lru_wa", "lru_wx")):
                    for hh in range(2):
                        lru_bd[l, c, 2 * dr + wi, hh * 64:(hh + 1) * 64, hh * 64:(hh + 1) * 64] = inp[nm][l][dr][2 * c + hh]
    shared = {
        "vecs": vecs, "w_mod": f32(inp["w_mod"]), "w_in": f32(inp["w_in"]), "pool_w": f32(inp["pool_w"]),
        "lru_bd": lru_bd, "w_branch": f32(np.asarray(inp["w_branch"]).reshape(DEPTH, 1536, D)), "w_out": f32(inp["w_out"]),
        "ffn_w_gate": f32(inp["ffn_w_gate"][0]), "ffn_w_up": f32(inp["ffn_w_up"][0]), "ffn_w_down": f32(inp["ffn_w_down"][0]),
        "moe_router": f32(inp["moe_router"][0]), "moe_g2": relayout_gu(inp["moe_w_gate"][0]), "moe_u2": relayout_gu(inp["moe_w_up"][0]),
        "moe_d2": relayout_d(inp["moe_w_down"][0]), "consts": consts, "cosT": cosT, "sinT": sinT, "invcnt": invcnt, "sel8": sel8, "mconst": mc, "mconst2": mc2,
    }
    maps = []
    cc = fm(inp["c_ctx"])
    for b in range(8):
        cvec = np.stack([fm(inp["c"][b]), cc], axis=-1)
        m = dict(shared)
        m["xT"] = f32(np.asarray(inp["x"][b]).T)
        m["ctxT"] = f32(np.asarray(inp["ctx"][b]).T)
        m["cvec"] = f32(cvec)
        maps.append(m)
    return maps


_NC_CACHE = {}


def kernel(**inputs):
    maps = prep_inputs(inputs)
    if "nc" not in _NC_CACHE:
        _NC_CACHE["nc"] = Ker().build()
    nc = _NC_CACHE["nc"]
    res = run_bass_kernel_spmd(nc, maps, core_ids=list(range(8)))
    out = np.empty((8, SEQ, D), np.float32)
    for b in range(8):
        o = np.asarray(res.results[b]["outT"]).reshape(128, 8, SEQ)
        out[b] = np.transpose(o, (2, 1, 0)).reshape(SEQ, D)
    return out
```

```python
import contextlib
import os
BLK_STAGE = int(os.environ.get('BLK_STAGE', '3'))
NBLK_RUN = int(os.environ.get('NBLK_RUN', '23'))
import numpy as np
import concourse.bass as bass
import concourse.mybir as mybir
from concourse.bass_utils import run_bass_kernel_spmd
from concourse.bass import IndirectOffsetOnAxis

F32 = mybir.dt.float32
BF16 = mybir.dt.bfloat16
I32 = mybir.dt.int32
U32 = mybir.dt.uint32
AF = mybir.ActivationFunctionType
ALU = mybir.AluOpType
AX = mybir.AxisListType

D = 1024
SEQ = 4096
CTX = 256
NT = SEQ + CTX
DEPTH = 2
W_IN = 5376
D_FF = 2816
D_EXP = 3584
N_EXP = 8
MB = 512
NBLK = 23
NSLOT = MB * NBLK
SPARSE_MOE = True
ALPHA = (2 * DEPTH) ** 0.25
LN_EPS = 1e-5
RMS_EPS = 1e-6
PADW = NT + 32
TILES = [(0, 256)] + [(256 + 512 * i, 512) for i in range(8)]
POOL_WINDOWS = (2, 4, 8, 16)

V_BMOD, V_BMERGE, V_PSCALE, V_CONVW, V_CONVB = 0, 48, 72, 76, 92
V_BA, V_BX, V_LAM, V_QN, V_KN = 96, 104, 112, 120, 121
V_LN1G, V_LN1B, V_LN2G, V_LN2B, V_RB = 122, 130, 138, 146, 154
NV = 162


def pad_pos(s):
    return s + 8 if s < CTX else s + 24


class Buf:
    __slots__ = ("name", "w", "r")

    def __init__(self, name=""):
        self.name = name
        self.w = None
        self.r = []


class Op:
    __slots__ = ("eng", "fn", "deps", "needs_inc", "count", "dma", "sem", "waits")

    def __init__(self, eng, fn, dma):
        self.eng = eng
        self.fn = fn
        self.deps = set()
        self.needs_inc = False
        self.count = 0
        self.dma = dma
        self.sem = None
        self.waits = []


ENGS = ["pe", "act", "dve", "pool", "sp"]


class Sched:
    def __init__(self, nc, es, n_dma_sems=48):
        self.nc = nc
        self.n_dma_sems = n_dma_sems
        self.esem = {e: es.enter_context(nc.semaphore(f"se_{e}")) for e in ENGS}
        self.dsem = [es.enter_context(nc.semaphore(f"sd_{i}")) for i in range(n_dma_sems)]
        self.ecount = {e: 0 for e in ENGS}
        self.dma_cnt = [0] * n_dma_sems
        self.dma_rr = 0
        self.total_ops = 0
        self._reset_phase()

    def _reset_phase(self):
        self.ops = {e: [] for e in ENGS}
        self.all = []
        self.dma_last = [None] * self.n_dma_sems
        self.touched = set()

    def add(self, eng, fn, reads=(), writes=(), dma=False):
        op = Op(eng, fn, dma)
        for b in reads:
            if b.w is not None:
                op.deps.add(b.w)
        for b in writes:
            if b.w is not None:
                op.deps.add(b.w)
            for r in b.r:
                op.deps.add(r)
        for b in reads:
            b.r.append(op)
            self.touched.add(b)
        for b in writes:
            b.w = op
            b.r = []
            self.touched.add(b)
        if dma:
            s = self.dma_rr
            self.dma_rr = (self.dma_rr + 1) % self.n_dma_sems
            prev = self.dma_last[s]
            if prev is not None:
                op.deps.add(prev)
            self.dma_last[s] = op
            self.dma_cnt[s] += 1
            op.sem = s
            op.count = self.dma_cnt[s] * 16
            op.needs_inc = True
        op.deps.discard(op)
        self.ops[eng].append(op)
        self.all.append(op)
        return op

    def flush(self):
        nc = self.nc
        for op in self.all:
            nd = set()
            for d in op.deps:
                if (not d.dma) and (not op.dma) and d.eng == "pe" and op.eng == "pe":
                    continue
                nd.add(d)
            op.deps = nd
            for d in nd:
                d.needs_inc = True
        for e in ENGS:
            for op in reversed(self.ops[e]):
                if not op.dma:
                    op.needs_inc = True
                    break
        for e in ENGS:
            c = self.ecount[e]
            for op in self.ops[e]:
                if op.dma:
                    continue
                if op.needs_inc:
                    c += 1
                    op.count = c
            self.ecount[e] = c
        seen = {e: {} for e in ENGS}
        for e in ENGS:
            sn = seen[e]
            for op in self.ops[e]:
                need = {}
                for d in op.deps:
                    key = ("d", d.sem) if d.dma else ("e", d.eng)
                    if d.count > need.get(key, 0):
                        need[key] = d.count
                for key, v in need.items():
                    if sn.get(key, 0) >= v:
                        continue
                    sn[key] = v
                    op.waits.append((key, v))
        bar = []
        for e in ENGS:
            if self.ecount[e] > 0:
                bar.append((("e", e), self.ecount[e]))
        for s in range(self.n_dma_sems):
            if self.dma_cnt[s] > 0:
                bar.append((("d", s), self.dma_cnt[s] * 16))
        handles = {"pe": "tensor", "act": "scalar", "dve": "vector", "pool": "gpsimd", "sp": "sync"}
        with nc.Block() as block:
            def run(ename, eng):
                sn = seen[ename]
                for op in self.ops[ename]:
                    for key, v in op.waits:
                        sem = self.dsem[key[1]] if key[0] == "d" else self.esem[key[1]]
                        eng.wait_ge(sem, v)
                    ins = op.fn(eng)
                    if op.dma:
                        ins.then_inc(self.dsem[op.sem], 16)
                    elif op.needs_inc:
                        ins.then_inc(self.esem[ename], 1)
                for key, v in bar:
                    if key == ("e", ename):
                        continue
                    if sn.get(key, 0) >= v:
                        continue
                    sem = self.dsem[key[1]] if key[0] == "d" else self.esem[key[1]]
                    eng.wait_ge(sem, v)

            for ename in ENGS:
                getattr(block, handles[ename])(lambda eng, ename=ename: run(ename, eng))
        self.total_ops += len(self.all)
        for b in self.touched:
            b.w = None
            b.r = []
        self._reset_phase()


def rev(ap2d):
    (ps, pn), (fs, fn) = ap2d.ap
    return bass.AP(ap2d.tensor, ap2d.offset + fs * (fn - 1), [[ps, pn], [-fs, fn]])


def bcast_last(ap2d, n):
    (ps, pn), (fs, fn) = ap2d.ap
    return bass.AP(ap2d.tensor, ap2d.offset, [[ps, pn], [fs, fn], [0, n]])


def pbcast(ap_row, nparts=128):
    dims = list(ap_row.ap)
    return bass.AP(ap_row.tensor, ap_row.offset, [[0, nparts]] + [list(d) for d in dims[1:]])


class Ker:
    def __init__(self, dbg=False, layers=(0, 1), stop_after=None):
        self.dbg = dbg
        self.layers = layers
        self.stop_after = stop_after
        self.nc = bass.Bass("TRN2", target_bir_lowering=False)
        self.rot = {}

    def dma(self, q, out, in_, reads=(), writes=()):
        if q == "st":
            q = "sp"
            for b in reads:
                if b.w is not None and (not b.w.dma) and b.w.eng in ("act", "dve", "pool"):
                    q = {"act": "act", "pool": "pool", "dve": getattr(self, "st_dve_q", "pool")}[b.w.eng]
                    break
        return self.S.add(q, lambda e: e.dma_start(out=out, in_=in_), reads, writes, dma=True)

    def mm(self, out, lhsT, rhs, start, stop, reads, writes):
        return self.S.add("pe", lambda e: e.matmul(out, lhsT=lhsT, rhs=rhs, start=start, stop=stop), reads, writes)

    def tr(self, out, in_, ident, reads, writes):
        return self.S.add("pe", lambda e: e.transpose(out, in_, ident), reads, writes)

    def act(self, out, in_, func, reads, writes, scale=1.0, bias=None):
        if bias is None:
            return self.S.add("act", lambda e: e.activation(out=out, in_=in_, func=func, scale=scale), reads, writes)
        return self.S.add("act", lambda e: e.activation(out=out, in_=in_, func=func, scale=scale, bias=bias), reads, writes)

    def tt(self, eng, out, in0, in1, op, reads, writes):
        return self.S.add(eng, lambda e: e.tensor_tensor(out=out, in0=in0, in1=in1, op=op), reads, writes)

    def ts(self, eng, out, in0, s1, op0, reads, writes, s2=None, op1=None):
        if op1 is None:
            return self.S.add(eng, lambda e: e.tensor_scalar(out, in0, s1, None, op0), reads, writes)
        return self.S.add(eng, lambda e: e.tensor_scalar(out=out, in0=in0, scalar1=s1, scalar2=s2, op0=op0, op1=op1), reads, writes)

    def stt(self, out, in0, scalar, in1, op0, op1, reads, writes):
        return self.S.add("dve", lambda e: e.scalar_tensor_tensor(out=out, in0=in0, scalar=scalar, in1=in1, op0=op0, op1=op1), reads, writes)

    def recip(self, out, in_, reads, writes):
        return self.S.add("dve", lambda e: e.reciprocal(out=out, in_=in_), reads, writes)

    def copy(self, eng, out, in_, reads, writes):
        if eng == "act":
            return self.act(out, in_, AF.Copy, reads, writes)
        return self.S.add(eng, lambda e: e.tensor_copy(out=out, in_=in_), reads, writes)

    def memset(self, eng, out, val, writes):
        return self.S.add(eng, lambda e: e.memset(out, val), (), writes)

    def scan(self, out, d0, d1, initial, reads, writes):
        return self.S.add("dve", lambda e: e.tensor_tensor_scan(out=out, data0=d0, data1=d1, initial=initial,
                                                                  op0=ALU.mult, op1=ALU.add), reads, writes)

    def rmax(self, out, in_, reads, writes):
        return self.S.add("dve", lambda e: e.tensor_reduce(out=out, in_=in_, op=ALU.max, axis=AX.X), reads, writes)

    def subflush(self, name):
        with self.nc.named_scope(name):
            self.S.flush()

    def un(self, name):
        self.uid = getattr(self, "uid", 0) + 1
        return f"{name}_{self.uid}"

    def sb(self, es, name, shape, dt):
        t = es.enter_context(self.nc.sbuf_tensor(self.un(name), shape, dt))
        return t, Buf(name)

    def sbn(self, es, name, shape, dt, n):
        ts_, bs = [], []
        for i in range(n):
            t, b = self.sb(es, f"{name}{i}", shape, dt)
            ts_.append(t)
            bs.append(b)
        return ts_, bs

    def psn(self, es, name, n, shape=(128, 512), dt=F32):
        ts_, bs = [], []
        for i in range(n):
            ts_.append(es.enter_context(self.nc.psum_tensor(self.un(f"{name}{i}"), list(shape), dt)))
            bs.append(Buf(f"{name}{i}"))
        return ts_, bs

    def nxt(self, key, n):
        v = self.rot.get(key, 0)
        self.rot[key] = v + 1
        return v % n

    def dram(self, name, shape, dt, out=False):
        kind = "ExternalOutput" if (out or self.dbg) else "Internal"
        return self.nc.dram_tensor(name, list(shape), dt, kind=kind).ap()

    def build(self):
        nc = self.nc
        I = lambda name, shape: nc.dram_tensor(name, list(shape), F32, kind="ExternalInput").ap()
        self.i_xT = I("xT", (D, SEQ))
        self.i_ctxT = I("ctxT", (D, CTX))
        self.i_cvec = I("cvec", (128, 8, 2))
        self.i_vecs = I("vecs", (DEPTH, 128, NV))
        self.i_wmod = I("w_mod", (DEPTH, D, 6 * D))
        self.i_win = I("w_in", (DEPTH, D, W_IN))
        self.i_poolw = I("pool_w", (DEPTH, 4, 128, 128))
        self.i_lrubd = I("lru_bd", (DEPTH, 4, 4, 128, 128))
        self.i_wbr = I("w_branch", (DEPTH, 1536, D))
        self.i_wout = I("w_out", (DEPTH, D, D))
        self.i_fg = I("ffn_w_gate", (D, D_FF))
        self.i_fu = I("ffn_w_up", (D, D_FF))
        self.i_fd = I("ffn_w_down", (D_FF, D))
        self.i_router = I("moe_router", (D, N_EXP))
        self.i_mg = I("moe_g2", (N_EXP * 128 * 16, 1792))
        self.i_mu = I("moe_u2", (N_EXP * 128 * 16, 1792))
        self.i_md = I("moe_d2", (N_EXP * 128 * 16, 1792))
        self.i_consts = I("consts", (6, 128, 128))
        self.i_cos = I("cosT", (128, SEQ))
        self.i_sin = I("sinT", (128, SEQ))
        self.i_invcnt = I("invcnt", (4, PADW))
        self.i_sel8 = I("sel8", (8, 8, 128))
        self.i_mc = I("mconst", (8, 64))
        self.i_mc2 = I("mconst2", (128, 32))
        self.d_xs1 = self.dram("xs1", (128, 8, NT), F32)
        self.d_h = self.dram("hT", (128, 8, NT), BF16)
        self.d_q = self.dram("qT", (4, 128, NT), BF16)
        self.d_y = self.dram("yT", (128, 12, NT), BF16)
        self.d_x1 = self.dram("x1T", (128, 8, NT), F32)
        self.d_h2 = self.dram("h2T", (128, 8, NT), BF16)
        self.d_out = self.dram("outT", (128, 8, SEQ), F32, out=True)
        self.d_gate = self.dram("gateT", (8, SEQ), F32)
        self.d_xs = self.dram("Xs", (NSLOT, D), BF16)
        self.d_ys = self.dram("Ys", (NSLOT, D), F32)
        if self.dbg:
            self.d_mod = self.dram("dbg_mod", (DEPTH, 128, 96), F32)
            self.d_k = self.dram("dbg_k", (2, 128, NT), BF16)
            self.d_v = self.dram("dbg_v", (128, 34 * 2 * 66), BF16)

        with contextlib.ExitStack() as es:
            self.S = Sched(nc, es)
            self.cst, self.b_cst = self.sb(es, "cst", [128, 6, 128], F32)
            self.vec, self.b_vec = self.sb(es, "vec", [128, NV], F32)
            self.mod, self.b_mod = self.sb(es, "mod", [128, 48, 2], F32)
            self.modp, self.b_modp = self.sb(es, "modp", [128, 16, 2], F32)
            self.lsc, self.b_lsc = self.sb(es, "lsc", [128, 2, 8], F32)
            self.nbias, self.b_nbias = self.sb(es, "nbias", [128, 1], F32)
            self.nbx, self.b_nbx = self.sb(es, "nbx", [128, 16], F32)
            self.sel, self.b_sel = self.sb(es, "sel", [8, 8, 128], F32)
            self.dma("sp", self.cst[:], self.i_consts.rearrange("c p n -> p c n"), (), [self.b_cst])
            self.dma("sp", self.sel[:], self.i_sel8, (), [self.b_sel])
            self.S.flush()
            for l in self.layers:
                last = l == DEPTH - 1
                if last and SPARSE_MOE:
                    tail = [("merge", self.phase_merge), ("route", self.phase_route), ("blk", self.phase_blocks),
                            ("ffn", self.phase_comb)]
                else:
                    tail = [("merge", self.phase_merge), ("ffn", self.phase_ffn)]
                groups = [[("mod", self.phase_mod)], [("h", self.phase_h)],
                          [("mix", self.phase_mix), ("attn", self.phase_attn)], tail]
                for gi, grp in enumerate(groups):
                    with contextlib.ExitStack() as ges:
                        if gi == 2:
                            self.kd, self.b_kd = self.sbn(ges, "kd", [128, NT], BF16, 4)
                            self.va, self.b_va = self.sb(ges, "va", [128, 34, 2, 66], BF16)
                        if gi == 3 and last:
                            self.P12, self.b_P12 = self.sb(ges, "P12", [128, 2, 32], F32)
                            self.D12, self.b_D12 = self.sb(ges, "D12", [128, 2, 32], I32)
                            self.IG, self.b_IG = self.sb(ges, "IG", [128, NBLK, 16], I32)
                        for name, fn in grp:
                            with contextlib.ExitStack() as pes:
                                fn(pes, l, last)
                                with nc.named_scope(f"L{l}_{name}"):
                                    self.S.flush()
                            if self.stop_after == (l, name):
                                return nc
        return nc

    def ident(self):
        return self.cst[:, 0, :]

    def onesD(self):
        return self.cst[:, 1, :]

    def blk64(self):
        return self.cst[:, 2, :]

    def perm(self):
        return self.cst[:, 3, :]

    def ones(self):
        return self.cst[:, 4, :]

    def xsrc(self, l, s, n):
        if l == 0:
            if s < CTX:
                return self.i_ctxT.rearrange("(k p) n -> p k n", p=128)[:, :, s:s + n]
            return self.i_xT.rearrange("(k p) n -> p k n", p=128)[:, :, s - CTX:s - CTX + n]
        return self.d_xs1[:, :, s:s + n]

    def phase_mod(self, es, l, last):
        vec, bv = self.vec, self.b_vec
        self.dma("sp", vec[:], self.i_vecs[l], (), [bv])
        cv, bcv = self.sb(es, "cv", [128, 8, 2], F32)
        sc, bsc = self.sb(es, "sc", [128, 8, 2], F32)
        self.dma("sp", cv[:], self.i_cvec, (), [bcv])
        self.act(sc[:], cv[:], AF.Silu, [bcv], [bsc])
        wm, bwm = self.sbn(es, "wm", [128, 8, 768], F32, 3)
        psm = es.enter_context(self.nc.psum_tensor(self.un("psm"), [128, 96], F32))
        bpsm = Buf("psm")
        wsrc = self.i_wmod[l].rearrange("(k p) n -> p k n", p=128)
        for g in range(8):
            sl = g % 3
            for hh in range(2):
                self.dma("sp" if hh == 0 else "act", wm[sl][:, hh * 4:(hh + 1) * 4, :], wsrc[:, hh * 4:(hh + 1) * 4, g * 768:(g + 1) * 768], (), [bwm[sl]])
            for jj in range(6):
                j = g * 6 + jj
                for k in range(8):
                    self.mm(psm[:, 2 * j:2 * j + 2], wm[sl][:, k, jj * 128:(jj + 1) * 128], sc[:, k, :],
                            k == 0, k == 7, [bwm[sl], bsc], [bpsm])
        psv = psm[:].rearrange("p (j w) -> p j w", w=2)
        for w in range(2):
            self.tt("dve", self.mod[:, :, w], psv[:, :, w], vec[:, V_BMOD:V_BMOD + 48], ALU.add, [bpsm, bv], [self.b_mod])
        self.ts("dve", self.modp[:, 0:8, :], self.mod[:, 8:16, :], 1.0, ALU.add, [self.b_mod], [self.b_modp])
        self.ts("dve", self.modp[:, 8:16, :], self.mod[:, 32:40, :], 1.0, ALU.add, [self.b_mod], [self.b_modp])
        if self.dbg:
            self.dma("sp", self.d_mod[l], self.mod[:].rearrange("p j w -> p (j w)"), [self.b_mod], [])
        lam = vec[:, V_LAM:V_LAM + 8]
        t = {}
        for nm in ["ab", "e", "y", "y2", "p", "r", "sp"]:
            t[nm], _ = self.sb(es, "sp_" + nm, [128, 8], F32)
        bt = Buf("sptmp")
        self.ts("dve", t["r"][:], lam, -1.0, ALU.mult, [bv], [bt])
        self.tt("dve", t["ab"][:], t["r"][:], lam, ALU.max, [bv, bt], [bt])
        self.act(t["e"][:], t["ab"][:], AF.Exp, [bt], [bt], scale=-1.0)
        self.ts("dve", t["y"][:], t["e"][:], 2.0, ALU.add, [bt], [bt])
        self.recip(t["y"][:], t["y"][:], [bt], [bt])
        self.tt("dve", t["y"][:], t["y"][:], t["e"][:], ALU.mult, [bt], [bt])
        self.tt("dve", t["y2"][:], t["y"][:], t["y"][:], ALU.mult, [bt], [bt])
        self.ts("dve", t["p"][:], t["y2"][:], 1.0 / 13, ALU.mult, [bt], [bt], s2=1.0 / 11, op1=ALU.add)
        for cf in [1.0 / 9, 1.0 / 7, 1.0 / 5, 1.0 / 3, 1.0]:
            self.tt("dve", t["p"][:], t["p"][:], t["y2"][:], ALU.mult, [bt], [bt])
            self.ts("dve", t["p"][:], t["p"][:], cf, ALU.add, [bt], [bt])
        self.tt("dve", t["p"][:], t["p"][:], t["y"][:], ALU.mult, [bt], [bt])
        self.ts("dve", t["r"][:], lam, -1.0, ALU.mult, [bv, bt], [bt], s2=0.0, op1=ALU.max)
        self.stt(t["sp"][:], t["p"][:], 2.0, t["r"][:], ALU.mult, ALU.add, [bt], [bt])
        lsv = self.lsc[:].rearrange("p a b -> p (a b)")
        self.ts("dve", self.lsc[:, 0, :], t["sp"][:], -8.0, ALU.mult, [bt], [self.b_lsc])
        self.ts("dve", self.lsc[:, 1, :], t["sp"][:], -16.0, ALU.mult, [bt], [self.b_lsc])
        self.ts("dve", self.nbx[:], vec[:, V_BA:V_BA + 16], -1.0, ALU.mult, [bv], [self.b_nbx])
        m2, _ = self.sb(es, "m2", [128, 2], F32)
        self.tt("dve", m2[:], vec[:, V_QN:V_QN + 2], vec[:, V_QN:V_QN + 2], ALU.mult, [bv], [bt])
        mx, _ = self.sb(es, "mx", [128, 2], F32)
        pst = es.enter_context(self.nc.psum_tensor(self.un("pst"), [128, 128], F32))
        bpst = Buf("pst")
        self.tr(pst[0:2, :], m2[:], self.ident(), [bt, self.b_cst], [bpst])
        r2, _ = self.sb(es, "r2", [2, 1], F32)
        self.rmax(r2[:], pst[0:2, :], [bpst], [bt])
        l2, _ = self.sb(es, "l2", [2, 1], F32)
        self.act(l2[:], r2[:], AF.Ln, [bt], [bt])
        psb = es.enter_context(self.nc.psum_tensor(self.un("psb"), [128, 2], F32))
        bpsb = Buf("psb")
        self.mm(psb[:, 0:1], self.ones()[0:2, :], l2[:], True, True, [bt, self.b_cst], [bpsb])
        self.act(mx[:, 0:1], psb[:, 0:1], AF.Exp, [bpsb], [bt], scale=0.5)
        self.ts("dve", self.nbias[:], mx[:, 0:1], -8.0, ALU.mult, [bt], [self.b_nbias])

    def phase_h(self, es, l, last):
        xt, bxt = self.sbn(es, "xt", [128, 8, 512], F32, 2)
        ht, bht = self.sbn(es, "ht", [128, 8, 512], BF16, 2)
        for ti, (s, n) in enumerate(TILES):
            w = 1 if s < CTX else 0
            sl = ti % 2
            self.dma("sp", xt[sl][:, :, 0:n], self.xsrc(l, s, n), (), [bxt[sl]])
            for k in range(8):
                if k % 2 == 0:
                    self.act(ht[sl][:, k, 0:n], xt[sl][:, k, 0:n], AF.Identity, [bxt[sl], self.b_mod, self.b_modp], [bht[sl]],
                             scale=self.modp[:, k, w:w + 1], bias=self.mod[:, k, w:w + 1])
                else:
                    self.ts("dve", ht[sl][:, k, 0:n], xt[sl][:, k, 0:n], self.modp[:, k, w:w + 1], ALU.mult,
                            [bxt[sl], self.b_mod, self.b_modp], [bht[sl]], s2=self.mod[:, k, w:w + 1], op1=ALU.add)
            self.dma("st", self.d_h[:, :, s:s + n], ht[sl][:, :, 0:n], [bht[sl]], [])

    def load_h(self, ti):
        s, n = TILES[ti]
        sl = self.nxt("hb", 3)
        self.dma("sp", self.hb[sl][:, :, 0:n], self.d_h[:, :, s:s + n], (), [self.b_hb[sl]])
        return self.hb[sl], self.b_hb[sl]

    def load_wz(self, l, cols):
        sl = self.nxt("wz", 4)
        wsrc = self.i_win[l].rearrange("(k p) n -> p k n", p=128)
        for (do, sc_, nn) in cols:
            self.dma("pool", self.wz[sl][:, :, do:do + nn], wsrc[:, :, sc_:sc_ + nn], (), [self.b_wz[sl]])
        return self.wz[sl], self.b_wz[sl]

    def zmm(self, ps, bps, wz, bwz, hb, bhb, n):
        for k in range(8):
            self.mm(ps[:, 0:n], wz[:, k, :], hb[:, k, 0:n], k == 0, k == 7, [bwz, bhb], [bps])

    def phase_mix(self, es, l, last):
        nc = self.nc
        vec, bv = self.vec, self.b_vec
        self.hb, self.b_hb = self.sbn(es, "hb", [128, 8, 512], BF16, 3)
        self.wz, self.b_wz = self.sbn(es, "wz", [128, 8, 128], BF16, 4)
        pz, bpz = self.psn(es, "pz", 3)
        pa, bpa = self.psn(es, "pa", 4)
        hl, bhl = self.sb(es, "hl", [128, 2], F32)
        tA, btA = self.sbn(es, "tA", [128, 544], F32, 2)
        tB, btB = self.sbn(es, "tB", [128, 544], F32, 2)
        tC, btC = self.sbn(es, "tC", [128, 512], F32, 2)
        tD, btD = self.sbn(es, "tD", [128, 512], F32, 2)
        tE, btE = self.sbn(es, "tE", [128, 512], F32, 2)
        tF, btF = self.sbn(es, "tF", [128, 512], F32, 2)
        ob, bob = self.sbn(es, "ob", [128, 512], BF16, 3)
        db, bdb = self.sbn(es, "db", [128, 512], BF16, 2)
        ic, bic = self.sbn(es, "ic", [128, 512], F32, 2)
        pw, bpw = self.sb(es, "pw", [128, 4, 128], BF16)
        bd, bbd = self.sbn(es, "bd", [128, 4, 128], BF16, 2)
        fes = contextlib.ExitStack()
        zp, bzp = self.sb(fes, "zp", [128, PADW], F32)
        xc, bxc = self.sb(fes, "xc", [128, NT], F32)
        xcb, bxcb = self.sb(fes, "xcb", [128, NT], BF16)
        hsum, bhs = self.sb(fes, "hsum", [128, NT], F32)
        gl, bgl = self.sb(fes, "gl", [128, NT], F32)
        self.memset("pool", zp[:], 0.0, [bzp])
        self.memset("pool", self.va[:, :, :, 64:66], 1.0, [self.b_va])
        for i_ in range(4):
            self.memset("pool", self.kd[i_][:], 0.0, [self.b_kd[i_]])
        self.dma("pool", pw[:], self.i_poolw[l].rearrange("g c d -> c g d"), (), [bpw])

        for g in range(4):
            wz, bwz = self.load_wz(l, [(0, g * 128, 128)])
            for ti, (s, n) in enumerate(TILES):
                hb, bhb = self.load_h(ti)
                p = self.nxt("pz", 3)
                self.zmm(pz[p], bpz[p], wz, bwz, hb, bhb, n)
                self.copy("act", zp[:, pad_pos(s):pad_pos(s) + n], pz[p][:, 0:n], [bpz[p]], [bzp])
            m = g + 1
            for ti, (s, n) in enumerate(TILES):
                if last and s < CTX:
                    continue
                p0 = pad_pos(s)
                a = p0 - (1 << (m - 1))
                lens = [n]
                for i in range(m, 0, -1):
                    lens.append(lens[-1] + (1 << (i - 1)))
                lens = lens[::-1]
                sl = ti % 2
                src, bsrc = zp[:, a:a + lens[0]], bzp
                cur = None
                for i in range(1, m + 1):
                    dst, bdst = (tA[sl], btA[sl]) if i % 2 == 1 else (tB[sl], btB[sl])
                    sh = 1 << (i - 1)
                    if i == 1:
                        in0, in1 = zp[:, a:a + lens[1]], zp[:, a + sh:a + sh + lens[1]]
                    else:
                        in0, in1 = cur[:, 0:lens[i]], cur[:, sh:sh + lens[i]]
                    self.tt("dve" if i % 2 else "pool", dst[:, 0:lens[i]], in0, in1, ALU.add, [bsrc], [bdst])
                    cur, bsrc = dst, bdst
                self.dma("sp", ic[sl][:, 0:n], pbcast(self.i_invcnt[g:g + 1, p0:p0 + n]), (), [bic[sl]])
                self.tt("pool", tC[sl][:, 0:n], cur[:, 0:n], ic[sl][:, 0:n], ALU.mult, [bsrc, bic[sl]], [btC[sl]])
                self.tt("dve", db[sl][:, 0:n], tC[sl][:, 0:n], zp[:, p0:p0 + n], ALU.subtract, [btC[sl], bzp], [bdb[sl]])
                p = self.nxt("pa", 4)
                self.mm(pa[p][:, 0:n], pw[:, g, :], db[sl][:, 0:n], True, True, [bpw, bdb[sl]], [bpa[p]])
                o = self.nxt("ob", 3)
                self.act(ob[o][:, 0:n], pa[p][:, 0:n], AF.Copy, [bpa[p], bv], [bob[o]], scale=vec[:, V_PSCALE + g:V_PSCALE + g + 1])
                self.dma("st", self.d_y[:, g, s:s + n], ob[o][:, 0:n], [bob[o]], [])

        self.subflush(f"L{l}_mixpool")
        for c in range(4):
            wzx, bwzx = self.load_wz(l, [(0, 1280 + c * 128, 128)])
            wzg, bwzg = self.load_wz(l, [(0, 1792 + c * 128, 128)])
            bsl = c % 2
            self.dma("pool", bd[bsl][:], self.i_lrubd[l, c].rearrange("m i j -> i m j"), (), [bbd[bsl]])
            for ti, (s, n) in enumerate(TILES):
                hb, bhb = self.load_h(ti)
                p = self.nxt("pz", 3)
                self.zmm(pz[p], bpz[p], wzx, bwzx, hb, bhb, n)
                self.copy("act", zp[:, pad_pos(s):pad_pos(s) + n], pz[p][:, 0:n], [bpz[p]], [bzp])
                if not (last and s < CTX):
                    p2 = self.nxt("pz", 3)
                    self.zmm(pz[p2], bpz[p2], wzg, bwzg, hb, bhb, n)
                    sl = ti % 2
                    self.act(tC[sl][:, 0:n], pz[p2][:, 0:n], AF.Square, [bpz[p2]], [btC[sl]])
                    self.ts("pool", tC[sl][:, 0:n], tC[sl][:, 0:n], 0.044715, ALU.mult, [btC[sl]], [btC[sl]], s2=1.0, op1=ALU.add)
                    self.tt("dve", tC[sl][:, 0:n], tC[sl][:, 0:n], pz[p2][:, 0:n], ALU.mult, [btC[sl], bpz[p2]], [btC[sl]])
                    self.act(tC[sl][:, 0:n], tC[sl][:, 0:n], AF.Sigmoid, [btC[sl]], [btC[sl]], scale=1.5957691216)
                    self.tt("dve", gl[:, s:s + n], tC[sl][:, 0:n], pz[p2][:, 0:n], ALU.mult, [btC[sl], bpz[p2]], [bgl])
            cw = lambda k: vec[:, V_CONVW + c * 4 + k:V_CONVW + c * 4 + k + 1]
            for ti, (s, n) in enumerate(TILES):
                p0 = pad_pos(s)
                self.ts("dve", xc[:, s:s + n], zp[:, p0 - 2:p0 - 2 + n], cw(0), ALU.mult, [bzp, bv], [bxc],
                        s2=vec[:, V_CONVB + c:V_CONVB + c + 1], op1=ALU.add)
                for k in range(1, 4):
                    self.stt(xc[:, s:s + n], zp[:, p0 - 2 + k:p0 - 2 + k + n], cw(k), xc[:, s:s + n], ALU.mult, ALU.add,
                             [bzp, bv, bxc], [bxc])
                self.copy("pool", xcb[:, s:s + n], xc[:, s:s + n], [bxc], [bxcb])
            for dr in (1, 0):
                order = [0] + list(range(8, 0, -1)) if dr == 1 else list(range(9))
                col = dr * 4 + c
                prev_tile = None
                for g0 in range(0, 9, 2):
                    grp = order[g0:g0 + 2]
                    info = []
                    for gi, ti in enumerate(grp):
                        s, n = TILES[ti]
                        pr = self.nxt("pa", 4)
                        self.mm(pa[pr][:, 0:n], bd[bsl][:, 2 * dr, :], xcb[:, s:s + n], True, True, [bbd[bsl], bxcb], [bpa[pr]])
                        pi = self.nxt("pa", 4)
                        self.mm(pa[pi][:, 0:n], bd[bsl][:, 2 * dr + 1, :], xcb[:, s:s + n], True, True, [bbd[bsl], bxcb], [bpa[pi]])
                        info.append((ti, gi, pr, pi))
                    for (ti, sl, pr, pi) in info:
                        s, n = TILES[ti]
                        self.act(tA[sl][:, 0:n], pa[pr][:, 0:n], AF.Sigmoid, [bpa[pr], bv], [btA[sl]],
                                 bias=vec[:, V_BA + col:V_BA + col + 1])
                        self.act(tB[sl][:, 0:n], pa[pi][:, 0:n], AF.Sigmoid, [bpa[pi], bv], [btB[sl]],
                                 bias=vec[:, V_BX + col:V_BX + col + 1])
                    for (ti, sl, pr, pi) in info:
                        s, n = TILES[ti]
                        self.act(tD[sl][:, 0:n], tA[sl][:, 0:n], AF.Exp, [btA[sl], self.b_lsc], [btD[sl]], scale=self.lsc[:, 0, col:col + 1])
                        self.act(tE[sl][:, 0:n], tA[sl][:, 0:n], AF.Exp, [btA[sl], self.b_lsc], [btE[sl]], scale=self.lsc[:, 1, col:col + 1])
                    for (ti, sl, pr, pi) in info:
                        s, n = TILES[ti]
                        self.act(tE[sl][:, 0:n], tE[sl][:, 0:n], AF.Sqrt, [btE[sl]], [btE[sl]], scale=-1.0, bias=self.cst[:, 5, 0:1])
                    for (ti, sl, pr, pi) in info:
                        s, n = TILES[ti]
                        self.tt("pool", tB[sl][:, 0:n], tB[sl][:, 0:n], tE[sl][:, 0:n], ALU.mult, [btB[sl], btE[sl]], [btB[sl]])
                        self.tt("dve", tB[sl][:, 0:n], tB[sl][:, 0:n], xc[:, s:s + n], ALU.mult, [btB[sl], bxc], [btB[sl]])
                        if dr == 1:
                            if prev_tile is None:
                                init, rd = 0.0, []
                            else:
                                ps_ = TILES[prev_tile[0]][0]
                                init, rd = hsum[:, ps_:ps_ + 1], [bhs]
                            self.scan(rev(hsum[:, s:s + n]), rev(tD[sl][:, 0:n]), rev(tB[sl][:, 0:n]), init,
                                      [btD[sl], btB[sl]] + rd, [bhs])
                        else:
                            if prev_tile is None:
                                init, rd = 0.0, []
                            else:
                                pn = TILES[prev_tile[0]][1]
                                psl = prev_tile[1]
                                init, rd = tF[psl][:, pn - 1:pn], [btF[psl]]
                            if prev_tile is not None and prev_tile[1] == sl:
                                self.copy("dve", hl[:, 0:1], tF[sl][:, TILES[prev_tile[0]][1] - 1:TILES[prev_tile[0]][1]], [btF[sl]], [bhl])
                                init, rd = hl[:, 0:1], [bhl]
                            self.scan(tF[sl][:, 0:n], tD[sl][:, 0:n], tB[sl][:, 0:n], init, [btD[sl], btB[sl]] + rd, [btF[sl]])
                            if not (last and s < CTX):
                                self.tt("dve", tC[sl][:, 0:n], tF[sl][:, 0:n], hsum[:, s:s + n], ALU.add, [btF[sl], bhs], [btC[sl]])
                                o = self.nxt("ob", 3)
                                self.tt("pool", ob[o][:, 0:n], tC[sl][:, 0:n], gl[:, s:s + n], ALU.mult, [btC[sl], bgl], [bob[o]])
                                self.dma("st", self.d_y[:, 8 + c, s:s + n], ob[o][:, 0:n], [bob[o]], [])
                        prev_tile = (ti, sl)

        self.subflush(f"L{l}_mixlru")
        fes.close()
        NQ = 4
        qA, bqA = self.sbn(es, "qA", [128, 512], F32, NQ)
        qB, bqB = self.sbn(es, "qB", [128, 512], F32, NQ)
        qC, bqC = self.sbn(es, "qC", [128, 512], F32, NQ)
        qD, bqD = self.sbn(es, "qD", [128, 512], F32, NQ)
        qE, bqE = self.sbn(es, "qE", [128, 512], F32, NQ)
        qF, bqF = self.sbn(es, "qF", [128, 512], F32, NQ)
        cs_, bcs = self.sbn(es, "cs4", [128, 512], F32, NQ)
        sn_, bsn = self.sbn(es, "sn4", [128, 512], F32, NQ)
        tA, btA, tB, btB, tC, btC, tD, btD, tE, btE, tF, btF = qA, bqA, qB, bqB, qC, bqC, qD, bqD, qE, bqE, qF, bqF
        qcnt = 0
        jobs = [("q", c) for c in range(4)] + [("k", kv) for kv in range(2)]
        wq, bwq = self.sbn(es, "wq", [128, 8, 128], BF16, 7)
        wsrc = self.i_win[l].rearrange("(k p) n -> p k n", p=128)
        for ji, (kind, c) in enumerate(jobs):
            if kind == "q":
                self.dma("pool", wq[ji][:], wsrc[:, :, 512 + c * 128:512 + (c + 1) * 128], (), [bwq[ji]])
            else:
                for hh in range(2):
                    self.dma("pool", wq[ji][:, :, 64 * hh:64 * hh + 64], wsrc[:, :, 1024 + c * 64:1024 + (c + 1) * 64], (), [bwq[ji]])
        self.dma("pool", wq[6][:], wsrc[:, :, 1152:1280], (), [bwq[6]])
        pend = []

        def stage2(sl, n, gcol):
            def f():
                p2 = self.nxt("pa", 4)
                self.mm(pa[p2][:, 0:n], self.blk64(), tB[sl][:, 0:n], True, True, [self.b_cst, btB[sl]], [bpa[p2]])
                self.act(tC[sl][:, 0:n], pa[p2][:, 0:n], AF.Sqrt, [bpa[p2]], [btC[sl]], bias=self.cst[:, 5, 1:2])
                self.recip(tC[sl][:, 0:n], tC[sl][:, 0:n], [btC[sl]], [btC[sl]])
                self.stt(tD[sl][:, 0:n], tA[sl][:, 0:n], vec[:, gcol:gcol + 1], tC[sl][:, 0:n], ALU.mult, ALU.mult,
                         [btA[sl], btC[sl], bv], [btD[sl]])
            return f

        def stage3(sl, n, s, kind, c, isctx, cq):
            def f():
                if kind == "q":
                    o = self.nxt("ob", 3)
                    dst, bdst = ob[o][:, 0:n], bob[o]
                if isctx:
                    if kind == "q":
                        self.copy("pool", dst, tD[sl][:, 0:n], [btD[sl]], [bdst])
                    else:
                        for hh in range(2):
                            self.copy("pool", self.kd[2 * c + hh][64 * hh:64 * hh + 64, s:s + n], tD[sl][64 * hh:64 * hh + 64, 0:n],
                                      [btD[sl]], [self.b_kd[2 * c + hh]])
                else:
                    p3 = self.nxt("pa", 4)
                    self.mm(pa[p3][:, 0:n], self.perm(), tD[sl][:, 0:n], True, True, [self.b_cst, btD[sl]], [bpa[p3]])
                    self.tt("pool", tE[sl][:, 0:n], tD[sl][:, 0:n], cs_[cq][:, 0:n], ALU.mult, [btD[sl], bcs[cq]], [btE[sl]])
                    self.tt("dve", tF[sl][:, 0:n], pa[p3][:, 0:n], sn_[cq][:, 0:n], ALU.mult, [bpa[p3], bsn[cq]], [btF[sl]])
                    if kind == "q":
                        self.tt("pool", dst, tE[sl][:, 0:n], tF[sl][:, 0:n], ALU.add, [btE[sl], btF[sl]], [bdst])
                    else:
                        for hh in range(2):
                            self.tt("dve" if hh else "pool", self.kd[2 * c + hh][64 * hh:64 * hh + 64, s:s + n], tE[sl][64 * hh:64 * hh + 64, 0:n],
                                    tF[sl][64 * hh:64 * hh + 64, 0:n], ALU.add, [btE[sl], btF[sl]], [self.b_kd[2 * c + hh]])
                if kind == "q":
                    self.dma("st", self.d_q[c, :, s:s + n], dst, [bdst], [])
            return f

        def advance():
            for ent in pend:
                ent[2] += 1
            for ent in pend:
                if ent[2] == 1:
                    ent[0]()
            while pend and pend[0][2] >= 2:
                pend.pop(0)[1]()

        tcnt = 0
        for ti, (s, n) in enumerate(TILES):
            isctx = s < CTX
            hb, bhb = self.load_h(ti)
            cq = tcnt % NQ
            tcnt += 1
            if not isctx:
                self.dma("sp", cs_[cq][:, 0:n], self.i_cos[:, s - CTX:s - CTX + n], (), [bcs[cq]])
                self.dma("sp", sn_[cq][:, 0:n], self.i_sin[:, s - CTX:s - CTX + n], (), [bsn[cq]])
            for ji, (kind, c) in enumerate(jobs):
                if kind == "q" and last and isctx:
                    continue
                gcol = V_QN if kind == "q" else V_KN
                wz, bwz = wq[ji], bwq[ji]
                sl = qcnt % NQ
                qcnt += 1
                p = self.nxt("pz", 3)
                self.zmm(pz[p], bpz[p], wz, bwz, hb, bhb, n)
                self.copy("act", tA[sl][:, 0:n], pz[p][:, 0:n], [bpz[p]], [btA[sl]])
                self.act(tB[sl][:, 0:n], pz[p][:, 0:n], AF.Square, [bpz[p]], [btB[sl]])
                pend.append([stage2(sl, n, gcol), stage3(sl, n, s, kind, c, isctx, cq), 0])
                advance()
            for sub in range(n // 128):
                tc = s // 128 + sub
                p = self.nxt("pa", 4)
                for k in range(8):
                    self.mm(pa[p][:, 0:128], hb[:, k, sub * 128:(sub + 1) * 128], wq[6][:, k, :], k == 0, k == 7, [bhb, bwq[6]], [bpa[p]])
                self.copy("act" if sub % 2 else "dve", self.va[:, tc, :, 0:64], pa[p][:, 0:128].rearrange("p (a b) -> p a b", a=2),
                          [bpa[p]], [self.b_va])
        advance()
        advance()
        assert not pend
        if self.dbg:
            for kv in range(2):
                for hh in range(2):
                    self.dma("sp", self.d_k[kv, 64 * hh:64 * hh + 64, :], self.kd[2 * kv + hh][64 * hh:64 * hh + 64, :], [self.b_kd[2 * kv + hh]], [])
            self.dma("sp", self.d_v, self.va[:].rearrange("p a b c -> p (a b c)"), [self.b_va], [])

    def phase_attn(self, es, l, last):
        qt, bqt = self.sbn(es, "qt", [128, 512], BF16, 3)
        pt, bpt = self.sbn(es, "pt", [128, 512], BF16, 6)
        pss, bpss = self.psn(es, "pss", 4)
        pso, bpso = self.psn(es, "pso", 2)
        psb, bpsb = self.psn(es, "psb", 1)
        rs, brs = self.sbn(es, "rs", [128, 512], F32, 2)
        bc, bbc = self.sbn(es, "bc", [64, 512], F32, 2)
        oa, boa = self.sbn(es, "oa", [64, 512], BF16, 3)
        LAG = 2
        work = [(c, ti) for c in range(4) for ti in range(len(TILES)) if not (last and TILES[ti][0] < CTX)]
        qslot = {}

        def load_q(idx):
            c, ti = work[idx]
            s, n = TILES[ti]
            qs = self.nxt("qt", 3)
            self.dma("sp", qt[qs][:, 0:n], self.d_q[c, :, s:s + n], (), [bqt[qs]])
            qslot[idx] = qs

        pending = []

        def make_pv(c, kv, s, n, j, kc, nkc, po, pp):
            def pv():
                self.mm(pso[po][0:65, 0:n], self.va[:, kc, kv, 0:65], pt[pp][:, 0:n], kc == 0, kc == nkc - 1,
                        [self.b_va, bpt[pp]], [bpso[po]])
                if kc == nkc - 1:
                    r = self.nxt("rs", 2)
                    self.recip(rs[r][64:65, 0:n], pso[po][64:65, 0:n], [bpso[po]], [brs[r]])
                    self.mm(psb[0][0:64, 0:n], self.ones()[64:65, 0:64], rs[r][64:65, 0:n], True, True,
                            [self.b_cst, brs[r]], [bpsb[0]])
                    self.copy("act", bc[r][:, 0:n], psb[0][0:64, 0:n], [bpsb[0]], [bbc[r]])
                    o = self.nxt("oa", 3)
                    self.tt("dve", oa[o][:, 0:n], pso[po][0:64, 0:n], bc[r][:, 0:n], ALU.mult, [bpso[po], bbc[r]], [boa[o]])
                    self.dma("st", self.d_y[64 * j:64 * j + 64, 4 + c, s:s + n], oa[o][:, 0:n], [boa[o]], [])
            return pv

        load_q(0)
        for idx, (c, ti) in enumerate(work):
            if idx + 1 < len(work):
                load_q(idx + 1)
            kv = c // 2
            s, n = TILES[ti]
            nkc = 2 if s < CTX else 34
            qs = qslot[idx]
            for j in range(2):
                po = self.nxt("pso", 2)
                for kc in range(nkc):
                    p = self.nxt("pss", 4)
                    self.mm(pss[p][:, 0:n], self.kd[2 * kv + j][:, kc * 128:(kc + 1) * 128],
                            qt[qs][:, 0:n], True, True, [self.b_kd[2 * kv + j], bqt[qs]], [bpss[p]])
                    pp = self.nxt("pt", 6)
                    self.act(pt[pp][:, 0:n], pss[p][:, 0:n], AF.Exp, [bpss[p], self.b_nbias], [bpt[pp]],
                             scale=0.125, bias=self.nbias[:, 0:1])
                    pending.append(make_pv(c, kv, s, n, j, kc, nkc, po, pp))
                    if len(pending) > LAG:
                        pending.pop(0)()
        while pending:
            pending.pop(0)()

    def ln_fm_stages(self, r, br, n, gcol, bcol, outs, bouts, psA, bpsA, psB, bpsB, tmp):
        vec, bv = self.vec, self.b_vec
        sq, bsq, mu, bmu, rstd, brstd = tmp
        if not isinstance(br, (list, tuple)):
            br = [br] * 8
        if not isinstance(bouts, (list, tuple)):
            bouts = [bouts] * 8
        st = []

        def s1():
            for k in range(8):
                self.mm(psA[:, 0:n], self.onesD(), r[k], k == 0, k == 7, [self.b_cst, br[k]], [bpsA])
            for k in range(8):
                s = k % 2
                self.act(sq[s][:, 0:n], r[k], AF.Square, [br[k]], [bsq[s]])
                self.mm(psB[:, 0:n], self.onesD(), sq[s][:, 0:n], k == 0, k == 7, [self.b_cst, bsq[s]], [bpsB])
        st.append(s1)

        def s2():
            self.copy("act", mu[:, 0:n], psA[:, 0:n], [bpsA], [bmu])
            self.act(sq[0][:, 0:n], psA[:, 0:n], AF.Square, [bpsA], [bsq[0]])
            self.tt("dve", rstd[:, 0:n], psB[:, 0:n], sq[0][:, 0:n], ALU.subtract, [bpsB, bsq[0]], [brstd])
            self.act(rstd[:, 0:n], rstd[:, 0:n], AF.Sqrt, [brstd], [brstd], bias=self.cst[:, 5, 2:3])
            self.recip(rstd[:, 0:n], rstd[:, 0:n], [brstd], [brstd])
        st.append(s2)

        def mk(k):
            def f():
                s = k % 2
                self.tt("pool", sq[s][:, 0:n], r[k], mu[:, 0:n], ALU.subtract, [br[k], bmu], [bsq[s]])
                self.tt("dve", sq[s][:, 0:n], sq[s][:, 0:n], rstd[:, 0:n], ALU.mult, [bsq[s], brstd], [bsq[s]])
                self.act(outs[k], sq[s][:, 0:n], AF.Identity, [bsq[s], bv], [bouts[k]],
                         scale=vec[:, gcol + k:gcol + k + 1], bias=vec[:, bcol + k:bcol + k + 1])
            return f
        for k in range(8):
            st.append(mk(k))
        return st

    def ln_fm(self, *a):
        for f in self.ln_fm_stages(*a):
            f()

    def phase_merge(self, es, l, last):
        vec, bv = self.vec, self.b_vec
        wbr, bwbr = self.sb(es, "wbr", [128, 12, 1024], BF16)
        wo, bwo = self.sb(es, "wo", [128, 8, 1024], BF16)
        wgz, bwgz = self.sb(es, "wgz", [128, 8, 3072], BF16)
        xt, bxt = self.sb(es, "mxt", [128, 8, 512], F32)
        ht, bht = self.sb(es, "mht", [128, 8, 512], BF16)
        yt, byt = self.sb(es, "myt", [128, 12, 512], BF16)
        mg, bmg = self.sb(es, "mmg", [128, 8, 512], BF16)
        h2b, bh2b = self.sb(es, "h2b", [128, 8, 512], BF16)
        gs, bgs = self.sbn(es, "gs", [128, 512], F32, 3)
        pr, bpr = self.sbn(es, "pr", [128, 512], F32, 3)
        sq, bsq = self.sbn(es, "lsq", [128, 512], F32, 2)
        mu, bmu = self.sb(es, "lmu", [128, 512], F32)
        rstd, brstd = self.sb(es, "lrs", [128, 512], F32)
        tt_, btt = self.sbn(es, "mtt", [128, 512], F32, 2)
        pg, bpg = self.psn(es, "pg", 2)
        pb, bpb = self.psn(es, "pb", 2)
        po, bpo = self.psn(es, "po", 2)
        pl, bpl = self.psn(es, "pl", 2)
        if last:
            gTt, bgTt = self.sbn(es, "gTt", [8, 512], F32, 2)
            h2f, bh2f = self.sbn(es, "h2f", [128, 512], F32, 2)
            rt, brt = self.sb(es, "rt", [128, 8, 8], F32)
            lgT, blgT = self.sb(es, "lgT", [8, 512], F32)
            L, bL = self.sb(es, "L", [128, 4, 8], F32)
            sm, bsm = self.sb(es, "sm", [128, 64], F32)
            E1, bE1 = self.sb(es, "E1", [128, 4, 8], F32)
            E2, bE2 = self.sb(es, "E2", [128, 4, 8], F32)
            L2, bL2 = self.sb(es, "L2", [128, 4, 8], F32)
            self.dma("sp", rt[:], self.i_router.rearrange("(k p) e -> p k e", p=128), (), [brt])
        wsrc = self.i_win[l].rearrange("(k p) n -> p k n", p=128)
        for cc in range(12):
            self.dma("pool", wbr[:, cc, :], self.i_wbr[l, cc * 128:(cc + 1) * 128, :], (), [bwbr])
        for k in range(8):
            self.dma("pool", wo[:, k, :], self.i_wout[l, k * 128:(k + 1) * 128, :], (), [bwo])
        for k in range(8):
            for hh in range(2):
                self.dma("pool", wgz[:, k, hh * 1536:(hh + 1) * 1536], wsrc[:, k, 2304 + hh * 1536:2304 + (hh + 1) * 1536], (), [bwgz])
        nxt_ = 2
        if nxt_ == 2:
            xt2, _ = self.sb(es, "mxt2", [128, 8, 512], F32)
            xts = [xt, xt2]
        else:
            xts = [xt]
        bxtk = [[Buf(f"xt{i}_{k}") for k in range(8)] for i in range(nxt_)]
        pending = []

        def make_post(xt, bxk, s, n, w):
            r = [xt[:, k, 0:n] for k in range(8)]
            st = self.ln_fm_stages(r, bxk, n, V_LN1G, V_LN1B, r, bxk, pl[0], bpl[0], pl[1], bpl[1], (sq, bsq, mu, bmu, rstd, brstd))
            st.append(lambda: self.dma("st", self.d_x1[:, :, s:s + n], xt[:, :, 0:n], bxk, []))
            state = {}

            def h2k(k):
                def f():
                    if last:
                        if k == 0:
                            state["a"] = self.nxt("pl", 2)
                        a = state["a"]
                        hf = self.nxt("h2f", 2)
                        self.ts("dve", h2f[hf][:, 0:n], xt[:, k, 0:n], self.modp[:, 8 + k, w:w + 1], ALU.mult,
                                [bxk[k], self.b_mod, self.b_modp], [bh2f[hf]], s2=self.mod[:, 24 + k, w:w + 1], op1=ALU.add)
                        self.copy("pool", h2b[:, k, 0:n], h2f[hf][:, 0:n], [bh2f[hf]], [bh2b])
                        self.mm(pl[a][0:8, 0:n], rt[:, k, :], h2f[hf][:, 0:n], k == 0, k == 7, [brt, bh2f[hf]], [bpl[a]])
                    else:
                        self.ts("dve" if k % 2 else "pool", h2b[:, k, 0:n], xt[:, k, 0:n], self.modp[:, 8 + k, w:w + 1], ALU.mult,
                                [bxk[k], self.b_mod, self.b_modp], [bh2b], s2=self.mod[:, 24 + k, w:w + 1], op1=ALU.add)
                return f
            for k in range(8):
                st.append(h2k(k))
            st.append(lambda: self.dma("st", self.d_h2[:, :, s:s + n], h2b[:, :, 0:n], [bh2b], []))
            if last:
                def router():
                    a = state["a"]
                    self.copy("act", lgT[:, 0:n], pl[a][0:8, 0:n], [bpl[a]], [blgT])
                    b_ = self.nxt("pl", 2)
                    for sub in range(4):
                        self.tr(pl[b_][:, sub * 8:(sub + 1) * 8], lgT[0:8, sub * 128:(sub + 1) * 128], self.cst[0:8, 0, 0:8],
                                [blgT, self.b_cst], [bpl[b_]])
                    self.tt("dve", L[:], pl[b_][:, 0:32].rearrange("p (a b) -> p a b", a=4),
                            bass.AP(vec[:, V_RB:V_RB + 8].tensor, vec[:, V_RB:V_RB + 8].offset, [list(vec[:, V_RB:V_RB + 8].ap[0]), [0, 4], [1, 8]]),
                            ALU.add, [bpl[b_], bv], [bL])
                    m1, m2_, d_, e_, p1, p2 = (sm[:, 0:4], sm[:, 4:8], sm[:, 8:12], sm[:, 12:16], sm[:, 16:20], sm[:, 20:24])
                    self.rmax(m1, L[:], [bL], [bsm])
                    self.tt("dve", E1[:], L[:], bcast_last(m1, 8), ALU.is_equal, [bL, bsm], [bE1])
                    self.stt(L2[:], E1[:], -1e30, L[:], ALU.mult, ALU.add, [bE1, bL], [bL2])
                    self.rmax(m2_, L2[:], [bL2], [bsm])
                    self.tt("dve", E2[:], L2[:], bcast_last(m2_, 8), ALU.is_equal, [bL2, bsm], [bE2])
                    self.tt("dve", d_, m2_, m1, ALU.subtract, [bsm], [bsm])
                    self.act(e_, d_, AF.Exp, [bsm], [bsm])
                    self.ts("dve", p1, e_, 1.0, ALU.add, [bsm], [bsm])
                    self.recip(p1, p1, [bsm], [bsm])
                    self.tt("dve", p2, e_, p1, ALU.mult, [bsm], [bsm])
                    self.tt("dve", E1[:], E1[:], bcast_last(p1, 8), ALU.mult, [bE1, bsm], [bE1])
                    self.tt("dve", E2[:], E2[:], bcast_last(p2, 8), ALU.mult, [bE2, bsm], [bE2])
                    self.tt("dve", E1[:], E1[:], E2[:], ALU.add, [bE1, bE2], [bE1])
                    c_ = self.nxt("pl", 2)
                    for sub in range(4):
                        self.tr(pl[c_][0:8, sub * 128:(sub + 1) * 128], E1[:, sub, :], self.ident(), [bE1, self.b_cst], [bpl[c_]])
                    gs_ = self.nxt("gTt", 2)
                    self.copy("act", gTt[gs_][:, 0:n], pl[c_][0:8, 0:n], [bpl[c_]], [bgTt[gs_]])
                    self.dma("st", self.d_gate[:, s - CTX:s - CTX + n], gTt[gs_][:, 0:n], [bgTt[gs_]], [])
                st.append(router)
            return st

        tnum = 0
        for ti, (s, n) in enumerate(TILES):
            isctx = s < CTX
            if isctx and last:
                continue
            w = 1 if isctx else 0
            xi = tnum % nxt_
            tnum += 1
            xt, bxk = xts[xi], bxtk[xi]
            self.dma("sp", ht[:, :, 0:n], self.d_h[:, :, s:s + n], (), [bht])
            self.dma("sp", yt[:, :, 0:n], self.d_y[:, :, s:s + n], (), [byt])
            if nxt_ == 2:
                self.dma("sp", xt[:, :, 0:n], self.xsrc(l, s, n), (), bxk)
            for j in range(8):
                prods = []
                for nb in range(3):
                    a = self.nxt("pg", 2)
                    for k in range(8):
                        self.mm(pg[a][:, 0:n], wgz[:, k, nb * 1024 + j * 128:nb * 1024 + (j + 1) * 128], ht[:, k, 0:n], k == 0, k == 7, [bwgz, bht], [bpg[a]])
                    g_ = self.nxt("gs", 3)
                    self.act(gs[g_][:, 0:n], pg[a][:, 0:n], AF.Sigmoid, [bpg[a], bv], [bgs[g_]],
                             bias=vec[:, V_BMERGE + nb * 8 + j:V_BMERGE + nb * 8 + j + 1])
                    b_ = self.nxt("pb", 2)
                    for cc in range(4):
                        self.mm(pb[b_][:, 0:n], wbr[:, nb * 4 + cc, j * 128:(j + 1) * 128], yt[:, nb * 4 + cc, 0:n],
                                cc == 0, cc == 3, [bwbr, byt], [bpb[b_]])
                    p_ = self.nxt("pr", 3)
                    self.tt("dve", pr[p_][:, 0:n], pb[b_][:, 0:n], gs[g_][:, 0:n], ALU.mult, [bpb[b_], bgs[g_]], [bpr[p_]])
                    prods.append((pr[p_], bpr[p_]))
                self.tt("pool", prods[0][0][:, 0:n], prods[0][0][:, 0:n], prods[1][0][:, 0:n], ALU.add,
                        [prods[0][1], prods[1][1]], [prods[0][1]])
                self.tt("dve", mg[:, j, 0:n], prods[0][0][:, 0:n], prods[2][0][:, 0:n], ALU.add, [prods[0][1], prods[2][1]], [bmg])
                npop = -(-len(pending) // (8 - j))
                for _ in range(npop):
                    pending.pop(0)()
            assert not pending
            if nxt_ == 1:
                self.dma("sp", xt[:, :, 0:n], self.xsrc(l, s, n), (), bxk)
            for j2 in range(8):
                a = self.nxt("po", 2)
                for j in range(8):
                    self.mm(po[a][:, 0:n], wo[:, j, j2 * 128:(j2 + 1) * 128], mg[:, j, 0:n], j == 0, j == 7, [bwo, bmg], [bpo[a]])
                t_ = self.nxt("mtt", 2)
                self.act(tt_[t_][:, 0:n], po[a][:, 0:n], AF.Copy, [bpo[a], self.b_mod], [btt[t_]], scale=self.mod[:, 16 + j2, w:w + 1])
                self.stt(xt[:, j2, 0:n], xt[:, j2, 0:n], ALPHA, tt_[t_][:, 0:n], ALU.mult, ALU.add, [bxk[j2], btt[t_]], [bxk[j2]])
            pending.extend(make_post(xt, bxk, s, n, w))
        while pending:
            pending.pop(0)()

    def phase_ffn(self, es, l, last):
        vec, bv = self.vec, self.b_vec
        moe = (l % 2 == 1)
        assert not moe, "dense-evaluated MoE path removed (weights are host-laid-out for the sparse path)"
        nF = (D_EXP if moe else D_FF) // 128
        nE = N_EXP if moe else 1
        if last:
            sts = [(CTX + 1024 * i, 1024) for i in range(4)]
        else:
            sts = [(0, 256)] + [(CTX + 1024 * i, 1024) for i in range(4)]
        if moe:
            self.gT, self.b_gT = self.sb(es, "gT3", [8, SEQ], F32)
            self.dma("sp", self.gT[:], self.d_gate, (), [self.b_gT])
        h2, bh2 = self.sb(es, "fh2", [128, 8, 1024], BF16)
        A, bA = self.sb(es, "fA", [128, nF, 1024], BF16)
        acc, _ = self.sb(es, "facc", [128, 8, 1024], F32)
        bacck = [Buf(f"facc{k}") for k in range(8)]
        fpend = []
        wgu, bwgu = self.sbn(es, "wgu", [128, 8, 2, 512], BF16, 2)
        wd, bwd = self.sbn(es, "wd", [128, nF, 128], BF16, 2)
        sg, bsg = self.sbn(es, "sg", [128, 512], F32, 2)
        gb, bgb = self.sbn(es, "gb", [128, 1024], F32, 2)
        x1, bx1 = self.sbn(es, "fx1", [128, 1024], F32, 2)
        tmp, btmp = self.sbn(es, "ftmp", [128, 512], F32, 2)
        sq, bsq = self.sbn(es, "fsq", [128, 512], F32, 2)
        mu, bmu = self.sb(es, "fmu", [128, 512], F32)
        rstd, brstd = self.sb(es, "frs", [128, 512], F32)
        pG, bpG = self.psn(es, "pG", 2)
        pU, bpU = self.psn(es, "pU", 2)
        pY, bpY = self.psn(es, "pY", 2)
        pL, bpL = self.psn(es, "pL", 2)
        for (s, N) in sts:
            isctx = s < CTX
            w = 1 if isctx else 0
            subs = [(o, min(512, N - o)) for o in range(0, N, 512)]
            self.dma("sp", h2[:, :, 0:N], self.d_h2[:, :, s:s + N], (), [bh2])
            for e in range(nE):
                if moe:
                    Wg, Wu, Wd = self.i_mg[e], self.i_mu[e], self.i_md[e]
                else:
                    Wg, Wu, Wd = self.i_fg, self.i_fu, self.i_fd
                Wg = Wg.rearrange("(k p) f -> p k f", p=128)
                Wu = Wu.rearrange("(k p) f -> p k f", p=128)
                Wd = Wd.rearrange("(g p) n -> p g n", p=128)
                if moe:
                    g_ = self.nxt("gb", 2)
                    for (o, n) in subs:
                        a = self.nxt("pL", 2)
                        self.mm(pL[a][:, 0:n], self.sel[0:8, e, :], self.gT[0:8, s - CTX + o:s - CTX + o + n], True, True,
                                [self.b_sel, self.b_gT], [bpL[a]])
                        self.copy("act", gb[g_][:, o:o + n], pL[a][:, 0:n], [bpL[a]], [bgb[g_]])
                for g0 in range(0, nF, 4):
                    ng = min(4, nF - g0)
                    ws = self.nxt("wgu", 2)
                    self.dma("pool", wgu[ws][:, :, 0, 0:ng * 128], Wg[:, :, g0 * 128:(g0 + ng) * 128], (), [bwgu[ws]])
                    self.dma("pool", wgu[ws][:, :, 1, 0:ng * 128], Wu[:, :, g0 * 128:(g0 + ng) * 128], (), [bwgu[ws]])
                    for fi in range(ng):
                        fg = g0 + fi
                        for (o, n) in subs:
                            a = self.nxt("pG", 2)
                            for k in range(8):
                                self.mm(pG[a][:, 0:n], wgu[ws][:, k, 0, fi * 128:(fi + 1) * 128], h2[:, k, o:o + n], k == 0, k == 7, [bwgu[ws], bh2], [bpG[a]])
                            b_ = self.nxt("pU", 2)
                            for k in range(8):
                                self.mm(pU[b_][:, 0:n], wgu[ws][:, k, 1, fi * 128:(fi + 1) * 128], h2[:, k, o:o + n], k == 0, k == 7, [bwgu[ws], bh2], [bpU[b_]])
                            s_ = self.nxt("sg", 2)
                            self.act(sg[s_][:, 0:n], pG[a][:, 0:n], AF.Silu, [bpG[a]], [bsg[s_]])
                            self.tt("dve", A[:, fg, o:o + n], pU[b_][:, 0:n], sg[s_][:, 0:n], ALU.mult, [bpU[b_], bsg[s_]], [bA])
                        if fpend:
                            for _ in range(-(-len(fpend) // max(1, (nF - 2 - fg)))):
                                if fpend:
                                    fpend.pop(0)()
                while fpend:
                    fpend.pop(0)()
                for j2 in range(8):
                    ds = self.nxt("wd", 2)
                    self.dma("pool", wd[ds][:], Wd[:, :, j2 * 128:(j2 + 1) * 128], (), [bwd[ds]])
                    for (o, n) in subs:
                        a = self.nxt("pY", 2)
                        for fg in range(nF):
                            self.mm(pY[a][:, 0:n], wd[ds][:, fg, :], A[:, fg, o:o + n], fg == 0, fg == nF - 1, [bwd[ds], bA], [bpY[a]])
                        if not moe:
                            self.copy("act", acc[:, j2, o:o + n], pY[a][:, 0:n], [bpY[a]], [bacck[j2]])
                        elif e == 0:
                            self.tt("dve", acc[:, j2, o:o + n], pY[a][:, 0:n], gb[g_][:, o:o + n], ALU.mult, [bpY[a], bgb[g_]], [bacc])
                        else:
                            t_ = self.nxt("ftmp", 2)
                            self.tt("dve", tmp[t_][:, 0:n], pY[a][:, 0:n], gb[g_][:, o:o + n], ALU.mult, [bpY[a], bgb[g_]], [btmp[t_]])
                            self.tt("pool", acc[:, j2, o:o + n], acc[:, j2, o:o + n], tmp[t_][:, 0:n], ALU.add, [bacc, btmp[t_]], [bacc])
            fpend.extend(self.ffn_epilogue_stages(s, N, subs, w, last, acc, bacck, x1, bx1, pL, bpL, (sq, bsq, mu, bmu, rstd, brstd)))
        while fpend:
            fpend.pop(0)()

    def ffn_epilogue_stages(self, s, N, subs, w, last, acc, bacc, x1, bx1, pL, bpL, lnt):
        if not isinstance(bacc, (list, tuple)):
            bacc = [bacc] * 8
        st = []

        def res(j2):
            def f():
                xs_ = self.nxt("fx1", 2)
                self.dma("sp", x1[xs_][:, 0:N], self.d_x1[:, j2, s:s + N], (), [bx1[xs_]])
                self.ts("dve", acc[:, j2, 0:N], acc[:, j2, 0:N], self.mod[:, 40 + j2, w:w + 1], ALU.mult, [bacc[j2], self.b_mod], [bacc[j2]])
                self.stt(acc[:, j2, 0:N], x1[xs_][:, 0:N], ALPHA, acc[:, j2, 0:N], ALU.mult, ALU.add, [bx1[xs_], bacc[j2]], [bacc[j2]])
            return f
        for j2 in range(8):
            st.append(res(j2))
        for (o, n) in subs:
            r = [acc[:, k, o:o + n] for k in range(8)]
            st.extend(self.ln_fm_stages(r, bacc, n, V_LN2G, V_LN2B, r, bacc, pL[0], bpL[0], pL[1], bpL[1], lnt))

        def store():
            if last:
                self.dma("st", self.d_out[:, :, s - CTX:s - CTX + N], acc[:, :, 0:N], bacc, [])
            else:
                self.dma("st", self.d_xs1[:, :, s:s + N], acc[:, :, 0:N], bacc, [])
        st.append(store)
        return st

    def ffn_epilogue(self, *a):
        for f in self.ffn_epilogue_stages(*a):
            f()

    def phase_route(self, es, l, last):
        gT, bgT = self.sb(es, "gT2", [8, SEQ], F32)
        self.dma("sp", gT[:], self.d_gate, (), [bgT])
        ones8, bo8 = self.sb(es, "ones8", [8, SEQ], F32)
        selT, bsel = self.sb(es, "selT", [8, SEQ], F32)
        incl, binc = self.sb(es, "incl", [8, SEQ], F32)
        dst, bdst = self.sb(es, "dstT", [8, SEQ], F32)
        mc, bmc = self.sb(es, "mc", [8, 64], F32)
        mc2, bmc2 = self.sb(es, "mc2", [128, 32], F32)
        sm, bsm = self.sb(es, "rsm", [8, 64], F32)
        ebf, bebf = self.sb(es, "ebf", [128, NBLK], F32)
        tf, btf = self.sb(es, "rtf", [128, NBLK * 28], F32)
        GT, bGT = self.sb(es, "GTk", [128, 32, 8], F32)
        DT, bDT = self.sb(es, "DTk", [128, 32, 8], F32)
        E1, bE1 = self.sb(es, "rE1", [128, 32, 8], F32)
        E2, bE2 = self.sb(es, "rE2", [128, 32, 8], F32)
        TM, bTM = self.sb(es, "rTM", [128, 32, 8], F32)
        r32, br32 = self.sb(es, "r32", [128, 4, 32], F32)
        ps, bps = self.psn(es, "rps", 3)
        self.dma("sp", mc[:], self.i_mc, (), [bmc])
        self.dma("sp", mc2[:], self.i_mc2, (), [bmc2])
        self.memset("dve", ones8[:], 1.0, [bo8])
        self.ts("dve", selT[:], gT[:], 0.0, ALU.is_gt, [bgT], [bsel])
        self.scan(incl[:], ones8[:], selT[:], 0.0, [bo8, bsel], [binc])
        cnt = incl[:, SEQ - 1:SEQ]
        self.ts("dve", sm[:, 0:8], mc[:, 0:8], cnt, ALU.is_lt, [bmc, binc], [bsm])
        self.S.add("dve", lambda e: e.tensor_reduce(out=sm[:, 8:9], in_=sm[:, 0:8], op=ALU.add, axis=AX.X), [bsm], [bsm])
        self.copy("dve", sm[:, 9:10], sm[:, 8:9], [bsm], [bsm])
        self.mm(ps[0][0:8, 0:2], mc[:, 32:40], sm[:, 8:10], True, True, [bmc, bsm], [bps[0]])
        self.ts("dve", sm[:, 10:11], ps[0][0:8, 0:1], float(MB), ALU.mult, [bps[0]], [bsm])
        self.stt(sm[:, 11:12], sm[:, 8:9], float(MB), sm[:, 10:11], ALU.mult, ALU.add, [bsm], [bsm])
        self.tt("dve", dst[:], incl[:], selT[:], ALU.subtract, [binc, bsel], [bdst])
        self.ts("dve", dst[:], dst[:], sm[:, 10:11], ALU.add, [bdst, bsm], [bdst])
        self.ts("dve", sm[:, 16:16 + NBLK], mc[:, 8:8 + NBLK], sm[:, 11:12], ALU.is_ge, [bmc, bsm], [bsm])
        self.mm(ps[1][:, 0:NBLK], self.ones()[0:8, :], sm[:, 16:16 + NBLK], True, True, [self.b_cst, bsm], [bps[1]])
        self.ts("dve", ebf[:], ps[1][:, 0:NBLK], 7.0, ALU.min, [bps[1]], [bebf])
        self.ts("dve", ebf[:], ebf[:], 2048.0, ALU.mult, [bebf, bmc2], [bebf], s2=mc2[:, 0:1], op1=ALU.add)
        m2ap = mc2[:, 2:18]
        self.tt("dve", tf[:, 0:NBLK * 16].rearrange("p (b f) -> p b f", f=16), bcast_last(ebf[:], 16),
                bass.AP(m2ap.tensor, m2ap.offset, [list(m2ap.ap[0]), [0, NBLK], [1, 16]]), ALU.add, [bebf, bmc2], [btf])
        self.copy("dve", self.IG[:].rearrange("p b f -> p (b f)"), tf[:, 0:NBLK * 16], [btf], [self.b_IG])
        for c in range(32):
            self.tr(ps[2][:, c * 8:(c + 1) * 8], gT[0:8, c * 128:(c + 1) * 128], self.cst[0:8, 0, 0:8], [bgT, self.b_cst], [bps[2]])
        self.copy("act", GT[:].rearrange("p a b -> p (a b)"), ps[2][:, 0:256], [bps[2]], [bGT])
        for c in range(32):
            self.tr(ps[0][:, c * 8:(c + 1) * 8], dst[0:8, c * 128:(c + 1) * 128], self.cst[0:8, 0, 0:8], [bdst, self.b_cst], [bps[0]])
        self.copy("act", DT[:].rearrange("p a b -> p (a b)"), ps[0][:, 0:256], [bps[0]], [bDT])
        m1 = r32[:, 0, :]
        self.rmax(m1, GT[:], [bGT], [br32])
        self.tt("dve", E1[:], GT[:], bcast_last(m1, 8), ALU.is_equal, [bGT, br32], [bE1])
        self.ts("dve", E2[:], GT[:], 0.0, ALU.is_gt, [bGT], [bE2])
        self.tt("dve", E2[:], E2[:], E1[:], ALU.subtract, [bE2, bE1], [bE2])
        rsum = lambda out, in_, rd: self.S.add("dve", lambda e: e.tensor_reduce(out=out, in_=in_, op=ALU.add, axis=AX.X), rd, [br32])
        self.copy("dve", self.P12[:, 0, :], m1, [br32], [self.b_P12])
        self.tt("dve", TM[:], GT[:], E2[:], ALU.mult, [bGT, bE2], [bTM])
        rsum(r32[:, 1, :], TM[:], [bTM])
        self.copy("dve", self.P12[:, 1, :], r32[:, 1, :], [br32], [self.b_P12])
        self.tt("dve", TM[:], DT[:], E1[:], ALU.mult, [bDT, bE1], [bTM])
        rsum(r32[:, 2, :], TM[:], [bTM])
        self.copy("dve", self.D12[:, 0, :], r32[:, 2, :], [br32], [self.b_D12])
        self.tt("dve", TM[:], DT[:], E2[:], ALU.mult, [bDT, bE2], [bTM])
        rsum(r32[:, 3, :], TM[:], [bTM])
        self.copy("dve", self.D12[:, 1, :], r32[:, 3, :], [br32], [self.b_D12])
        if self.dbg:
            dd = self.nc.dram_tensor("dbg_D12", [128, 64], I32, kind="ExternalOutput").ap()
            dg = self.nc.dram_tensor("dbg_IG", [128, 16 * NBLK], I32, kind="ExternalOutput").ap()
            dp = self.nc.dram_tensor("dbg_P12", [128, 64], F32, kind="ExternalOutput").ap()
            ds = self.nc.dram_tensor("dbg_sm", [8, 64], F32, kind="ExternalOutput").ap()
            self.dma("sp", dd, self.D12[:].rearrange("p a b -> p (a b)"), [self.b_D12], [])
            self.dma("sp", dg, self.IG[:].rearrange("p a b -> p (a b)"), [self.b_IG], [])
            self.dma("sp", dp, self.P12[:].rearrange("p a b -> p (a b)"), [self.b_P12], [])
            self.dma("sp", ds, sm[:], [bsm], [])

    def phase_blocks(self, es, l, last):
        nF = D_EXP // 128
        self.st_dve_q = "sp"
        idb, bidb = self.sb(es, "idb", [128, 128], BF16)
        rows, brows = self.sbn(es, "rows", [128, 1024], BF16, 2)
        XT, bXT = self.sbn(es, "XT", [128, 8, MB], BF16, 2)
        A, bA = self.sb(es, "bA", [128, nF, MB], BF16)
        wg, bwg = self.sbn(es, "bwg", [128, 8 * 896], BF16, 2)
        wu, bwu = self.sbn(es, "bwu", [128, 8 * 896], BF16, 2)
        wd, bwd = self.sbn(es, "bwd", [128, nF * 256], BF16, 2)
        Yr, bYr = self.sbn(es, "Yr", [128, 4, 1024], F32, 1)
        h4, bh4 = XT, bXT
        sg, bsg = self.sbn(es, "bsg", [128, MB], F32, 2)
        ptb, bptb = self.psn(es, "ptb", 2, shape=(128, 1024), dt=BF16)
        pG, bpG = self.psn(es, "bpG", 2)
        pU, bpU = self.psn(es, "bpU", 2)
        pY, bpY = self.psn(es, "bpY", 2)
        self.copy("dve", idb[:], self.ident(), [self.b_cst], [bidb])
        bxs = [Buf(f"xs{c}") for c in range(32)]

        def gather(dst_ap, src, idx, reads, writes):
            return self.S.add("pool", lambda e: e.indirect_dma_start(out=dst_ap, out_offset=None, in_=src,
                                                                     in_offset=IndirectOffsetOnAxis(ap=idx, axis=0)),
                              reads, writes, dma=True)

        wslot = {}

        def load_w1(b, cg):
            ws = self.nxt("bwg", 2)
            for kp in range(4):
                idx = self.IG[:, b, cg * 4 + kp:cg * 4 + kp + 1].bitcast(U32)
                gather(wg[ws][:, kp * 1792:(kp + 1) * 1792], self.i_mg, idx, [self.b_IG], [bwg[ws]])
                gather(wu[ws][:, kp * 1792:(kp + 1) * 1792], self.i_mu, idx, [self.b_IG], [bwu[ws]])
            wslot[(b, cg)] = ws

        load_w1(0, 0)
        load_w1(0, 1)
        for g4 in range(8):
            hs = g4 % 2
            self.dma("sp", h4[hs][:], self.d_h2[:, :, CTX + g4 * 512:CTX + (g4 + 1) * 512], (), [bh4[hs]])
            for c4 in range(4):
                c = g4 * 4 + c4
                pp = self.nxt("ptb", 2)
                for k in range(8):
                    self.tr(ptb[pp][:, k * 128:(k + 1) * 128], h4[hs][:, k, c4 * 128:(c4 + 1) * 128], idb[:], [bh4[hs], bidb], [bptb[pp]])
                rs_ = self.nxt("rows", 2)
                self.copy("act" if c % 2 else "dve", rows[rs_][:], ptb[pp][:], [bptb[pp]], [brows[rs_]])
                for t2 in range(2):
                    idx = self.D12[:, t2, c:c + 1].bitcast(U32)
                    self.S.add("pool", lambda e, idx=idx, src=rows[rs_]: e.indirect_dma_start(
                        out=self.d_xs, out_offset=IndirectOffsetOnAxis(ap=idx, axis=0), in_=src[:], in_offset=None),
                        [brows[rs_], self.b_D12], [bxs[c]], dma=True)
        bys = Buf("ys")
        for b in range(NBLK_RUN if BLK_STAGE > 0 else 0):
            xs_ = b % 2
            for c4 in range(4):
                rs_ = self.nxt("rows", 2)
                self.dma("sp", rows[rs_][:], self.d_xs[b * MB + c4 * 128:b * MB + (c4 + 1) * 128, :], bxs, [brows[rs_]])
                pp = self.nxt("ptb", 2)
                for k in range(8):
                    self.tr(ptb[pp][:, k * 128:(k + 1) * 128], rows[rs_][:, k * 128:(k + 1) * 128], idb[:], [brows[rs_], bidb], [bptb[pp]])
                self.copy("act" if c4 % 2 else "dve", XT[xs_][:, :, c4 * 128:(c4 + 1) * 128],
                          ptb[pp][:].rearrange("p (k t) -> p k t", k=8), [bptb[pp]], [bXT[xs_]])
            for cg in range(4 if BLK_STAGE > 1 else 0):
                if (b, cg) not in wslot:
                    load_w1(b, cg)
                ws = wslot[(b, cg)]
                for f7 in range(7):
                    fg = cg * 7 + f7
                    a = self.nxt("bpG", 2)
                    for k in range(8):
                        self.mm(pG[a][:, 0:MB], wg[ws][:, k * 896 + f7 * 128:k * 896 + (f7 + 1) * 128], XT[xs_][:, k, :], k == 0, k == 7, [bwg[ws], bXT[xs_]], [bpG[a]])
                    b_ = self.nxt("bpU", 2)
                    for k in range(8):
                        self.mm(pU[b_][:, 0:MB], wu[ws][:, k * 896 + f7 * 128:k * 896 + (f7 + 1) * 128], XT[xs_][:, k, :], k == 0, k == 7, [bwu[ws], bXT[xs_]], [bpU[b_]])
                    s_ = self.nxt("bsg", 2)
                    self.act(sg[s_][:], pG[a][:, 0:MB], AF.Silu, [bpG[a]], [bsg[s_]])
                    self.tt("dve", A[:, fg, :], pU[b_][:, 0:MB], sg[s_][:], ALU.mult, [bpU[b_], bsg[s_]], [bA])
            ys_ = 0
            for dq in range(4 if BLK_STAGE > 2 else 0):
                ds_ = self.nxt("bwd", 2)
                for fq in range(4):
                    idx = self.IG[:, b, dq * 4 + fq:dq * 4 + fq + 1].bitcast(U32)
                    gather(wd[ds_][:, fq * 1792:(fq + 1) * 1792], self.i_md, idx, [self.b_IG], [bwd[ds_]])
                for c4 in range(4):
                    a = self.nxt("bpY", 2)
                    for fg in range(nF):
                        self.mm(pY[a][:, 0:256], A[:, fg, c4 * 128:(c4 + 1) * 128], wd[ds_][:, fg * 256:(fg + 1) * 256], fg == 0, fg == nF - 1, [bA, bwd[ds_]], [bpY[a]])
                    self.copy("act" if c4 % 2 else "dve", Yr[ys_][:, c4, dq * 256:(dq + 1) * 256], pY[a][:, 0:256], [bpY[a]], [bYr[ys_]])
            self.dma("st", self.d_ys[b * MB:(b + 1) * MB, :].rearrange("(c p) d -> p c d", p=128), Yr[ys_][:], [bYr[ys_]], [bys])
        self.b_ys = bys
        self.st_dve_q = "pool"

    def phase_comb(self, es, l, last):
        accs = [self.sb(es, f"cacc{i}", [128, 8, 1024], F32)[0] for i in range(2)]
        baccs = [[Buf(f"cacc{i}_{k}") for k in range(8)] for i in range(2)]
        Y1, bY1 = self.sbn(es, "cY1", [128, 1024], F32, 2)
        Y2, bY2 = self.sbn(es, "cY2", [128, 1024], F32, 2)
        x1, bx1 = self.sbn(es, "cx1", [128, 1024], F32, 2)
        sq, bsq = self.sbn(es, "csq", [128, 512], F32, 2)
        mu, bmu = self.sb(es, "cmu", [128, 512], F32)
        rstd, brstd = self.sb(es, "crs", [128, 512], F32)
        pT, bpT = self.psn(es, "cpT", 2)
        pL, bpL = self.psn(es, "cpL", 2)
        pend = []
        for st in range(4):
            s, N = CTX + 1024 * st, 1024
            acc, bacc = accs[st % 2], baccs[st % 2]
            for c8 in range(8):
                c = st * 8 + c8
                ys_ = c % 2
                for t2, (Yt, bYt) in enumerate(((Y1, bY1), (Y2, bY2))):
                    idx = self.D12[:, t2, c:c + 1].bitcast(U32)
                    self.S.add("pool", lambda e, idx=idx, dst=Yt[ys_]: e.indirect_dma_start(
                        out=dst[:], out_offset=None, in_=self.d_ys, in_offset=IndirectOffsetOnAxis(ap=idx, axis=0)),
                        [self.b_D12], [bYt[ys_]], dma=True)
                self.ts("dve", Y1[ys_][:], Y1[ys_][:], self.P12[:, 0, c:c + 1], ALU.mult, [bY1[ys_], self.b_P12], [bY1[ys_]])
                self.stt(Y1[ys_][:], Y2[ys_][:], self.P12[:, 1, c:c + 1], Y1[ys_][:], ALU.mult, ALU.add,
                         [bY2[ys_], bY1[ys_], self.b_P12], [bY1[ys_]])
                for kq in range(2):
                    a = self.nxt("cpT", 2)
                    for k4 in range(4):
                        k = kq * 4 + k4
                        self.tr(pT[a][:, k4 * 128:(k4 + 1) * 128], Y1[ys_][:, k * 128:(k + 1) * 128], self.ident(), [bY1[ys_], self.b_cst], [bpT[a]])
                    self.copy("act", acc[:, kq * 4:(kq + 1) * 4, c8 * 128:(c8 + 1) * 128], pT[a][:].rearrange("p (k t) -> p k t", k=4),
                              [bpT[a]], bacc[kq * 4:(kq + 1) * 4])
                for _ in range(-(-len(pend) // (8 - c8))):
                    pend.pop(0)()
            subs = [(0, 512), (512, 512)]
            pend.extend(self.ffn_epilogue_stages(s, N, subs, 0, last, acc, bacc, x1, bx1, pL, bpL, (sq, bsq, mu, bmu, rstd, brstd)))
        while pend:
            pend.pop(0)()


def fm(v):
    v = np.asarray(v, dtype=np.float32)
    n = v.shape[-1] // 128
    return np.swapaxes(v.reshape(v.shape[:-1] + (n, 128)), -1, -2)


def host_consts():
    c = np.zeros((6, 128, 128), np.float32)
    c[0] = np.eye(128)
    c[1] = 1.0 / 1024
    for h in range(2):
        c[2, h * 64:(h + 1) * 64, h * 64:(h + 1) * 64] = 1.0 / 64
    for m in range(128):
        d = m % 32
        partner = m + 16 if d < 16 else m - 16
        c[3, partner, m] = 1.0
    c[4] = 1.0
    c[5, :, 0] = 1.0
    c[5, :, 1] = RMS_EPS
    c[5, :, 2] = LN_EPS
    t = np.arange(SEQ)
    row = (t // 64).astype(np.float32)
    col = (t % 64).astype(np.float32)
    nf = 16
    inv = (10000.0 ** (-np.arange(nf, dtype=np.float32) / nf)).astype(np.float32)
    cosT = np.zeros((128, SEQ), np.float32)
    sinT = np.zeros((128, SEQ), np.float32)
    for p in range(128):
        d = p % 64
        pos = row if d < 32 else col
        ang = (pos * inv[d % 16]).astype(np.float32)
        cosT[p] = np.cos(ang)
        sinT[p] = -np.sin(ang) if (d % 32) < 16 else np.sin(ang)
    invcnt = np.zeros((4, PADW), np.float32)
    for g, wdw in enumerate(POOL_WINDOWS):
        lo = wdw // 2
        for (L, base) in ((CTX, 8), (SEQ, 8 + CTX + 16)):
            tt = np.arange(L)
            start = np.clip(tt - lo, 0, L)
            end = np.clip(tt - lo + wdw, 0, L)
            invcnt[g, base:base + L] = 1.0 / (end - start).astype(np.float32)
    sel8 = np.zeros((8, 8, 128), np.float32)
    for e in range(8):
        sel8[e, e, :] = 1.0
    mc = np.zeros((8, 64), np.float32)
    mc[:, 0:8] = (np.arange(8) * MB)[None, :]
    mc[:, 8:8 + NBLK] = (np.arange(NBLK) * MB)[None, :]
    for e1 in range(8):
        for e2 in range(8):
            mc[e1, 32 + e2] = 1.0 if e1 < e2 else 0.0
    mc2 = np.zeros((128, 32), np.float32)
    mc2[:, 0] = np.arange(128) * 16
    for j in range(16):
        mc2[:, 2 + j] = j
    return c, cosT, sinT, invcnt, sel8, mc, mc2


def relayout_gu(w):
    w = np.asarray(w, dtype=np.float32).reshape(N_EXP, 8, 128, 4, 896)
    w = np.transpose(w, (0, 2, 3, 1, 4))
    return np.ascontiguousarray(w).reshape(N_EXP * 128 * 16, 1792)


def relayout_d(w):
    w = np.asarray(w, dtype=np.float32).reshape(N_EXP, 28, 128, 4, 256)
    w = np.transpose(w, (0, 2, 3, 1, 4))
    return np.ascontiguousarray(w).reshape(N_EXP * 128 * 16, 1792)


def prep_inputs(inp):
    f32 = lambda a: np.ascontiguousarray(np.asarray(a, dtype=np.float32))
    consts, cosT, sinT, invcnt, sel8, mc, mc2 = host_consts()
    vecs = np.zeros((DEPTH, 128, NV), np.float32)
    for l in range(DEPTH):
        vecs[l, :, V_BMOD:V_BMOD + 48] = fm(inp["b_mod"][l])
        vecs[l, :, V_BMERGE:V_BMERGE + 24] = fm(np.asarray(inp["b_merge"][l]).reshape(-1))
        vecs[l, :, V_PSCALE:V_PSCALE + 4] = fm(inp["pool_scale"][l])
        cw = fm(inp["conv_w"][l])
        vecs[l, :, V_CONVW:V_CONVW + 16] = np.transpose(cw, (1, 2, 0)).reshape(128, 16)
        vecs[l, :, V_CONVB:V_CONVB + 4] = fm(inp["conv_b"][l])
        for nm, off in (("lru_ba", V_BA), ("lru_bx", V_BX), ("lru_lambda", V_LAM)):
            a = fm(inp[nm][l])
            vecs[l, :, off:off + 8] = np.transpose(a, (1, 0, 2)).reshape(128, 8)
        vecs[l, :, V_QN] = np.tile(np.asarray(inp["q_norm"][l], np.float32), 2)
        vecs[l, :, V_KN] = np.tile(np.asarray(inp["k_norm"][l], np.float32), 2)
        vecs[l, :, V_LN1G:V_LN1G + 8] = fm(inp["ln1_g"][l])
        vecs[l, :, V_LN1B:V_LN1B + 8] = fm(inp["ln1_b"][l])
        vecs[l, :, V_LN2G:V_LN2G + 8] = fm(inp["ln2_g"][l])
        vecs[l, :, V_LN2B:V_LN2B + 8] = fm(inp["ln2_b"][l])
        vecs[l, :, V_RB:V_RB + 8] = np.asarray(inp["moe_router_b"][0], np.float32)[None, :]
    lru_bd = np.zeros((DEPTH, 4, 4, 128, 128), np.float32)
    for l in range(DEPTH):
        for c in range(4):
            for dr in range(2):
                for wi, nm in enumerate(("lru_wa", "lru_wx")):
                    for hh in range(2):
                        lru_bd[l, c, 2 * dr + wi, hh * 64:(hh + 1) * 64, hh * 64:(hh + 1) * 64] = inp[nm][l][dr][2 * c + hh]
    shared = {
        "vecs": vecs, "w_mod": f32(inp["w_mod"]), "w_in": f32(inp["w_in"]), "pool_w": f32(inp["pool_w"]),
        "lru_bd": lru_bd, "w_branch": f32(np.asarray(inp["w_branch"]).reshape(DEPTH, 1536, D)), "w_out": f32(inp["w_out"]),
        "ffn_w_gate": f32(inp["ffn_w_gate"][0]), "ffn_w_up": f32(inp["ffn_w_up"][0]), "ffn_w_down": f32(inp["ffn_w_down"][0]),
        "moe_router": f32(inp["moe_router"][0]), "moe_g2": relayout_gu(inp["moe_w_gate"][0]), "moe_u2": relayout_gu(inp["moe_w_up"][0]),
        "moe_d2": relayout_d(inp["moe_w_down"][0]), "consts": consts, "cosT": cosT, "sinT": sinT, "invcnt": invcnt, "sel8": sel8, "mconst": mc, "mconst2": mc2,
    }
    maps = []
    cc = fm(inp["c_ctx"])
    for b in range(8):
        cvec = np.stack([fm(inp["c"][b]), cc], axis=-1)
        m = dict(shared)
        m["xT"] = f32(np.asarray(inp["x"][b]).T)
        m["ctxT"] = f32(np.asarray(inp["ctx"][b]).T)
        m["cvec"] = f32(cvec)
        maps.append(m)
    return maps


_NC_CACHE = {}


def kernel(**inputs):
    maps = prep_inputs(inputs)
    if "nc" not in _NC_CACHE:
        _NC_CACHE["nc"] = Ker().build()
    nc = _NC_CACHE["nc"]
    res = run_bass_kernel_spmd(nc, maps, core_ids=list(range(8)))
    out = np.empty((8, SEQ, D), np.float32)
    for b in range(8):
        o = np.asarray(res.results[b]["outT"]).reshape(128, 8, SEQ)
        out[b] = np.transpose(o, (2, 1, 0)).reshape(SEQ, D)
    return out
```
